# Optimizing a Trainium2 kernel written in Bass

```python
import math
import jax, jax.numpy as jnp
from jax import lax
import numpy as np

D_MODEL = 4096
BATCH = 1
SEQ = 8192
DEPTH = 4

N_BRANCH = 4
BR_W = D_MODEL // 4
MEM_LEN = 256

HG_DK = 128
HG_HEADS = BR_W // HG_DK
HG_DV = BR_W // HG_HEADS
HG_CHUNK = 64

DF_DH = 64
DF_HEADS = BR_W // (2 * DF_DH)
DF_QBLOCK = 128

RW_N = 64
RW_HEADS = BR_W // RW_N
RW_DECAY_LORA = max(32, int(round((BR_W ** 0.5) * 1.8 / 32)) * 32)
RW_AAA_LORA = max(32, int(round((BR_W ** 0.5) * 1.8 / 32)) * 32)
RW_GATE_LORA = max(32, int(round((BR_W ** 0.8) * 0.6 / 32)) * 32)
RW_LORA_COLS = RW_DECAY_LORA + RW_AAA_LORA + RW_GATE_LORA
RW_COLS = 3 * BR_W + RW_LORA_COLS
RW_LNX_EPS = 64e-5

XA_HEADS = 4
XA_DH = BR_W // XA_HEADS

GATE_RANK = 256

IN_COLS = (2 * HG_HEADS * HG_DK + 2 * HG_HEADS * HG_DV) + 3 * BR_W + RW_COLS + BR_W + GATE_RANK

N_EXPERTS = 32
TOP_K = 4
EXPERT_FF = 256
SWIGLU_LIMIT = 7.0
SWIGLU_ALPHA = 1.702

DEEPNORM_ALPHA = (2 * DEPTH) ** 0.25
DEEPNORM_BETA = (8 * DEPTH) ** -0.25
LN_EPS = 1e-5

kernel_name = "hybrid_hgrn2_diffattn_rwkv7_memxattn_moe"


def _split(a, widths):
    out, off = [], 0
    for w in widths:
        out.append(a[..., off:off + w])
        off += w
    return out


def _layer_norm(x, g, b):
    xf = x.astype(jnp.float32)
    mu = jnp.mean(xf, axis=-1, keepdims=True)
    var = jnp.mean(jnp.square(xf - mu), axis=-1, keepdims=True)
    return ((xf - mu) * lax.rsqrt(var + LN_EPS) * g + b).astype(x.dtype)


def _rms_norm(x, g, eps):
    xf = x.astype(jnp.float32)
    return xf * lax.rsqrt(jnp.mean(xf * xf, axis=-1, keepdims=True) + eps) * g


def _hgrn2(q_raw, f_raw, i_in, og, lb, norm_g):
    B, T, _ = q_raw.shape
    nc = T // HG_CHUNK
    lbf = lb.astype(jnp.float32)
    z = f_raw.astype(jnp.float32)
    log_f = jnp.logaddexp(jnp.log(lbf), jnp.log1p(-lbf) + jax.nn.log_sigmoid(z))
    k = (1.0 - lbf) * jax.nn.sigmoid(-z)
    q = jax.nn.silu(q_raw.astype(jnp.float32))
    v = i_in.astype(jnp.float32)

    def to_chunks(a, d):
        return a.reshape(B, nc, HG_CHUNK, HG_HEADS, d).transpose(1, 0, 3, 2, 4)

    qc, kc, gc, vc = to_chunks(q, HG_DK), to_chunks(k, HG_DK), to_chunks(log_f, HG_DK), to_chunks(v, HG_DV)
    causal = jnp.tril(jnp.ones((HG_CHUNK, HG_CHUNK), bool))[:, :, None]

    def step(S, inp):
        qb, kb, gb, vb = inp
        b = jnp.cumsum(gb, axis=-2)
        o_inter = jnp.einsum('bhck,bhkv->bhcv', qb * jnp.exp(b), S)
        diff = b[:, :, :, None, :] - b[:, :, None, :, :]
        decay = jnp.exp(jnp.where(causal, diff, -jnp.inf))
        att = jnp.einsum('bhtk,bhtsk,bhsk->bhts', qb, decay, kb)
        o = o_inter + jnp.einsum('bhts,bhsv->bhtv', att, vb)
        b_last = b[:, :, -1:, :]
        S = jnp.exp(b_last[:, :, 0, :])[..., None] * S + jnp.einsum('bhsk,bhsv->bhkv', kb * jnp.exp(b_last - b), vb)
        return S, o

    S0 = jnp.zeros((B, HG_HEADS, HG_DK, HG_DV), jnp.float32)
    _, o = lax.scan(step, S0, (qc, kc, gc, vc))
    o = o.transpose(1, 0, 3, 2, 4).reshape(B, T, HG_HEADS, HG_DV)
    gate = jax.nn.silu(og.astype(jnp.float32)).reshape(B, T, HG_HEADS, HG_DV)
    o = _rms_norm(o, norm_g, 1e-5) * gate
    return o.reshape(B, T, BR_W).astype(i_in.dtype)


def _diff_attn(q, k, v, lam, lam_init, subln_g):
    B, T, _ = q.shape
    scale = DF_DH ** -0.5
    q = q.reshape(B, T, DF_HEADS, 2, DF_DH).transpose(0, 2, 3, 1, 4)
    k = k.reshape(B, T, DF_HEADS, 2, DF_DH).transpose(0, 2, 3, 1, 4)
    v = v.reshape(B, T, DF_HEADS, 2 * DF_DH).transpose(0, 2, 1, 3)
    nb = T // DF_QBLOCK
    q_blocks = q.reshape(B, DF_HEADS, 2, nb, DF_QBLOCK, DF_DH).transpose(3, 0, 1, 2, 4, 5)
    kpos = jnp.arange(T)

    def block(args):
        qb, start = args
        s = jnp.einsum('bhmqd,bhmkd->bhmqk', qb, k).astype(jnp.float32) * scale
        qpos = start + jnp.arange(DF_QBLOCK)
        mask = kpos[None, :] <= qpos[:, None]
        p = jax.nn.softmax(jnp.where(mask, s, -jnp.inf), axis=-1)
        a = p[:, :, 0] - lam * p[:, :, 1]
        return jnp.einsum('bhqk,bhkv->bhqv', a.astype(v.dtype), v)

    starts = jnp.arange(nb) * DF_QBLOCK
    o = lax.map(block, (q_blocks, starts))
    o = o.transpose(1, 0, 3, 2, 4).reshape(B, T, DF_HEADS, 2 * DF_DH)
    o = _rms_norm(o, subln_g, 1e-5) * (1.0 - lam_init)
    return o.reshape(B, T, BR_W).astype(v.dtype)


def _rwkv7(streams, shift_mu, w0, w2, a0, a2, g2, k_k, k_a, r_k, lnx_g, lnx_b):
    f32 = jnp.float32
    B, T, _ = streams.shape
    prev = jnp.pad(streams, ((0, 0), (1, 0), (0, 0)))[:, :-1]
    xs = streams + (prev - streams) * shift_mu
    r, k, v, wd, ad, gd = _split(xs, [BR_W, BR_W, BR_W, RW_DECAY_LORA, RW_AAA_LORA, RW_GATE_LORA])
    w_log = -jax.nn.softplus(-(w0 + jnp.tanh(wd) @ w2).astype(f32)) - 0.5
    decay = jnp.exp(-jnp.exp(w_log))
    a = jax.nn.sigmoid((a0 + ad @ a2).astype(f32))
    g = jax.nn.sigmoid(gd) @ g2
    heads = lambda t: t.astype(f32).reshape(B, T, RW_HEADS, RW_N)
    kk = heads(k * k_k)
    kk = kk / jnp.maximum(jnp.sqrt(jnp.sum(kk * kk, axis=-1, keepdims=True)), 1e-12)
    k = k.astype(f32) * (1.0 + (a - 1.0) * k_a)
    rh, kh, vh, wh, ah = heads(r), heads(k), heads(v), heads(decay), heads(a)

    def step(S, inp):
        r_t, w_t, k_t, v_t, kk_t, a_t = inp
        sa = jnp.einsum('bhvk,bhk->bhv', S, -kk_t)
        S = S * w_t[:, :, None, :] + sa[..., None] * (kk_t * a_t)[:, :, None, :] + v_t[..., None] * k_t[:, :, None, :]
        return S, jnp.einsum('bhvk,bhk->bhv', S, r_t)

    tmaj = lambda t: t.transpose(1, 0, 2, 3)
    S0 = jnp.zeros((B, RW_HEADS, RW_N, RW_N), f32)
    _, o = lax.scan(step, S0, (tmaj(rh), tmaj(wh), tmaj(kh), tmaj(vh), tmaj(kk), tmaj(ah)))
    o = o.transpose(1, 0, 2, 3)
    mu = jnp.mean(o, axis=-1, keepdims=True)
    var = jnp.mean(jnp.square(o - mu), axis=-1, keepdims=True)
    o = ((o - mu) * lax.rsqrt(var + RW_LNX_EPS)).reshape(B, T, BR_W) * lnx_g + lnx_b
    bonus = jnp.sum(rh * kh * r_k, axis=-1, keepdims=True) * vh
    o = (o + bonus.reshape(B, T, BR_W)) * g
    return o.astype(streams.dtype)


def _mem_attn(q, mem_k, mem_v):
    B, T, _ = q.shape
    M = mem_k.shape[1]
    qh = q.reshape(B, T, XA_HEADS, XA_DH)
    kh = mem_k.reshape(B, M, XA_HEADS, XA_DH)
    vh = mem_v.reshape(B, M, XA_HEADS, XA_DH)
    s = jnp.einsum('bthd,bmhd->bhtm', qh, kh).astype(jnp.float32) * (XA_DH ** -0.5)
    p = jax.nn.softmax(s, axis=-1)
    o = jnp.einsum('bhtm,bmhd->bthd', p.astype(vh.dtype), vh)
    return o.reshape(B, T, BR_W)


def _moe(h, router_w, router_b, w1, b1, w2, b2):
    B, T, D = h.shape
    t = h.reshape(B * T, D)
    logits = (t @ router_w + router_b).astype(jnp.float32)
    top_v, top_i = lax.top_k(logits, TOP_K)
    top_p = jax.nn.softmax(top_v, axis=-1)
    gates = jnp.sum(jax.nn.one_hot(top_i, N_EXPERTS, dtype=jnp.float32) * top_p[..., None], axis=1)
    hid = jnp.einsum('nd,edf->nef', t, w1) + b1
    x_glu = jnp.minimum(hid[..., ::2], SWIGLU_LIMIT)
    x_lin = jnp.clip(hid[..., 1::2], -SWIGLU_LIMIT, SWIGLU_LIMIT)
    act = x_glu * jax.nn.sigmoid(SWIGLU_ALPHA * x_glu) * (x_lin + 1.0)
    out = jnp.einsum('nef,efd->nd', act * gates[..., None], w2) + gates @ b2
    return out.reshape(B, T, D)


def setup_inputs(seed: int = 0) -> dict:
    key = jax.random.key(seed)
    ks = iter(jax.random.split(key, 48))
    f32 = jnp.float32

    def nrm(shape, scale):
        return jax.random.normal(next(ks), shape, f32) * scale

    def gain(shape):
        return 1.0 + 0.02 * jax.random.normal(next(ks), shape, f32)

    L, D = DEPTH, D_MODEL
    return {
        "x": nrm((BATCH, SEQ, D), 1.0),
        "mem": nrm((BATCH, MEM_LEN, D), 1.0),
        "mem_ln_g": gain((D,)),
        "mem_ln_b": nrm((D,), 0.02),
        "w_in": nrm((L, D, IN_COLS), D ** -0.5),
        "hg_lb_raw": nrm((L, HG_HEADS * HG_DK), 1.0),
        "hg_norm_g": gain((L, HG_DV)),
        "df_lam_q1": nrm((L, DF_DH), 0.1),
        "df_lam_k1": nrm((L, DF_DH), 0.1),
        "df_lam_q2": nrm((L, DF_DH), 0.1),
        "df_lam_k2": nrm((L, DF_DH), 0.1),
        "df_subln_g": gain((L, 2 * DF_DH)),
        "rw_shift_mu": jax.random.uniform(next(ks), (L, RW_COLS), f32),
        "rw_w0": nrm((L, BR_W), 0.5),
        "rw_w2": nrm((L, RW_DECAY_LORA, BR_W), RW_DECAY_LORA ** -0.5),
        "rw_a0": nrm((L, BR_W), 0.1),
        "rw_a2": nrm((L, RW_AAA_LORA, BR_W), RW_AAA_LORA ** -0.5),
        "rw_g2": nrm((L, RW_GATE_LORA, BR_W), RW_GATE_LORA ** -0.5),
        "rw_k_k": 0.85 + nrm((L, BR_W), 0.02),
        "rw_k_a": gain((L, BR_W)),
        "rw_r_k": nrm((L, RW_HEADS, RW_N), 0.1),
        "rw_lnx_g": gain((L, BR_W)),
        "rw_lnx_b": nrm((L, BR_W), 0.02),
        "w_mem_kv": nrm((L, D, 2 * BR_W), D ** -0.5),
        "w_br": nrm((L, N_BRANCH, BR_W, D), (BR_W ** -0.5) * DEEPNORM_BETA),
        "w_gate_up": nrm((L, GATE_RANK, N_BRANCH * D), GATE_RANK ** -0.5),
        "b_gate": nrm((L, N_BRANCH * D), 0.02),
        "w_o": nrm((L, D, D), (D ** -0.5) * DEEPNORM_BETA),
        "ln1_g": gain((L, D)),
        "ln1_b": nrm((L, D), 0.02),
        "router_w": nrm((L, D, N_EXPERTS), D ** -0.5),
        "router_b": nrm((L, N_EXPERTS), 0.01),
        "exp_w1": nrm((L, N_EXPERTS, D, 2 * EXPERT_FF), D ** -0.5),
        "exp_b1": nrm((L, N_EXPERTS, 2 * EXPERT_FF), 0.02),
        "exp_w2": nrm((L, N_EXPERTS, EXPERT_FF, D), (EXPERT_FF ** -0.5) * DEEPNORM_BETA),
        "exp_b2": nrm((L, N_EXPERTS, D), 0.02),
        "ln2_g": gain((L, D)),
        "ln2_b": nrm((L, D), 0.02),
    }


def reference(x, mem, mem_ln_g, mem_ln_b, w_in, hg_lb_raw, hg_norm_g, df_lam_q1, df_lam_k1, df_lam_q2, df_lam_k2,
              df_subln_g, rw_shift_mu, rw_w0, rw_w2, rw_a0, rw_a2, rw_g2, rw_k_k, rw_k_a, rw_r_k, rw_lnx_g, rw_lnx_b,
              w_mem_kv, w_br, w_gate_up, b_gate, w_o, ln1_g, ln1_b, router_w, router_b, exp_w1, exp_b1, exp_w2,
              exp_b2, ln2_g, ln2_b):
    f32 = jnp.float32
    memn = _layer_norm(mem, mem_ln_g, mem_ln_b)
    lb_cum = jnp.cumsum(jax.nn.softmax(hg_lb_raw.astype(f32), axis=0), axis=0)
    lb_all = lb_cum - lb_cum[:1]
    in_widths = [HG_HEADS * HG_DK, HG_HEADS * HG_DK, HG_HEADS * HG_DV, HG_HEADS * HG_DV,
                 2 * DF_HEADS * DF_DH, 2 * DF_HEADS * DF_DH, 2 * DF_HEADS * DF_DH,
                 RW_COLS, BR_W, GATE_RANK]
    for l in range(DEPTH):
        proj = x @ w_in[l]
        hq, hf, hi, hog, dq, dk, dv, rw, xq, gdown = _split(proj, in_widths)
        o_hg = _hgrn2(hq, hf, hi, hog, lb_all[l], hg_norm_g[l])
        lam_init = 0.8 - 0.6 * math.exp(-0.3 * l)
        lam = (jnp.exp(jnp.sum(df_lam_q1[l] * df_lam_k1[l]).astype(f32))
               - jnp.exp(jnp.sum(df_lam_q2[l] * df_lam_k2[l]).astype(f32)) + lam_init)
        o_df = _diff_attn(dq, dk, dv, lam, lam_init, df_subln_g[l])
        o_rw = _rwkv7(rw, rw_shift_mu[l], rw_w0[l], rw_w2[l], rw_a0[l], rw_a2[l], rw_g2[l],
                      rw_k_k[l], rw_k_a[l], rw_r_k[l], rw_lnx_g[l], rw_lnx_b[l])
        mem_k, mem_v = _split(memn @ w_mem_kv[l], [BR_W, BR_W])
        o_xa = _mem_attn(xq, mem_k, mem_v)
        gates = jax.nn.sigmoid((gdown @ w_gate_up[l] + b_gate[l]).astype(f32))
        merged = 0.0
        for n, o_b in enumerate((o_hg, o_df, o_rw, o_xa)):
            merged = merged + gates[..., n * D_MODEL:(n + 1) * D_MODEL] * (o_b @ w_br[l, n])
        y = merged.astype(x.dtype) @ w_o[l]
        x = _layer_norm(DEEPNORM_ALPHA * x + y.astype(x.dtype), ln1_g[l], ln1_b[l])
        y = _moe(x, router_w[l], router_b[l], exp_w1[l], exp_b1[l], exp_w2[l], exp_b2[l])
        x = _layer_norm(DEEPNORM_ALPHA * x + y.astype(x.dtype), ln2_g[l], ln2_b[l])
    return x
```

```python
import math
import numpy as np
import concourse.bass as bass
import concourse.mybir as mybir
from concourse.bass_utils import run_bass_kernel_spmd

F32 = mybir.dt.float32
BF16 = mybir.dt.bfloat16
AF = mybir.ActivationFunctionType
ALU = mybir.AluOpType
AX = mybir.AxisListType

D = 4096
T_FULL = 8192
NCORE = 8
KC = D // 128
ALPHA = (2 * 4) ** 0.25


class Prog:
    def __init__(self, self_sync=True):
        self.nc = bass.Bass("TRN2", target_bir_lowering=False)
        nc = self.nc
        self.eng = dict(pe=nc.tensor, act=nc.scalar, dve=nc.vector, pool=nc.gpsimd, sp=nc.sync)
        self.sems = {}
        self.cnt = {}
        self.seen = {k: {} for k in self.eng}
        self.last_w = {}
        self.rd = {}
        self.self_sync = self_sync

    def sem(self, name):
        if name not in self.sems:
            self.sems[name] = self.nc.alloc_semaphore(name=name)
            self.cnt[name] = 0
        return self.sems[name]

    def _deps(self, reads, writes):
        deps = {}

        def add(s, v):
            if deps.get(s, 0) < v:
                deps[s] = v

        for k in reads:
            d = self.last_w.get(k)
            if d is not None:
                add(*d)
        for k in writes:
            d = self.last_w.get(k)
            if d is not None:
                add(*d)
            for s, v in self.rd.get(k, {}).items():
                add(s, v)
        return deps

    def _wait(self, e, deps, nosync=False):
        own = "E_" + e
        for s, v in deps.items():
            if s == own and (nosync or not self.self_sync):
                continue
            if self.seen[e].get(s, 0) < v:
                self.eng[e].wait_ge(self.sems[s], v)
                self.seen[e][s] = v

    def _record(self, tok, reads, writes):
        s, v = tok
        for k in reads:
            d = self.rd.setdefault(k, {})
            if d.get(s, 0) < v:
                d[s] = v
        for k in writes:
            self.last_w[k] = tok
            self.rd[k] = {}

    def op(self, e, fn, reads=(), writes=(), nosync=False):
        self._wait(e, self._deps(reads, writes), nosync)
        ins = fn(self.eng[e])
        s = "E_" + e
        self.sem(s)
        self.cnt[s] += 1
        ins.then_inc(self.sems[s], 1)
        self._record((s, self.cnt[s]), reads, writes)
        return ins

    def dma(self, q, out, in_, reads=(), writes=(), sem="x", **kw):
        self._wait(q, self._deps(reads, writes))
        ins = self.eng[q].dma_start(out=out, in_=in_, **kw)
        s = "D_" + sem
        self.sem(s)
        self.cnt[s] += 16
        ins.then_inc(self.sems[s], 16)
        self._record((s, self.cnt[s]), reads, writes)
        return ins

    def finish(self, keys, e="sp"):
        self._wait(e, self._deps(keys, ()))

    def sb(self, name, shape, dt):
        return self.nc.alloc_sbuf_tensor(name, shape, dt)

    def ps(self, name, shape=(128, 512), dt=F32):
        return self.nc.alloc_psum_tensor(name, list(shape), dt)

    def din(self, name, shape, dt=F32):
        return self.nc.dram_tensor(name, list(shape), dt, kind="ExternalInput").ap()

    def dout(self, name, shape, dt=F32):
        return self.nc.dram_tensor(name, list(shape), dt, kind="ExternalOutput").ap()


def _consts(p):
    cid = p.din("c_ident", [128, 128])
    ident = p.sb("ident", [128, 128], F32)
    ones = p.sb("ones", [128, 128], F32)
    p.dma("sp", ident[:, :], cid, writes=["ident"], sem="cst")
    p.op("pool", lambda e: e.memset(ones[:, :], 1.0), writes=["ones"])
    return ident, ones


def load_xt_tile(p, xT, xt_sb, key, tt, TT=512):
    src = xT.rearrange("(kc p) t -> p kc t", p=128)
    for h in range(2):
        p.dma("pool", xt_sb[:, h * 16:(h + 1) * 16, :], src[:, h * 16:(h + 1) * 16, tt * TT:(tt + 1) * TT],
              writes=[key], sem=key)


def load_w_cols(p, w_dram, w_sb, key, ncols):
    src = w_dram.rearrange("(kc p) n -> p kc n", p=128)
    for h in range(4):
        p.dma("pool", w_sb[:, h * 8:(h + 1) * 8, :], src[:, h * 8:(h + 1) * 8, :], writes=[key], sem=key)


def inproj_fm(p, ps, pskey, w_sb, wkey, c0, ncol, xt_sb, xkey, n=512):
    for kc in range(KC):
        p.op("pe", lambda e: e.matmul(ps[0:ncol, 0:n], w_sb[:, kc, c0:c0 + ncol], xt_sb[:, kc, 0:n],
                                      start=(kc == 0), stop=(kc == KC - 1)),
             reads=[wkey, xkey], writes=[pskey])


def build_hgrn2(T=T_FULL, self_sync=True):
    p = Prog(self_sync=self_sync)
    xT = p.din("xT", [D, T])
    w = p.din("w_hg", [D, 512])
    lbraw = p.din("lb_raw", [128, 4])
    lmask = p.din("lmask", [128, 4])
    ng = p.din("norm_g", [128, 1])
    out = p.dout("o_hg", [128, T])
    ident, ones = _consts(p)
    NT = T // 512

    w_sb = p.sb("w_sb", [128, KC, 512], BF16)
    load_w_cols(p, w, w_sb, "w_sb", 512)
    xt = [p.sb(f"xt{i}", [128, KC, 512], BF16) for i in range(2)]
    sm = p.sb("sm", [128, 8], F32)
    lb = p.sb("lb", [128, 4], F32)
    p.dma("sp", sm[:, 0:4], lbraw, writes=["sm"], sem="sm")
    p.dma("sp", sm[:, 4:8], lmask, writes=["sm"], sem="sm")
    p.dma("sp", lb[:, 2:3], ng, writes=["lb"], sem="lb")
    p.op("act", lambda e: e.activation(out=sm[:, 0:4], in_=sm[:, 0:4], func=AF.Exp), reads=["sm"], writes=["sm"])
    p.op("dve", lambda e: e.reduce_sum(out=lb[:, 3:4], in_=sm[:, 0:4], axis=AX.X), reads=["sm"], writes=["lb"])
    p.op("dve", lambda e: e.reciprocal(out=lb[:, 3:4], in_=lb[:, 3:4]), reads=["lb"], writes=["lb"])
    p.op("dve", lambda e: e.tensor_mul(out=sm[:, 0:4], in0=sm[:, 0:4], in1=sm[:, 4:8]), reads=["sm"], writes=["sm"])
    p.op("dve", lambda e: e.reduce_sum(out=lb[:, 0:1], in_=sm[:, 0:4], axis=AX.X), reads=["sm"], writes=["lb"])
    p.op("dve", lambda e: e.tensor_mul(out=lb[:, 0:1], in0=lb[:, 0:1], in1=lb[:, 3:4]), reads=["lb"], writes=["lb"])
    p.op("dve", lambda e: e.tensor_scalar(out=lb[:, 1:2], in0=lb[:, 0:1], scalar1=-1.0, scalar2=1.0,
                                          op0=ALU.mult, op1=ALU.add), reads=["lb"], writes=["lb"])

    pq, pf, pi_, pog = (p.ps(n) for n in ("pq", "pf", "pi", "pog"))
    ib = [p.ps("ib0"), p.ps("ib1")]
    o_ps = p.ps("o_ps")
    ssq = p.ps("ssq")
    qT = p.sb("qT", [128, 512], F32)
    fT = p.sb("fT", [128, 512], F32)
    kT = p.sb("kT", [128, 512], F32)
    iT = p.sb("iT", [128, 512], F32)
    gT = p.sb("gT", [128, 512], F32)
    sg = p.sb("sg", [128, 512], F32)
    Dg = [p.sb(f"Dg{i}", [128, 4, 128], F32) for i in range(2)]
    S = [p.sb(f"S{i}", [128, 128], F32) for i in range(2)]
    t1 = [p.sb(f"t1{i}", [128, 128], F32) for i in range(2)]
    osb = p.sb("osb", [128, 512], F32)
    osq = p.sb("osq", [128, 512], F32)
    ob = [p.sb(f"ob{i}", [128, 512], F32) for i in range(2)]
    p.op("pool", lambda e: e.memset(S[0][:, :], 0.0), writes=["S0"])
    load_xt_tile(p, xT, xt[0], "xt0", 0)
    tok = 0
    for tt in range(NT):
        xk = f"xt{tt % 2}"
        xs = xt[tt % 2]
        if tt + 1 < NT:
            load_xt_tile(p, xT, xt[(tt + 1) % 2], f"xt{(tt + 1) % 2}", tt + 1)
        for j, (ps_, k_) in enumerate(((pq, "pq"), (pf, "pf"), (pi_, "pi"), (pog, "pog"))):
            inproj_fm(p, ps_, k_, w_sb, "w_sb", j * 128, 128, xs, xk)
        p.op("act", lambda e: e.activation(out=sg[:, :], in_=pq[:, :], func=AF.Sigmoid), reads=["pq"], writes=["sg"])
        p.op("dve", lambda e: e.tensor_mul(out=qT[:, :], in0=pq[:, :], in1=sg[:, :]), reads=["pq", "sg"], writes=["qT"])
        p.op("act", lambda e: e.activation(out=sg[:, :], in_=pf[:, :], func=AF.Sigmoid), reads=["pf"], writes=["sg"])
        p.op("dve", lambda e: e.tensor_scalar(out=fT[:, :], in0=sg[:, :], scalar1=lb[:, 1:2], scalar2=lb[:, 0:1],
                                              op0=ALU.mult, op1=ALU.add), reads=["sg", "lb"], writes=["fT"])
        p.op("dve", lambda e: e.tensor_scalar(out=kT[:, :], in0=fT[:, :], scalar1=-1.0, scalar2=1.0,
                                              op0=ALU.mult, op1=ALU.add), reads=["fT"], writes=["kT"])
        p.op("act", lambda e: e.activation(out=iT[:, :], in_=pi_[:, :], func=AF.Copy), reads=["pi"], writes=["iT"])
        p.op("act", lambda e: e.activation(out=sg[:, :], in_=pog[:, :], func=AF.Sigmoid), reads=["pog"], writes=["sg"])
        p.op("dve", lambda e: e.tensor_mul(out=gT[:, :], in0=pog[:, :], in1=sg[:, :]), reads=["pog", "sg"], writes=["gT"])
        for g in range(128):
            b = g % 2
            t0 = g * 4
            p.op("pool", lambda e: e.tensor_tensor(
                out=Dg[b][:, :, :], in0=iT[:, t0:t0 + 4].unsqueeze(2).to_broadcast([128, 4, 128]),
                in1=ident[:, :].unsqueeze(1).to_broadcast([128, 4, 128]), op=ALU.mult),
                reads=["iT", "ident"], writes=[f"Dg{b}"], nosync=True)
            p.op("pe", lambda e: e.matmul(ib[b][:, :], ones[:, :], Dg[b][:, :, :].rearrange("p a b -> p (a b)"),
                                          start=True, stop=True), reads=[f"Dg{b}", "ones"], writes=[f"ib{b}"], nosync=True)
            for j in range(4):
                t = t0 + j
                so, sn = tok % 2, (tok + 1) % 2
                tb = tok % 2
                p.op("dve", lambda e: e.tensor_scalar(out=t1[tb][:, :], in0=ib[b][:, j * 128:(j + 1) * 128],
                                                      scalar1=kT[:, t:t + 1], scalar2=None, op0=ALU.mult),
                     reads=[f"ib{b}", "kT"], writes=[f"t1{tb}"], nosync=True)
                p.op("dve", lambda e: e.scalar_tensor_tensor(out=S[sn][:, :], in0=S[so][:, :], scalar=fT[:, t:t + 1],
                                                             in1=t1[tb][:, :], op0=ALU.mult, op1=ALU.add),
                     reads=[f"S{so}", "fT", f"t1{tb}"], writes=[f"S{sn}"], nosync=True)
                p.op("pe", lambda e: e.matmul(o_ps[:, t:t + 1], S[sn][:, :], qT[:, t:t + 1], start=True, stop=True),
                     reads=[f"S{sn}", "qT"], writes=["o_ps"], nosync=True)
                tok += 1
        p.op("act", lambda e: e.activation(out=osb[:, :], in_=o_ps[:, :], func=AF.Copy), reads=["o_ps"], writes=["osb"])
        p.op("act", lambda e: e.activation(out=osq[:, :], in_=o_ps[:, :], func=AF.Square), reads=["o_ps"], writes=["osq"])
        p.op("pe", lambda e: e.matmul(ssq[:, :], ones[:, :], osq[:, :], start=True, stop=True),
             reads=["osq", "ones"], writes=["ssq"])
        p.op("dve", lambda e: e.tensor_scalar(out=osq[:, :], in0=ssq[:, :], scalar1=1.0 / 128, scalar2=1e-5,
                                              op0=ALU.mult, op1=ALU.add), reads=["ssq"], writes=["osq"])
        p.op("act", lambda e: e.activation(out=osq[:, :], in_=osq[:, :], func=AF.Sqrt), reads=["osq"], writes=["osq"])
        p.op("dve", lambda e: e.reciprocal(out=osq[:, :], in_=osq[:, :]), reads=["osq"], writes=["osq"])
        p.op("dve", lambda e: e.tensor_mul(out=osb[:, :], in0=osb[:, :], in1=osq[:, :]), reads=["osb", "osq"], writes=["osb"])
        obk = f"ob{tt % 2}"
        p.op("dve", lambda e: e.scalar_tensor_tensor(out=ob[tt % 2][:, :], in0=osb[:, :], scalar=lb[:, 2:3], in1=gT[:, :],
                                                     op0=ALU.mult, op1=ALU.mult), reads=["osb", "gT", "lb"], writes=[obk])
        p.dma("sp", out[:, tt * 512:(tt + 1) * 512], ob[tt % 2][:, :], reads=[obk], writes=["out"], sem="out")
    p.finish(["out"])
    return p


def build_hgrn2c(T=T_FULL):
    p = Prog()
    xT = p.din("xT", [D, T])
    w = p.din("w_hg", [D, 512])
    lbraw = p.din("lb_raw", [128, 4])
    lmask = p.din("lmask", [128, 4])
    ng = p.din("norm_g", [128, 1])
    tri_d = p.din("c_tri", [64, 64])
    out = p.dout("o_hg", [128, T])
    ident, ones = _consts(p)
    NT = T // 512
    w_sb = p.sb("w_sb", [128, KC, 512], BF16)
    load_w_cols(p, w, w_sb, "w_sb", 512)
    xt = [p.sb(f"xt{i}", [128, KC, 512], BF16) for i in range(2)]
    sm = p.sb("sm", [128, 8], F32)
    lb = p.sb("lb", [128, 4], F32)
    tri = p.sb("tri", [64, 64], F32)
    identb = p.sb("identb", [128, 128], BF16)
    p.dma("sp", sm[:, 0:4], lbraw, writes=["sm"], sem="sm")
    p.dma("sp", sm[:, 4:8], lmask, writes=["sm"], sem="sm")
    p.dma("sp", lb[:, 2:3], ng, writes=["lb"], sem="lb")
    p.dma("sp", tri[:, :], tri_d, writes=["tri"], sem="tri")
    p.op("act", lambda e: e.activation(out=identb[:, :], in_=ident[:, :], func=AF.Copy), reads=["ident"], writes=["identb"])
    p.op("act", lambda e: e.activation(out=sm[:, 0:4], in_=sm[:, 0:4], func=AF.Exp), reads=["sm"], writes=["sm"])
    p.op("dve", lambda e: e.reduce_sum(out=lb[:, 3:4], in_=sm[:, 0:4], axis=AX.X), reads=["sm"], writes=["lb"])
    p.op("dve", lambda e: e.reciprocal(out=lb[:, 3:4], in_=lb[:, 3:4]), reads=["lb"], writes=["lb"])
    p.op("dve", lambda e: e.tensor_mul(out=sm[:, 0:4], in0=sm[:, 0:4], in1=sm[:, 4:8]), reads=["sm"], writes=["sm"])
    p.op("dve", lambda e: e.reduce_sum(out=lb[:, 0:1], in_=sm[:, 0:4], axis=AX.X), reads=["sm"], writes=["lb"])
    p.op("dve", lambda e: e.tensor_mul(out=lb[:, 0:1], in0=lb[:, 0:1], in1=lb[:, 3:4]), reads=["lb"], writes=["lb"])
    p.op("dve", lambda e: e.tensor_scalar(out=lb[:, 1:2], in0=lb[:, 0:1], scalar1=-1.0, scalar2=1.0,
                                          op0=ALU.mult, op1=ALU.add), reads=["lb"], writes=["lb"])
    wk = [p.ps("wk0"), p.ps("wk1")]
    trp = [p.ps("trp0", (128, 512), BF16), p.ps("trp1", (128, 512), BF16)]
    attp = p.ps("attp")
    o_ps = p.ps("o_ps")
    snp = [p.ps("snp0"), p.ps("snp1")]
    F = {n: p.sb("h_" + n, [128, 512], F32) for n in ["qT", "fT", "kT", "gT", "sg", "bA", "bB", "e", "d2", "osb", "osq"]}
    B = {n: p.sb("hb_" + n, [128, 512], BF16) for n in ["ib", "qd", "kd", "qb", "kl"]}
    el = p.sb("el", [128, 8], F32)
    S = p.sb("S", [128, 128], F32)
    Sb = p.sb("Sb", [128, 128], BF16)
    tm = [p.sb(f"tm{i}", [64, 256], BF16) for i in range(2)]
    attb = [p.sb(f"attb{i}", [64, 64], BF16) for i in range(2)]
    ob = [p.sb(f"ob{i}", [128, 512], F32) for i in range(2)]
    p.op("pool", lambda e: e.memset(S[:, :], 0.0), writes=["S"])
    p.op("pool", lambda e: e.memset(Sb[:, :], 0.0), writes=["Sb"])
    load_xt_tile(p, xT, xt[0], "xt0", 0)

    def v3(t):
        return t[:, :].rearrange("p (c s) -> p c s", s=64)

    for tt in range(NT):
        xk = f"xt{tt % 2}"
        xs = xt[tt % 2]
        if tt + 1 < NT:
            load_xt_tile(p, xT, xt[(tt + 1) % 2], f"xt{(tt + 1) % 2}", tt + 1)
        inproj_fm(p, wk[0], "wk0", w_sb, "w_sb", 0, 128, xs, xk)
        p.op("act", lambda e: e.activation(out=F["sg"][:, :], in_=wk[0][:, :], func=AF.Sigmoid), reads=["wk0"], writes=["sg"])
        p.op("dve", lambda e: e.tensor_mul(out=F["qT"][:, :], in0=wk[0][:, :], in1=F["sg"][:, :]), reads=["wk0", "sg"], writes=["qT"])
        inproj_fm(p, wk[1], "wk1", w_sb, "w_sb", 128, 128, xs, xk)
        p.op("act", lambda e: e.activation(out=F["sg"][:, :], in_=wk[1][:, :], func=AF.Sigmoid), reads=["wk1"], writes=["sg"])
        p.op("dve", lambda e: e.tensor_scalar(out=F["fT"][:, :], in0=F["sg"][:, :], scalar1=lb[:, 1:2], scalar2=lb[:, 0:1],
                                              op0=ALU.mult, op1=ALU.add), reads=["sg", "lb"], writes=["fT"])
        p.op("dve", lambda e: e.tensor_scalar(out=F["kT"][:, :], in0=F["fT"][:, :], scalar1=-1.0, scalar2=1.0,
                                              op0=ALU.mult, op1=ALU.add), reads=["fT"], writes=["kT"])
        inproj_fm(p, wk[0], "wk0", w_sb, "w_sb", 256, 128, xs, xk)
        p.op("act", lambda e: e.activation(out=B["ib"][:, :], in_=wk[0][:, :], func=AF.Copy), reads=["wk0"], writes=["ib"])
        inproj_fm(p, wk[1], "wk1", w_sb, "w_sb", 384, 128, xs, xk)
        p.op("act", lambda e: e.activation(out=F["sg"][:, :], in_=wk[1][:, :], func=AF.Sigmoid), reads=["wk1"], writes=["sg"])
        p.op("dve", lambda e: e.tensor_mul(out=F["gT"][:, :], in0=wk[1][:, :], in1=F["sg"][:, :]), reads=["wk1", "sg"], writes=["gT"])
        p.op("act", lambda e: e.activation(out=F["bA"][:, :], in_=F["fT"][:, :], func=AF.Ln), reads=["fT"], writes=["bA"])
        src, dst = "bA", "bB"
        for d in (1, 2, 4, 8, 16, 32):
            p.op("pool", lambda e: e.tensor_copy(out=v3(F[dst])[:, :, 0:d], in_=v3(F[src])[:, :, 0:d]), reads=[src], writes=[dst])
            p.op("dve", lambda e: e.tensor_tensor(out=v3(F[dst])[:, :, d:64], in0=v3(F[src])[:, :, d:64], in1=v3(F[src])[:, :, 0:64 - d],
                                                  op=ALU.add), reads=[src], writes=[dst])
            src, dst = dst, src
        bk = src
        ok = dst
        b3 = v3(F[bk])
        p.op("act", lambda e: e.activation(out=F["e"][:, :], in_=F[bk][:, :], func=AF.Exp), reads=[bk], writes=["e"])
        p.op("dve", lambda e: e.tensor_mul(out=B["qb"][:, :], in0=F["qT"][:, :], in1=F["e"][:, :]), reads=["qT", "e"], writes=["qb"])
        p.op("dve", lambda e: e.tensor_tensor(out=v3(F["d2"]), in0=b3, in1=b3[:, :, 63:64].to_broadcast([128, 8, 64]), op=ALU.subtract), reads=[bk], writes=["d2"])
        p.op("act", lambda e: e.activation(out=F["e"][:, :], in_=F["d2"][:, :], func=AF.Exp, scale=-1.0), reads=["d2"], writes=["e"])
        p.op("dve", lambda e: e.tensor_mul(out=B["kl"][:, :], in0=F["kT"][:, :], in1=F["e"][:, :]), reads=["kT", "e"], writes=["kl"])
        p.op("act", lambda e: e.activation(out=el[:, :].unsqueeze(2), in_=b3[:, :, 63:64], func=AF.Exp), reads=[bk], writes=["el"])
        p.op("dve", lambda e: e.tensor_tensor(out=v3(F["d2"]), in0=b3, in1=b3[:, :, 31:32].to_broadcast([128, 8, 64]), op=ALU.subtract), reads=[bk], writes=["d2"])
        p.op("act", lambda e: e.activation(out=F["e"][:, :], in_=F["d2"][:, :], func=AF.Exp), reads=["d2"], writes=["e"])
        p.op("dve", lambda e: e.tensor_mul(out=B["qd"][:, :], in0=F["qT"][:, :], in1=F["e"][:, :]), reads=["qT", "e"], writes=["qd"])
        p.op("act", lambda e: e.activation(out=F[ok][:, :], in_=F["d2"][:, :], func=AF.Exp, scale=-1.0), reads=["d2"], writes=[ok])
        p.op("dve", lambda e: e.tensor_mul(out=B["kd"][:, :], in0=F["kT"][:, :], in1=F[ok][:, :]), reads=["kT", ok], writes=["kd"])
        for c in range(8):
            cs_ = slice(c * 64, (c + 1) * 64)
            b = c % 2
            p.op("pe", lambda e: e.transpose(trp[b][0:64, 0:128], B["ib"][:, cs_], identb[:, :]), reads=["ib", "identb"], writes=[f"trp{b}"])
            p.op("pe", lambda e: e.transpose(trp[b][0:64, 128:256], B["kl"][:, cs_], identb[:, :]), reads=["kl", "identb"], writes=[f"trp{b}"])
            p.op("act", lambda e: e.activation(out=tm[b][:, :], in_=trp[b][0:64, 0:256], func=AF.Copy), reads=[f"trp{b}"], writes=[f"tm{b}"])
            p.op("pe", lambda e: e.matmul(attp[0:64, 0:64], B["kd"][:, cs_], B["qd"][:, cs_], start=True, stop=True), reads=["kd", "qd"], writes=["attp"])
            p.op("dve", lambda e: e.tensor_mul(out=attb[b][:, :], in0=attp[0:64, 0:64], in1=tri[:, :]), reads=["attp", "tri"], writes=[f"attb{b}"])
            p.op("pe", lambda e: e.matmul(o_ps[:, cs_], Sb[:, :], B["qb"][:, cs_], start=True, stop=False), reads=["Sb", "qb"], writes=["o_ps"])
            p.op("pe", lambda e: e.matmul(o_ps[:, cs_], tm[b][:, 0:128], attb[b][:, :], start=False, stop=True), reads=[f"tm{b}", f"attb{b}"], writes=["o_ps"])
            p.op("pe", lambda e: e.matmul(snp[b][:, 0:128], tm[b][:, 128:256], tm[b][:, 0:128], start=True, stop=True), reads=[f"tm{b}"], writes=[f"snp{b}"])
            p.op("dve", lambda e: e.scalar_tensor_tensor(out=S[:, :], in0=S[:, :], scalar=el[:, c:c + 1], in1=snp[b][:, 0:128],
                                                         op0=ALU.mult, op1=ALU.add), reads=["S", "el", f"snp{b}"], writes=["S"])
            p.op("act", lambda e: e.activation(out=Sb[:, :], in_=S[:, :], func=AF.Copy), reads=["S"], writes=["Sb"])
        p.op("act", lambda e: e.activation(out=F["osb"][:, :], in_=o_ps[:, :], func=AF.Copy), reads=["o_ps"], writes=["osb"])
        p.op("act", lambda e: e.activation(out=F["osq"][:, :], in_=o_ps[:, :], func=AF.Square), reads=["o_ps"], writes=["osq"])
        p.op("pe", lambda e: e.matmul(wk[0][:, :], ones[:, :], F["osq"][:, :], start=True, stop=True), reads=["osq", "ones"], writes=["wk0"])
        p.op("dve", lambda e: e.tensor_scalar(out=F["osq"][:, :], in0=wk[0][:, :], scalar1=1.0 / 128, scalar2=1e-5,
                                              op0=ALU.mult, op1=ALU.add), reads=["wk0"], writes=["osq"])
        p.op("act", lambda e: e.activation(out=F["osq"][:, :], in_=F["osq"][:, :], func=AF.Sqrt), reads=["osq"], writes=["osq"])
        p.op("dve", lambda e: e.reciprocal(out=F["osq"][:, :], in_=F["osq"][:, :]), reads=["osq"], writes=["osq"])
        p.op("dve", lambda e: e.tensor_mul(out=F["osb"][:, :], in0=F["osb"][:, :], in1=F["osq"][:, :]), reads=["osb", "osq"], writes=["osb"])
        obk = f"ob{tt % 2}"
        p.op("dve", lambda e: e.scalar_tensor_tensor(out=ob[tt % 2][:, :], in0=F["osb"][:, :], scalar=lb[:, 2:3], in1=F["gT"][:, :],
                                                     op0=ALU.mult, op1=ALU.mult), reads=["osb", "gT", "lb"], writes=[obk])
        p.dma("sp", out[:, tt * 512:(tt + 1) * 512], ob[tt % 2][:, :], reads=[obk], writes=["out"], sem="out")
    p.finish(["out"])
    return p


def build_rwkv(T=T_FULL, stage=99, self_sync=True):
    p = Prog(self_sync=self_sync)
    xT = p.din("xT", [D, T])
    w = p.din("w_rw", [D, 672])
    mu = p.din("rw_mu", [128, 6])
    lw = p.din("rw_lora", [128, 512])
    vec = p.din("rw_vec", [128, 8])
    cst = p.din("c_rw", [128, 256])
    out = p.dout("o_rw", [128, T])
    NT = T // 512
    w_sb = p.sb("w_sb", [128, KC, 672], BF16)
    load_w_cols(p, w, w_sb, "w_sb", 672)
    xt = [p.sb(f"xt{i}", [128, KC, 512], BF16) for i in range(2)]
    mus = p.sb("mus", [128, 12], F32)
    lws = p.sb("lws", [128, 512], BF16)
    vs_ = p.sb("vecs", [128, 8], F32)
    cs = p.sb("cs", [128, 256], F32)
    p.dma("sp", mus[:, 0:6], mu, writes=["mus"], sem="c0")
    p.dma("pool", lws[:, :], lw, writes=["lws"], sem="c1")
    p.dma("sp", vs_[:, :], vec, writes=["vecs"], sem="c2")
    p.dma("sp", cs[:, :], cst, writes=["cs"], sem="c3")
    p.op("dve", lambda e: e.tensor_scalar(out=mus[:, 6:12], in0=mus[:, 0:6], scalar1=-1.0, scalar2=1.0,
                                          op0=ALU.mult, op1=ALU.add), reads=["mus"], writes=["mus"])
    ident2 = cs[:, 0:64]
    bones = cs[:, 64:192]
    wk = [p.ps("wk0"), p.ps("wk1")]
    br = [[p.ps(f"br{b}{i}") for i in range(3)] for b in range(2)]
    raw = [p.sb(f"raw{j}", [128, 520], F32) for j in range(6)]
    for j in range(6):
        p.op("pool", lambda e: e.memset(raw[j][:, 0:8], 0.0), writes=[f"raw{j}"])
    names = ["rs", "ks", "vs", "was", "g1s", "g2s", "tmp", "wT", "aT", "kkT", "kka", "nkk", "kmod", "bon", "gT", "oT",
             "cen", "sq"]
    A = {n: p.sb("a_" + n, [128, 512], F32) for n in names}
    wab = p.sb("wab", [128, 512], BF16)
    sg1 = p.sb("sg1", [128, 512], BF16)
    sg2 = p.sb("sg2", [128, 512], BF16)
    Dq = [[p.sb(f"Dq{b}{i}", [128, 4, 64], F32) for i in range(5)] for b in range(2)]
    S = p.sb("S", [128, 64], F32)
    S1 = p.sb("S1", [128, 64], F32)
    junk = p.sb("junk", [128, 64], F32)
    sa = p.sb("sa", [128, 2], F32)
    ob = [p.sb(f"ob{i}", [128, 512], F32) for i in range(2)]
    p.op("pool", lambda e: e.memset(S[:, :], 0.0), writes=["S"])
    load_xt_tile(p, xT, xt[0], "xt0", 0)

    def ew(eng, fn, r, w_, ns=False):
        p.op(eng, fn, reads=r, writes=w_, nosync=ns)

    for tt in range(NT):
        xk = f"xt{tt % 2}"
        xs = xt[tt % 2]
        if tt + 1 < NT:
            load_xt_tile(p, xT, xt[(tt + 1) % 2], f"xt{(tt + 1) % 2}", tt + 1)
        blocks = [(0, 128), (128, 128), (256, 128), (384, 128), (512, 128), (640, 32)]
        shifted = ["rs", "ks", "vs", "was", "g1s", "g2s"]
        for j, (c0, nc_) in enumerate(blocks):
            ps_ = wk[j % 2]
            pk = f"wk{j % 2}"
            inproj_fm(p, ps_, pk, w_sb, "w_sb", c0, nc_, xs, xk)
            rj = raw[j]
            ew("act", lambda e: e.activation(out=rj[0:nc_, 8:520], in_=ps_[0:nc_, :], func=AF.Copy), [pk], [f"raw{j}"])
            ew("dve", lambda e: e.tensor_scalar(out=A["tmp"][0:nc_, :], in0=rj[0:nc_, 7:519], scalar1=mus[0:nc_, j:j + 1],
                                                scalar2=None, op0=ALU.mult), [f"raw{j}", "mus"], ["tmp"])
            ew("dve", lambda e: e.scalar_tensor_tensor(out=A[shifted[j]][0:nc_, :], in0=rj[0:nc_, 8:520],
                                                       scalar=mus[0:nc_, 6 + j:7 + j], in1=A["tmp"][0:nc_, :],
                                                       op0=ALU.mult, op1=ALU.add), [f"raw{j}", "mus", "tmp"], [shifted[j]])
            ew("act", lambda e: e.activation(out=rj[0:nc_, 7:8], in_=rj[0:nc_, 519:520], func=AF.Copy), [f"raw{j}"], [f"raw{j}"])
        if stage < 2:
            p.dma('sp', out[:, tt * 512:(tt + 1) * 512], A['rs'][:, :], reads=['rs'], writes=['out'], sem='out')
            continue
        ew("act", lambda e: e.activation(out=wab[0:64, :], in_=A["was"][0:64, :], func=AF.Tanh), ["was"], ["wab"])
        ew("act", lambda e: e.activation(out=wab[64:128, :], in_=A["was"][64:128, :], func=AF.Copy), ["was"], ["wab"])
        ew("act", lambda e: e.activation(out=sg1[:, :], in_=A["g1s"][:, :], func=AF.Sigmoid), ["g1s"], ["sg1"])
        ew("act", lambda e: e.activation(out=sg2[0:32, :], in_=A["g2s"][0:32, :], func=AF.Sigmoid), ["g2s"], ["sg2"])
        ew("pe", lambda e: e.matmul(wk[0][:, :], lws[0:64, 0:128], wab[0:64, :], start=True, stop=True), ["lws", "wab"], ["wk0"])
        ew("pe", lambda e: e.matmul(wk[1][:, :], lws[64:128, 0:128], wab[64:128, :], start=True, stop=True), ["lws", "wab"], ["wk1"])
        ew("act", lambda e: e.activation(out=A["tmp"][:, :], in_=wk[0][:, :], func=AF.Sigmoid, bias=vs_[:, 0:1]), ["wk0", "vecs"], ["tmp"])
        ew("act", lambda e: e.activation(out=A["wT"][:, :], in_=A["tmp"][:, :], func=AF.Exp, scale=-math.exp(-0.5)), ["tmp"], ["wT"])
        ew("act", lambda e: e.activation(out=A["aT"][:, :], in_=wk[1][:, :], func=AF.Sigmoid, bias=vs_[:, 1:2]), ["wk1", "vecs"], ["aT"])
        ew("pe", lambda e: e.matmul(wk[0][:, :], lws[:, 128:256], sg1[:, :], start=True, stop=False), ["lws", "sg1"], ["wk0"])
        ew("pe", lambda e: e.matmul(wk[0][:, :], lws[0:32, 256:384], sg2[0:32, :], start=False, stop=True), ["lws", "sg2"], ["wk0"])
        ew("act", lambda e: e.activation(out=A["gT"][:, :], in_=wk[0][:, :], func=AF.Copy), ["wk0"], ["gT"])
        if stage < 3:
            p.dma('sp', out[:, tt * 512:(tt + 1) * 512], A['gT'][:, :], reads=['gT', 'wT', 'aT'], writes=['out'], sem='out')
            continue
        ew("dve", lambda e: e.tensor_scalar(out=A["kkT"][:, :], in0=A["ks"][:, :], scalar1=vs_[:, 2:3], scalar2=None, op0=ALU.mult), ["ks", "vecs"], ["kkT"])
        ew("act", lambda e: e.activation(out=A["sq"][:, :], in_=A["kkT"][:, :], func=AF.Square), ["kkT"], ["sq"])
        ew("pe", lambda e: e.matmul(wk[1][:, :], bones, A["sq"][:, :], start=True, stop=True), ["cs", "sq"], ["wk1"])
        ew("act", lambda e: e.activation(out=A["sq"][:, :], in_=wk[1][:, :], func=AF.Sqrt), ["wk1"], ["sq"])
        ew("dve", lambda e: e.tensor_scalar(out=A["sq"][:, :], in0=A["sq"][:, :], scalar1=1e-12, scalar2=None, op0=ALU.max), ["sq"], ["sq"])
        ew("dve", lambda e: e.reciprocal(out=A["sq"][:, :], in_=A["sq"][:, :]), ["sq"], ["sq"])
        ew("dve", lambda e: e.tensor_mul(out=A["kkT"][:, :], in0=A["kkT"][:, :], in1=A["sq"][:, :]), ["kkT", "sq"], ["kkT"])
        ew("dve", lambda e: e.tensor_mul(out=A["kka"][:, :], in0=A["kkT"][:, :], in1=A["aT"][:, :]), ["kkT", "aT"], ["kka"])
        ew("dve", lambda e: e.tensor_scalar(out=A["nkk"][:, :], in0=A["kkT"][:, :], scalar1=-1.0, scalar2=None, op0=ALU.mult), ["kkT"], ["nkk"])
        ew("dve", lambda e: e.tensor_scalar(out=A["tmp"][:, :], in0=A["aT"][:, :], scalar1=-1.0, scalar2=vs_[:, 3:4], op0=ALU.add, op1=ALU.mult), ["aT", "vecs"], ["tmp"])
        ew("dve", lambda e: e.scalar_tensor_tensor(out=A["kmod"][:, :], in0=A["tmp"][:, :], scalar=1.0, in1=A["ks"][:, :], op0=ALU.add, op1=ALU.mult), ["tmp", "ks"], ["kmod"])
        ew("dve", lambda e: e.scalar_tensor_tensor(out=A["sq"][:, :], in0=A["rs"][:, :], scalar=vs_[:, 4:5], in1=A["kmod"][:, :], op0=ALU.mult, op1=ALU.mult), ["rs", "kmod", "vecs"], ["sq"])
        ew("pe", lambda e: e.matmul(wk[1][:, :], bones, A["sq"][:, :], start=True, stop=True), ["cs", "sq"], ["wk1"])
        ew("dve", lambda e: e.tensor_mul(out=A["bon"][:, :], in0=wk[1][:, :], in1=A["vs"][:, :]), ["wk1", "vs"], ["bon"])
        if stage < 4:
            p.dma('sp', out[:, tt * 512:(tt + 1) * 512], A['bon'][:, :], reads=['bon', 'nkk', 'kka'], writes=['out'], sem='out')
            continue
        quants = ["nkk", "wT", "kka", "kmod", "rs"]
        for g in range(128):
            b = g % 2
            t0 = g * 4
            for qi, qn in enumerate(quants):
                ew("pool", lambda e: e.tensor_tensor(
                    out=Dq[b][qi][:, :, :], in0=A[qn][:, t0:t0 + 4].unsqueeze(2).to_broadcast([128, 4, 64]),
                    in1=ident2.unsqueeze(1).to_broadcast([128, 4, 64]), op=ALU.mult), [qn, "cs"], [f"Dq{b}{qi}"], ns=True)
                bank = br[b][qi // 2]
                off = (qi % 2) * 256
                ew("pe", lambda e: e.matmul(bank[:, off:off + 256], bones, Dq[b][qi][:, :, :].rearrange("p a b -> p (a b)"),
                                            start=True, stop=True), [f"Dq{b}{qi}", "cs"], [f"br{b}"], ns=True)

            def bq(qi, j):
                bank = br[b][qi // 2]
                off = (qi % 2) * 256 + j * 64
                return bank[:, off:off + 64]
            if stage == 5:
                ew('dve', lambda e: e.tensor_copy(out=A['oT'][:, t0 * 1:t0 + 4], in_=br[b][2][:, 0:4]), [f'br{b}'], ['oT'], ns=True)
                continue
            for j in range(4):
                t = t0 + j
                if stage == 6 and j > 0:
                    continue
                ew("dve", lambda e: e.scalar_tensor_tensor(out=junk[:, :], in0=bq(0, j), scalar=1.0, in1=S[:, :], op0=ALU.mult,
                                                           op1=ALU.mult, accum_out=sa[:, 0:1]), ["S", f"br{b}"], ["junk", "sa"], ns=True)
                ew("dve", lambda e: e.tensor_tensor(out=S1[:, :], in0=bq(1, j), in1=S[:, :], op=ALU.mult), ["S", f"br{b}"], ["S1"], ns=True)
                ew("dve", lambda e: e.scalar_tensor_tensor(out=S1[:, :], in0=bq(2, j), scalar=sa[:, 0:1], in1=S1[:, :], op0=ALU.mult,
                                                           op1=ALU.add), ["S1", "sa", f"br{b}"], ["S1"], ns=True)
                ew("dve", lambda e: e.scalar_tensor_tensor(out=S[:, :], in0=bq(3, j), scalar=A["vs"][:, t:t + 1], in1=S1[:, :],
                                                           op0=ALU.mult, op1=ALU.add), ["S1", "vs", f"br{b}"], ["S"], ns=True)
                ew("dve", lambda e: e.scalar_tensor_tensor(out=junk[:, :], in0=bq(4, j), scalar=1.0, in1=S[:, :], op0=ALU.mult,
                                                           op1=ALU.mult, accum_out=A["oT"][:, t:t + 1]), ["S", f"br{b}"], ["junk", "oT"], ns=True)
        ew("pe", lambda e: e.matmul(wk[0][:, :], bones, A["oT"][:, :], start=True, stop=True), ["cs", "oT"], ["wk0"])
        ew("dve", lambda e: e.scalar_tensor_tensor(out=A["cen"][:, :], in0=wk[0][:, :], scalar=-1.0 / 64, in1=A["oT"][:, :], op0=ALU.mult, op1=ALU.add), ["wk0", "oT"], ["cen"])
        ew("act", lambda e: e.activation(out=A["sq"][:, :], in_=A["cen"][:, :], func=AF.Square), ["cen"], ["sq"])
        ew("pe", lambda e: e.matmul(wk[1][:, :], bones, A["sq"][:, :], start=True, stop=True), ["cs", "sq"], ["wk1"])
        ew("dve", lambda e: e.tensor_scalar(out=A["sq"][:, :], in0=wk[1][:, :], scalar1=1.0 / 64, scalar2=64e-5, op0=ALU.mult, op1=ALU.add), ["wk1"], ["sq"])
        ew("act", lambda e: e.activation(out=A["sq"][:, :], in_=A["sq"][:, :], func=AF.Sqrt), ["sq"], ["sq"])
        ew("dve", lambda e: e.reciprocal(out=A["sq"][:, :], in_=A["sq"][:, :]), ["sq"], ["sq"])
        ew("dve", lambda e: e.tensor_mul(out=A["cen"][:, :], in0=A["cen"][:, :], in1=A["sq"][:, :]), ["cen", "sq"], ["cen"])
        ew("dve", lambda e: e.tensor_scalar(out=A["cen"][:, :], in0=A["cen"][:, :], scalar1=vs_[:, 5:6], scalar2=vs_[:, 6:7], op0=ALU.mult, op1=ALU.add), ["cen", "vecs"], ["cen"])
        ew("dve", lambda e: e.tensor_add(out=A["cen"][:, :], in0=A["cen"][:, :], in1=A["bon"][:, :]), ["cen", "bon"], ["cen"])
        obk = f"ob{tt % 2}"
        ew("dve", lambda e: e.tensor_mul(out=ob[tt % 2][:, :], in0=A["cen"][:, :], in1=A["gT"][:, :]), ["cen", "gT"], [obk])
        p.dma("sp", out[:, tt * 512:(tt + 1) * 512], ob[tt % 2][:, :], reads=[obk], writes=["out"], sem="out")
    p.finish(["out"])
    return p


def build_diff(T=T_FULL):
    p = Prog()
    xT = p.din("xT", [D, T])
    w = p.din("w_df", [D, 384])
    lamv = p.din("df_lamv", [128, 256])
    lc = p.din("df_const", [128, 2])
    gv = p.din("df_g", [128, 128])
    mk = p.din("df_mask", [128, 2048])
    out = p.dout("o_df", [128, T])
    ident, ones = _consts(p)
    NT = T // 512
    NB = T // 128
    w_sb = p.sb("w_sb", [128, KC, 384], BF16)
    load_w_cols(p, w, w_sb, "w_sb", 384)
    xt = [p.sb(f"xt{i}", [128, KC, 512], BF16) for i in range(2)]
    lam = p.sb("lam", [128, 264], F32)
    cst = p.sb("cst", [128, 4], F32)
    g_sb = p.sb("g_sb", [128, 128], F32)
    mask = p.sb("mask", [128, 2048], BF16)
    p.dma("sp", lam[:, 0:256], lamv, writes=["lam"], sem="c0")
    p.dma("sp", cst[:, 0:2], lc, writes=["cst"], sem="c1")
    p.dma("sp", g_sb[:, :], gv, writes=["g_sb"], sem="c2")
    p.dma("pool", mask[:, :], mk, writes=["mask"], sem="c3")
    p.op("dve", lambda e: e.tensor_mul(out=lam[:, 0:64], in0=lam[:, 0:64], in1=lam[:, 64:128]), reads=["lam"], writes=["lam"])
    p.op("dve", lambda e: e.tensor_mul(out=lam[:, 128:192], in0=lam[:, 128:192], in1=lam[:, 192:256]), reads=["lam"], writes=["lam"])
    p.op("dve", lambda e: e.reduce_sum(out=lam[:, 256:257], in_=lam[:, 0:64], axis=AX.X), reads=["lam"], writes=["lam"])
    p.op("dve", lambda e: e.reduce_sum(out=lam[:, 257:258], in_=lam[:, 128:192], axis=AX.X), reads=["lam"], writes=["lam"])
    p.op("act", lambda e: e.activation(out=lam[:, 256:258], in_=lam[:, 256:258], func=AF.Exp), reads=["lam"], writes=["lam"])
    p.op("dve", lambda e: e.tensor_sub(out=cst[:, 2:3], in0=lam[:, 257:258], in1=lam[:, 256:257]), reads=["lam", "cst"], writes=["cst"])
    p.op("dve", lambda e: e.tensor_sub(out=cst[:, 2:3], in0=cst[:, 2:3], in1=cst[:, 0:1]), reads=["cst"], writes=["cst"])
    p.op("dve", lambda e: e.tensor_scalar(out=g_sb[:, :], in0=g_sb[:, :], scalar1=cst[:, 1:2], scalar2=None, op0=ALU.mult),
         reads=["g_sb", "cst"], writes=["g_sb"])
    KT = [p.sb("KT0", [128, T], BF16), p.sb("KT1", [128, T], BF16)]
    p.op("pool", lambda e: e.memset(KT[0][64:128, :], 0.0), writes=["KT"])
    p.op("pool", lambda e: e.memset(KT[1][0:64, :], 0.0), writes=["KT"])
    QT = p.sb("QT", [128, 512], BF16)
    V = p.sb("V", [128, NB, 132], BF16)
    p.op("pool", lambda e: e.memset(V[:, :, 128:132], 1.0), writes=["V"])
    wk = [p.ps("wk0"), p.ps("wk1")]
    sc = [p.ps("sc0"), p.ps("sc1")]
    acc = [p.ps(f"acc{i}") for i in range(4)]
    PT = [p.sb(f"PT{i}", [128, 512], BF16) for i in range(4)]
    scb = [(sc[0], "sc0"), (sc[1], "sc1"), (wk[0], "wk0"), (wk[1], "wk1")]
    om = [p.sb(f"om{i}", [128, 4, 128], F32) for i in range(2)]
    rc = p.sb("rc", [128, 8], F32)
    junk = p.sb("junk", [128, 128], F32)
    otm = p.sb("otm", [128, 128], F32)
    ob = [p.sb(f"ob{i}", [128, 512], F32) for i in range(2)]
    load_xt_tile(p, xT, xt[0], "xt0", 0)
    it = 0
    for tt in range(NT):
        xk = f"xt{tt % 2}"
        xs = xt[tt % 2]
        if tt + 1 < NT:
            load_xt_tile(p, xT, xt[(tt + 1) % 2], f"xt{(tt + 1) % 2}", tt + 1)
        inproj_fm(p, wk[0], "wk0", w_sb, "w_sb", 0, 128, xs, xk)
        p.op("act", lambda e: e.activation(out=QT[:, :], in_=wk[0][:, :], func=AF.Copy), reads=["wk0"], writes=["QT"])
        inproj_fm(p, wk[1], "wk1", w_sb, "w_sb", 128, 128, xs, xk)
        p.op("act", lambda e: e.activation(out=KT[0][0:64, tt * 512:(tt + 1) * 512], in_=wk[1][0:64, :], func=AF.Copy), reads=["wk1"], writes=["KT"])
        p.op("act", lambda e: e.activation(out=KT[1][64:128, tt * 512:(tt + 1) * 512], in_=wk[1][64:128, :], func=AF.Copy), reads=["wk1"], writes=["KT"])
        for blk in range(4):
            for kc in range(KC):
                p.op("pe", lambda e: e.matmul(wk[0][:, blk * 128:(blk + 1) * 128], xs[:, kc, blk * 128:(blk + 1) * 128],
                                              w_sb[:, kc, 256:384], start=(kc == 0), stop=(kc == KC - 1)),
                     reads=["w_sb", xk], writes=["wk0"])
        p.op("act", lambda e: e.activation(out=V[:, tt * 4:tt * 4 + 4, 0:128],
                                           in_=wk[0][:, :].rearrange("p (a b) -> p a b", a=4), func=AF.Copy),
             reads=["wk0"], writes=["V"])
        for m in range(2):
            pr = slice(m * 64, (m + 1) * 64)
            nkb = 4 * tt + 4
            for kb in range(nkb):
                b = it % 4
                it += 1
                scp, sck = scb[b]
                p.op("pe", lambda e: e.matmul(scp[:, :], KT[m][:, kb * 128:(kb + 1) * 128], QT[:, :], start=True, stop=True),
                     reads=["KT", "QT"], writes=[sck])
                p.op("act", lambda e: e.activation(out=PT[b][:, :], in_=scp[:, :], func=AF.Exp, scale=0.125),
                     reads=[sck], writes=[f"PT{b}"])
                j = kb - 4 * tt
                if j >= 0:
                    p.op("dve", lambda e: e.tensor_mul(out=PT[b][:, :], in0=PT[b][:, :], in1=mask[:, j * 512:(j + 1) * 512]),
                         reads=[f"PT{b}", "mask"], writes=[f"PT{b}"])
                for qb in range(4):
                    if j > qb:
                        continue
                    p.op("pe", lambda e: e.matmul(acc[qb][:, 0:129], PT[b][:, qb * 128:(qb + 1) * 128], V[:, kb, 0:129],
                                                  start=(kb == 0), stop=(kb == 4 * tt + qb)),
                         reads=[f"PT{b}", "V"], writes=[f"acc{qb}"])
            for qb in range(4):
                p.op("dve", lambda e: e.reciprocal(out=rc[:, qb:qb + 1], in_=acc[qb][:, 128:129]), reads=[f"acc{qb}"], writes=["rc"])
                p.op("dve", lambda e: e.tensor_scalar(out=om[m][:, qb, :], in0=acc[qb][:, 0:128], scalar1=rc[:, qb:qb + 1],
                                                      scalar2=None, op0=ALU.mult), reads=[f"acc{qb}", "rc"], writes=[f"om{m}"])
        obk = f"ob{tt % 2}"
        for qb in range(4):
            p.op("dve", lambda e: e.scalar_tensor_tensor(out=otm[:, :], in0=om[1][:, qb, :], scalar=cst[:, 2:3], in1=om[0][:, qb, :],
                                                         op0=ALU.mult, op1=ALU.add), reads=["om0", "om1", "cst"], writes=["otm"])
            p.op("dve", lambda e: e.tensor_mul(out=junk[:, :], in0=otm[:, :], in1=otm[:, :]), reads=["otm"], writes=["junk"])
            p.op("dve", lambda e: e.reduce_sum(out=rc[:, 4:5], in_=junk[:, :], axis=AX.X), reads=["junk"], writes=["rc"])
            p.op("dve", lambda e: e.tensor_scalar(out=rc[:, 4:5], in0=rc[:, 4:5], scalar1=1.0 / 128, scalar2=1e-5, op0=ALU.mult,
                                                  op1=ALU.add), reads=["rc"], writes=["rc"])
            p.op("act", lambda e: e.activation(out=rc[:, 4:5], in_=rc[:, 4:5], func=AF.Sqrt), reads=["rc"], writes=["rc"])
            p.op("dve", lambda e: e.reciprocal(out=rc[:, 4:5], in_=rc[:, 4:5]), reads=["rc"], writes=["rc"])
            p.op("dve", lambda e: e.scalar_tensor_tensor(out=otm[:, :], in0=otm[:, :], scalar=rc[:, 4:5], in1=g_sb[:, :],
                                                         op0=ALU.mult, op1=ALU.mult), reads=["otm", "rc", "g_sb"], writes=["otm"])
            p.op("pe", lambda e: e.transpose(wk[1][:, qb * 128:(qb + 1) * 128], otm[:, :], ident[:, :]), reads=["otm", "ident"], writes=["wk1"])
        p.op("act", lambda e: e.activation(out=ob[tt % 2][:, :], in_=wk[1][:, :], func=AF.Copy), reads=["wk1"], writes=[obk])
        p.dma("sp", out[:, tt * 512:(tt + 1) * 512], ob[tt % 2][:, :], reads=[obk], writes=["out"], sem="out")
    p.finish(["out"])
    return p


def stream_w(p, st, src_ap, nchunk=32):
    i = st["i"] % len(st["bufs"])
    st["i"] += 1
    buf, key = st["bufs"][i], st["keys"][i]
    p.dma("pool", buf[:, 0:nchunk, :], src_ap, writes=[key], sem=key)
    return buf, key


def ln_finish(p, s1, s2, mean, rstd, sq, eps, n=512):
    p.op("dve", lambda e: e.tensor_scalar(out=mean[:, 0:n], in0=s1[:, 0:n], scalar1=1.0 / D, scalar2=None, op0=ALU.mult), reads=["s1"], writes=["mean"])
    p.op("dve", lambda e: e.tensor_mul(out=sq[:, 0:n], in0=mean[:, 0:n], in1=mean[:, 0:n]), reads=["mean"], writes=["sq"])
    p.op("dve", lambda e: e.scalar_tensor_tensor(out=rstd[:, 0:n], in0=s2[:, 0:n], scalar=1.0 / D, in1=sq[:, 0:n], op0=ALU.mult, op1=ALU.subtract), reads=["s2", "sq"], writes=["rstd"])
    p.op("dve", lambda e: e.tensor_scalar(out=rstd[:, 0:n], in0=rstd[:, 0:n], scalar1=eps, scalar2=None, op0=ALU.add), reads=["rstd"], writes=["rstd"])
    p.op("act", lambda e: e.activation(out=rstd[:, 0:n], in_=rstd[:, 0:n], func=AF.Sqrt), reads=["rstd"], writes=["rstd"])
    p.op("dve", lambda e: e.reciprocal(out=rstd[:, 0:n], in_=rstd[:, 0:n]), reads=["rstd"], writes=["rstd"])


def build_b1(NTOK=1024):
    p = Prog()
    xT = p.din("xT", [D, NTOK])
    oT = p.din("oT", [3072, NTOK])
    memT = p.din("memT", [D, 256])
    mln = p.din("mem_ln", [128, 64])
    w_xg = p.din("w_xg", [D, 1280])
    w_kv = p.din("w_kv", [D, 2048])
    w_br = p.din("w_br", [4096, D])
    w_gu = p.din("w_gu", [256, 16384])
    bg = p.din("b_gate", [128, 128])
    w_o = p.din("w_o", [D, D])
    ln1 = p.din("ln1", [128, 64])
    x1T = p.dout("x1T", [D, NTOK])
    NT = NTOK // 512
    ones = p.sb("ones", [128, 128], F32)
    onesb = p.sb("onesb", [128, 128], BF16)
    p.op("pool", lambda e: e.memset(ones[:, :], 1.0), writes=["ones"])
    p.op("pool", lambda e: e.memset(onesb[:, :], 1.0), writes=["onesb"])
    mlns = p.sb("mlns", [128, 64], F32)
    ln1s = p.sb("ln1s", [128, 64], F32)
    bgs = p.sb("bgs", [128, 128], F32)
    p.dma("sp", mlns[:, :], mln, writes=["mlns"], sem="c0")
    p.dma("sp", ln1s[:, :], ln1, writes=["ln1s"], sem="c1")
    p.dma("sp", bgs[:, :], bg, writes=["bgs"], sem="c2")
    st = dict(i=0, bufs=[p.sb(f"wst{i}", [128, 32, 128], BF16) for i in range(6)], keys=[f"wst{i}" for i in range(6)])
    R1 = p.sb("R1", [128, 32, 512], BF16)
    obT = p.sb("obT", [128, 24, 512], BF16)
    memn = p.sb("memn", [128, 32, 256], BF16)
    mkT = p.sb("mkT", [128, 8, 256], BF16)
    mv = p.sb("mv", [128, 2, 1024], BF16)
    xqT = p.sb("xqT", [128, 8, 512], BF16)
    gdT = p.sb("gdT", [128, 2, 512], BF16)
    oxa = p.sb("oxa", [128, 8, 512], BF16)
    PT = [p.sb(f"PT{i}", [128, 512], BF16) for i in range(2)]
    fa = {n: p.sb("f_" + n, [128, 512], F32) for n in ["sq", "mean", "rstd", "gs", "macc", "tmp", "rden", "h0", "h1", "x0", "x1", "m0", "m1"]}
    wk = [p.ps(f"wk{i}") for i in range(6)]
    s1, s2 = p.ps("s1"), p.ps("s2")
    memv = memT.rearrange("(kc p) m -> p kc m", p=128)
    for ps_ in range(2):
        for kc in range(KC):
            mk_ = f"m{kc % 2}"
            p.dma("sp", fa[mk_][:, 0:256], memv[:, kc, :], writes=[mk_], sem=mk_)
            if ps_ == 0:
                p.op("act", lambda e: e.activation(out=fa["sq"][:, 0:256], in_=fa[mk_][:, 0:256], func=AF.Square), reads=[mk_], writes=["sq"])
                p.op("pe", lambda e: e.matmul(s1[:, 0:256], ones[:, :], fa[mk_][:, 0:256], start=(kc == 0), stop=(kc == KC - 1)), reads=[mk_, "ones"], writes=["s1"])
                p.op("pe", lambda e: e.matmul(s2[:, 0:256], ones[:, :], fa["sq"][:, 0:256], start=(kc == 0), stop=(kc == KC - 1)), reads=["sq", "ones"], writes=["s2"])
            else:
                p.op("dve", lambda e: e.tensor_sub(out=fa["tmp"][:, 0:256], in0=fa[mk_][:, 0:256], in1=fa["mean"][:, 0:256]), reads=[mk_, "mean"], writes=["tmp"])
                p.op("dve", lambda e: e.tensor_mul(out=fa["tmp"][:, 0:256], in0=fa["tmp"][:, 0:256], in1=fa["rstd"][:, 0:256]), reads=["tmp", "rstd"], writes=["tmp"])
                p.op("dve", lambda e: e.tensor_scalar(out=memn[:, kc, :], in0=fa["tmp"][:, 0:256], scalar1=mlns[:, kc:kc + 1], scalar2=mlns[:, 32 + kc:33 + kc], op0=ALU.mult, op1=ALU.add), reads=["tmp", "mlns"], writes=["memn"])
        if ps_ == 0:
            ln_finish(p, s1, s2, fa["mean"], fa["rstd"], fa["sq"], 1e-5, n=256)
    kvv = w_kv.rearrange("(kc p) n -> p kc n", p=128)
    for blk in range(8):
        wb, wkey = stream_w(p, st, kvv[:, :, blk * 128:(blk + 1) * 128])
        ps_ = wk[blk % 2]
        for kc in range(KC):
            p.op("pe", lambda e: e.matmul(ps_[:, 0:256], wb[:, kc, :], memn[:, kc, :], start=(kc == 0), stop=(kc == KC - 1)), reads=[wkey, "memn"], writes=[f"wk{blk % 2}"])
        p.op("act", lambda e: e.activation(out=mkT[:, blk, :], in_=ps_[:, 0:256], func=AF.Copy), reads=[f"wk{blk % 2}"], writes=["mkT"])
    for ct in range(8):
        wb, wkey = stream_w(p, st, kvv[:, :, 1024 + ct * 128:1024 + (ct + 1) * 128])
        for mb in range(2):
            ps_ = wk[mb]
            for kc in range(KC):
                p.op("pe", lambda e: e.matmul(ps_[:, 0:128], memn[:, kc, mb * 128:(mb + 1) * 128], wb[:, kc, :], start=(kc == 0), stop=(kc == KC - 1)), reads=[wkey, "memn"], writes=[f"wk{mb}"])
            p.op("act", lambda e: e.activation(out=mv[:, mb, ct * 128:(ct + 1) * 128], in_=ps_[:, 0:128], func=AF.Copy), reads=[f"wk{mb}"], writes=["mv"])
    xgv = w_xg.rearrange("(kc p) n -> p kc n", p=128)
    brv = w_br.rearrange("(c p) f -> p c f", p=128)
    guv = w_gu.rearrange("(rc p) f -> p rc f", p=128)
    wov = w_o.rearrange("(kc p) f -> p kc f", p=128)
    xTv = xT.rearrange("(kc p) t -> p kc t", p=128)
    oTv = oT.rearrange("(c p) t -> p c t", p=128)
    wg = [p.sb(f"wg{i}", [128, 4, 2, 128], BF16) for i in range(2)]
    for tt in range(NT):
        ts_ = slice(tt * 512, (tt + 1) * 512)
        for h in range(2):
            p.dma("pool", R1[:, h * 16:(h + 1) * 16, :], xTv[:, h * 16:(h + 1) * 16, ts_], writes=["R1"], sem="R1")
        for h in range(3):
            p.dma("pool", obT[:, h * 8:(h + 1) * 8, :], oTv[:, h * 8:(h + 1) * 8, ts_], writes=["obT"], sem="obT")
        for blk in range(10):
            wb, wkey = stream_w(p, st, xgv[:, :, blk * 128:(blk + 1) * 128])
            ps_, pk = wk[blk % 2], f"wk{blk % 2}"
            for kc in range(KC):
                p.op("pe", lambda e: e.matmul(ps_[:, :], wb[:, kc, :], R1[:, kc, :], start=(kc == 0), stop=(kc == KC - 1)), reads=[wkey, "R1"], writes=[pk])
            dst = xqT[:, blk, :] if blk < 8 else gdT[:, blk - 8, :]
            p.op("act", lambda e: e.activation(out=dst, in_=ps_[:, :], func=AF.Copy), reads=[pk], writes=["xqT" if blk < 8 else "gdT"])
        for h in range(4):
            for mb in range(2):
                for dc in range(2):
                    p.op("pe", lambda e: e.matmul(wk[mb][:, :], mkT[:, 2 * h + dc, mb * 128:(mb + 1) * 128], xqT[:, 2 * h + dc, :], start=(dc == 0), stop=(dc == 1)), reads=["mkT", "xqT"], writes=[f"wk{mb}"])
                p.op("act", lambda e: e.activation(out=PT[mb][:, :], in_=wk[mb][:, :], func=AF.Exp, scale=1.0 / 16), reads=[f"wk{mb}"], writes=[f"PT{mb}"])
            for mb in range(2):
                p.op("pe", lambda e: e.matmul(wk[2][:, :], onesb[:, :], PT[mb][:, :], start=(mb == 0), stop=(mb == 1)), reads=["onesb", f"PT{mb}"], writes=["wk2"])
            p.op("dve", lambda e: e.reciprocal(out=fa["rden"][:, :], in_=wk[2][:, :]), reads=["wk2"], writes=["rden"])
            for dvb in range(2):
                for mb in range(2):
                    p.op("pe", lambda e: e.matmul(wk[3 + dvb][:, :], mv[:, mb, h * 256 + dvb * 128:h * 256 + (dvb + 1) * 128], PT[mb][:, :], start=(mb == 0), stop=(mb == 1)), reads=["mv", f"PT{mb}"], writes=[f"wk{3 + dvb}"])
                p.op("dve", lambda e: e.tensor_mul(out=oxa[:, 2 * h + dvb, :], in0=wk[3 + dvb][:, :], in1=fa["rden"][:, :]), reads=[f"wk{3 + dvb}", "rden"], writes=["oxa"])
        for fb in range(32):
            wb, wkey = stream_w(p, st, brv[:, :, fb * 128:(fb + 1) * 128])
            wgb, wgk = wg[fb % 2], f"wg{fb % 2}"
            for n in range(4):
                p.dma("pool", wgb[:, n, :, :], guv[:, :, n * 4096 + fb * 128:n * 4096 + (fb + 1) * 128], writes=[wgk], sem=wgk)
            for n in range(4):
                pb, pbk = wk[n % 2], f"wk{n % 2}"
                pg, pgk = wk[2 + n % 2], f"wk{2 + n % 2}"
                for kc in range(8):
                    rhs = obT[:, n * 8 + kc, :] if n < 3 else oxa[:, kc, :]
                    p.op("pe", lambda e: e.matmul(pb[:, :], wb[:, n * 8 + kc, :], rhs, start=(kc == 0), stop=(kc == 7)), reads=[wkey, "obT" if n < 3 else "oxa"], writes=[pbk])
                for rc in range(2):
                    p.op("pe", lambda e: e.matmul(pg[:, :], wgb[:, n, rc, :], gdT[:, rc, :], start=(rc == 0), stop=(rc == 1)), reads=[wgk, "gdT"], writes=[pgk])
                p.op("act", lambda e: e.activation(out=fa["gs"][:, :], in_=pg[:, :], func=AF.Sigmoid, bias=bgs[:, n * 32 + fb:n * 32 + fb + 1]), reads=[pgk, "bgs"], writes=["gs"])
                if n == 0:
                    p.op("dve", lambda e: e.tensor_mul(out=fa["macc"][:, :], in0=pb[:, :], in1=fa["gs"][:, :]), reads=[pbk, "gs"], writes=["macc"])
                else:
                    p.op("dve", lambda e: e.tensor_mul(out=fa["tmp"][:, :], in0=pb[:, :], in1=fa["gs"][:, :]), reads=[pbk, "gs"], writes=["tmp"])
                    dst = R1[:, fb, :] if n == 3 else fa["macc"][:, :]
                    p.op("dve", lambda e: e.tensor_add(out=dst, in0=fa["macc"][:, :], in1=fa["tmp"][:, :]), reads=["macc", "tmp"], writes=["R1" if n == 3 else "macc"])
        for fb in range(32):
            wb, wkey = stream_w(p, st, wov[:, :, fb * 128:(fb + 1) * 128])
            xk_, hk_ = f"x{fb % 2}", f"h{fb % 2}"
            p.dma("sp", fa[xk_][:, :], xT[fb * 128:(fb + 1) * 128, ts_], writes=[xk_], sem=xk_)
            py, pyk = wk[fb % 2], f"wk{fb % 2}"
            for kc in range(KC):
                p.op("pe", lambda e: e.matmul(py[:, :], wb[:, kc, :], R1[:, kc, :], start=(kc == 0), stop=(kc == KC - 1)), reads=[wkey, "R1"], writes=[pyk])
            p.op("dve", lambda e: e.scalar_tensor_tensor(out=fa[hk_][:, :], in0=fa[xk_][:, :], scalar=ALPHA, in1=py[:, :], op0=ALU.mult, op1=ALU.add), reads=[xk_, pyk], writes=[hk_])
            p.op("act", lambda e: e.activation(out=fa["sq"][:, :], in_=fa[hk_][:, :], func=AF.Square), reads=[hk_], writes=["sq"])
            p.op("pe", lambda e: e.matmul(s1[:, :], ones[:, :], fa[hk_][:, :], start=(fb == 0), stop=(fb == 31)), reads=[hk_, "ones"], writes=["s1"])
            p.op("pe", lambda e: e.matmul(s2[:, :], ones[:, :], fa["sq"][:, :], start=(fb == 0), stop=(fb == 31)), reads=["sq", "ones"], writes=["s2"])
            p.dma("sp", x1T[fb * 128:(fb + 1) * 128, ts_], fa[hk_][:, :], reads=[hk_], writes=["x1T"], sem="hout")
        ln_finish(p, s1, s2, fa["mean"], fa["rstd"], fa["sq"], 1e-5)
        for fb in range(32):
            hk_, xk_ = f"h{fb % 2}", f"x{fb % 2}"
            p.dma("sp", fa[hk_][:, :], x1T[fb * 128:(fb + 1) * 128, ts_], reads=["x1T"], writes=[hk_], sem=hk_)
            p.op("dve", lambda e: e.tensor_sub(out=fa["tmp"][:, :], in0=fa[hk_][:, :], in1=fa["mean"][:, :]), reads=[hk_, "mean"], writes=["tmp"])
            p.op("dve", lambda e: e.tensor_mul(out=fa["tmp"][:, :], in0=fa["tmp"][:, :], in1=fa["rstd"][:, :]), reads=["tmp", "rstd"], writes=["tmp"])
            p.op("dve", lambda e: e.tensor_scalar(out=fa[xk_][:, :], in0=fa["tmp"][:, :], scalar1=ln1s[:, fb:fb + 1], scalar2=ln1s[:, 32 + fb:33 + fb], op0=ALU.mult, op1=ALU.add), reads=["tmp", "ln1s"], writes=[xk_])
            p.dma("sp", x1T[fb * 128:(fb + 1) * 128, ts_], fa[xk_][:, :], reads=[xk_, "x1T"], writes=["x1T_f"], sem="xout")
    p.finish(["x1T_f"])
    return p


def build_b2(NTOK=1024):
    p = Prog()
    x1T = p.din("x1T", [D, NTOK])
    rw = p.din("router_w", [D, 32])
    rb = p.din("router_b", [32, 1])
    w1 = p.din("w1", [32 * D, 512])
    b1 = p.din("b1", [128, 128])
    w2 = p.din("w2", [32 * 256, D])
    b2 = p.din("b2", [32, D])
    ln2 = p.din("ln2", [128, 64])
    cid = p.din("c_ident", [128, 128])
    x2T = p.dout("x2T", [D, NTOK])
    NT = NTOK // 512
    ident = p.sb("ident", [128, 128], F32)
    ones = p.sb("ones", [128, 128], F32)
    p.dma("sp", ident[:, :], cid, writes=["ident"], sem="c0")
    p.op("pool", lambda e: e.memset(ones[:, :], 1.0), writes=["ones"])
    rws = p.sb("rws", [128, 32, 32], F32)
    rbs = p.sb("rbs", [32, 1], F32)
    b1s = p.sb("b1s", [128, 128], F32)
    ln2s = p.sb("ln2s", [128, 64], F32)
    p.dma("sp", rws[:, :, :], rw.rearrange("(kc p) e -> p kc e", p=128), writes=["rws"], sem="c1")
    p.dma("sp", rbs[:, :], rb, writes=["rbs"], sem="c2")
    p.dma("sp", b1s[:, :], b1, writes=["b1s"], sem="c3")
    p.dma("sp", ln2s[:, :], ln2, writes=["ln2s"], sem="c4")
    xb = p.sb("xb", [128, 32, 512], BF16)
    AT = p.sb("AT", [128, 64, 512], BF16)
    w1b = [p.sb(f"w1b{i}", [128, 8, 512], BF16) for i in range(3)]
    w2b = [p.sb(f"w2b{i}", [128, 64, 128], BF16) for i in range(2)]
    b2s = [p.sb(f"b2s{i}", [32, 128], F32) for i in range(2)]
    fa = {n: p.sb("f_" + n, [128, 512], F32) for n in ["sq", "mean", "rstd", "g", "sg", "l", "tmp", "h0", "h1", "x0", "x1", "lgT", "gT", "gm"]}
    lgt = p.sb("lgt", [128, 4, 32], F32)
    gt = p.sb("gt", [128, 4, 32], F32)
    m8 = p.sb("m8", [128, 8], F32)
    sm = p.sb("sm", [128, 4], F32)
    hp = [p.ps(f"hp{i}") for i in range(4)]
    wk = [p.ps("wk0"), p.ps("wk1")]
    s1, s2 = p.ps("s1"), p.ps("s2")
    w1v = w1.rearrange("(e kc p) f -> p e kc f", p=128, kc=32)
    w2v = w2.rearrange("(c p) d -> p c d", p=128)
    wi = 0
    for tt in range(NT):
        ts_ = slice(tt * 512, (tt + 1) * 512)
        for kc in range(KC):
            xk_ = f"x{kc % 2}"
            p.dma("sp", fa[xk_][:, :], x1T[kc * 128:(kc + 1) * 128, ts_], writes=[xk_], sem=xk_)
            p.op("pe", lambda e: e.matmul(wk[0][0:32, :], rws[:, kc, :], fa[xk_][:, :], start=(kc == 0), stop=(kc == KC - 1)), reads=["rws", xk_], writes=["wk0"])
            p.op("act", lambda e: e.activation(out=xb[:, kc, :], in_=fa[xk_][:, :], func=AF.Copy), reads=[xk_], writes=["xb"])
        p.op("dve", lambda e: e.tensor_scalar(out=fa["lgT"][0:32, :], in0=wk[0][0:32, :], scalar1=rbs[:, 0:1], scalar2=None, op0=ALU.add), reads=["wk0", "rbs"], writes=["lgT"])
        for q in range(4):
            p.op("pe", lambda e: e.transpose(wk[1][:, q * 32:(q + 1) * 32], fa["lgT"][0:32, q * 128:(q + 1) * 128], ident[0:32, 0:32]), reads=["lgT", "ident"], writes=["wk1"])
        p.op("dve", lambda e: e.tensor_copy(out=lgt[:, :, :], in_=wk[1][:, 0:128].rearrange("p (a b) -> p a b", a=4)), reads=["wk1"], writes=["lgt"])
        for q in range(4):
            p.op("dve", lambda e: e.max(out=m8[:, :], in_=lgt[:, q, :]), reads=["lgt"], writes=["m8"])
            p.op("dve", lambda e: e.tensor_scalar(out=sm[:, 0:1], in0=m8[:, 0:1], scalar1=-1.0, scalar2=None, op0=ALU.mult), reads=["m8"], writes=["sm"])
            p.op("act", lambda e: e.activation(out=gt[:, q, :], in_=lgt[:, q, :], func=AF.Exp, bias=sm[:, 0:1]), reads=["lgt", "sm"], writes=["gt"])
            p.op("dve", lambda e: e.scalar_tensor_tensor(out=gt[:, q, :], in0=lgt[:, q, :], scalar=m8[:, 3:4], in1=gt[:, q, :], op0=ALU.is_ge, op1=ALU.mult), reads=["lgt", "m8", "gt"], writes=["gt"])
            p.op("dve", lambda e: e.reduce_sum(out=sm[:, 1:2], in_=gt[:, q, :], axis=AX.X), reads=["gt"], writes=["sm"])
            p.op("dve", lambda e: e.reciprocal(out=sm[:, 1:2], in_=sm[:, 1:2]), reads=["sm"], writes=["sm"])
            p.op("dve", lambda e: e.tensor_scalar(out=gt[:, q, :], in0=gt[:, q, :], scalar1=sm[:, 1:2], scalar2=None, op0=ALU.mult), reads=["gt", "sm"], writes=["gt"])
            p.op("pe", lambda e: e.transpose(wk[0][0:32, q * 128:(q + 1) * 128], gt[:, q, :], ident[:, :]), reads=["gt", "ident"], writes=["wk0"])
        p.op("act", lambda e: e.activation(out=fa["gT"][0:32, :], in_=wk[0][0:32, :], func=AF.Copy), reads=["wk0"], writes=["gT"])
        for ex in range(32):
            for qd in range(4):
                wb, wkey = w1b[wi % 3], f"w1b{wi % 3}"
                wi += 1
                p.dma("pool", wb[:, :, :], w1v[:, ex, qd * 8:(qd + 1) * 8, :], writes=[wkey], sem=wkey)
                for k8 in range(8):
                    kc = qd * 8 + k8
                    for blk in range(4):
                        c0 = (blk % 2) * 256 + (blk // 2)
                        p.op("pe", lambda e: e.matmul(hp[blk][:, :], wb[:, k8, c0:min(c0 + 256, 512):2], xb[:, kc, :], start=(kc == 0), stop=(kc == KC - 1)), reads=[wkey, "xb"], writes=[f"hp{blk}"])
            p.op("dve", lambda e: e.tensor_scalar(out=fa["gm"][0:32, :], in0=fa["gT"][0:32, :], scalar1=ident[0:32, ex:ex + 1], scalar2=None, op0=ALU.mult), reads=["gT", "ident"], writes=["gm"])
            p.op("pe", lambda e: e.matmul(wk[1][:, :], ones[0:32, :], fa["gm"][0:32, :], start=True, stop=True), reads=["gm", "ones"], writes=["wk1"])
            for j in range(2):
                bc = ex * 4 + j
                p.op("dve", lambda e: e.tensor_scalar(out=fa["g"][:, :], in0=hp[j][:, :], scalar1=b1s[:, bc:bc + 1], scalar2=7.0, op0=ALU.add, op1=ALU.min), reads=[f"hp{j}", "b1s"], writes=["g"])
                p.op("act", lambda e: e.activation(out=fa["sg"][:, :], in_=fa["g"][:, :], func=AF.Sigmoid, scale=1.702), reads=["g"], writes=["sg"])
                p.op("dve", lambda e: e.tensor_scalar(out=fa["l"][:, :], in0=hp[2 + j][:, :], scalar1=b1s[:, bc + 2:bc + 3], scalar2=7.0, op0=ALU.add, op1=ALU.min), reads=[f"hp{2 + j}", "b1s"], writes=["l"])
                p.op("dve", lambda e: e.tensor_scalar(out=fa["l"][:, :], in0=fa["l"][:, :], scalar1=-7.0, scalar2=1.0, op0=ALU.max, op1=ALU.add), reads=["l"], writes=["l"])
                p.op("pool", lambda e: e.tensor_tensor(out=fa["g"][:, :], in0=fa["g"][:, :], in1=fa["sg"][:, :], op=ALU.mult), reads=["g", "sg"], writes=["g"])
                p.op("pool", lambda e: e.tensor_tensor(out=fa["g"][:, :], in0=fa["g"][:, :], in1=fa["l"][:, :], op=ALU.mult), reads=["g", "l"], writes=["g"])
                p.op("dve", lambda e: e.tensor_mul(out=AT[:, ex * 2 + j, :], in0=wk[1][:, :], in1=fa["g"][:, :]), reads=["wk1", "g"], writes=["AT"])
        for fb in range(32):
            wb, wkey = w2b[fb % 2], f"w2b{fb % 2}"
            for h in range(2):
                p.dma("pool", wb[:, h * 32:(h + 1) * 32, :], w2v[:, h * 32:(h + 1) * 32, fb * 128:(fb + 1) * 128], writes=[wkey], sem=wkey)
            bb, bk = b2s[fb % 2], f"b2s{fb % 2}"
            p.dma("sp", bb[:, :], b2[:, fb * 128:(fb + 1) * 128], writes=[bk], sem=bk)
            xk_, hk_ = f"x{fb % 2}", f"h{fb % 2}"
            p.dma("sp", fa[xk_][:, :], x1T[fb * 128:(fb + 1) * 128, ts_], writes=[xk_], sem=xk_)
            py, pyk = hp[fb % 2], f"hp{fb % 2}"
            for c in range(64):
                p.op("pe", lambda e: e.matmul(py[:, :], wb[:, c, :], AT[:, c, :], start=(c == 0), stop=(c == 63)), reads=[wkey, "AT"], writes=[pyk])
            pb, pbk = hp[2 + fb % 2], f"hp{2 + fb % 2}"
            p.op("pe", lambda e: e.matmul(pb[:, :], bb[:, :], fa["gT"][0:32, :], start=True, stop=True), reads=[bk, "gT"], writes=[pbk])
            p.op("dve", lambda e: e.scalar_tensor_tensor(out=fa["tmp"][:, :], in0=fa[xk_][:, :], scalar=ALPHA, in1=py[:, :], op0=ALU.mult, op1=ALU.add), reads=[xk_, pyk], writes=["tmp"])
            p.op("dve", lambda e: e.tensor_add(out=fa[hk_][:, :], in0=fa["tmp"][:, :], in1=pb[:, :]), reads=["tmp", pbk], writes=[hk_])
            p.op("act", lambda e: e.activation(out=fa["sq"][:, :], in_=fa[hk_][:, :], func=AF.Square), reads=[hk_], writes=["sq"])
            p.op("pe", lambda e: e.matmul(s1[:, :], ones[:, :], fa[hk_][:, :], start=(fb == 0), stop=(fb == 31)), reads=[hk_, "ones"], writes=["s1"])
            p.op("pe", lambda e: e.matmul(s2[:, :], ones[:, :], fa["sq"][:, :], start=(fb == 0), stop=(fb == 31)), reads=["sq", "ones"], writes=["s2"])
            p.dma("sp", x2T[fb * 128:(fb + 1) * 128, ts_], fa[hk_][:, :], reads=[hk_], writes=["x2T"], sem="hout")
        ln_finish(p, s1, s2, fa["mean"], fa["rstd"], fa["sq"], 1e-5)
        for fb in range(32):
            hk_, xk_ = f"h{fb % 2}", f"x{fb % 2}"
            p.dma("sp", fa[hk_][:, :], x2T[fb * 128:(fb + 1) * 128, ts_], reads=["x2T"], writes=[hk_], sem=hk_)
            p.op("dve", lambda e: e.tensor_sub(out=fa["tmp"][:, :], in0=fa[hk_][:, :], in1=fa["mean"][:, :]), reads=[hk_, "mean"], writes=["tmp"])
            p.op("dve", lambda e: e.tensor_mul(out=fa["tmp"][:, :], in0=fa["tmp"][:, :], in1=fa["rstd"][:, :]), reads=["tmp", "rstd"], writes=["tmp"])
            p.op("dve", lambda e: e.tensor_scalar(out=fa[xk_][:, :], in0=fa["tmp"][:, :], scalar1=ln2s[:, fb:fb + 1], scalar2=ln2s[:, 32 + fb:33 + fb], op0=ALU.mult, op1=ALU.add), reads=["tmp", "ln2s"], writes=[xk_])
            p.dma("sp", x2T[fb * 128:(fb + 1) * 128, ts_], fa[xk_][:, :], reads=[xk_, "x2T"], writes=["x2T_f"], sem="xout")
    p.finish(["x2T_f"])
    return p


def _chunk(v):
    return np.ascontiguousarray(np.asarray(v, np.float32).reshape(32, 128).T)


_PROGS = {}


def _prog(name, fn):
    return fn()


def _run(p, ins):
    return run_bass_kernel_spmd(p.nc, ins, core_ids=list(range(NCORE))).results


def kernel(**inputs):
    z = {k: np.asarray(v) for k, v in inputs.items()}
    x = np.ascontiguousarray(z["x"][0], dtype=np.float32)
    T = x.shape[0]
    ident = np.eye(128, dtype=np.float32)
    memT = np.ascontiguousarray(z["mem"][0].T)
    mem_ln = np.concatenate([_chunk(z["mem_ln_g"]), _chunk(z["mem_ln_b"])], axis=1)
    kk = np.arange(128)[:, None]
    qq = np.arange(512)[None, :]
    dmask = np.concatenate([(kk + 128 * j <= qq).astype(np.float32) for j in range(4)], axis=1)
    crw = np.zeros((128, 256), np.float32)
    crw[:, 0:64] = np.tile(np.eye(64, dtype=np.float32), (2, 1))
    crw[0:64, 64:128] = 1
    crw[64:128, 128:192] = 1
    RW0 = 7 * 1024
    for l in range(4):
        w_in = z["w_in"][l]
        xT = np.ascontiguousarray(x.T)
        ins_hg, ins_df, ins_rw = [], [], []
        li = 0.8 - 0.6 * math.exp(-0.3 * l)
        for c in range(NCORE):
            ch = np.arange(c * 128, (c + 1) * 128)
            lm = np.zeros((128, 4), np.float32)
            lm[:, 1:l + 1] = 1.0
            ins_hg.append({"xT": xT, "w_hg": np.ascontiguousarray(w_in[:, np.concatenate([j * 1024 + ch for j in range(4)])]),
                           "lb_raw": np.ascontiguousarray(z["hg_lb_raw"][:, ch].T), "lmask": lm,
                           "norm_g": np.ascontiguousarray(z["hg_norm_g"][l][:, None]), "c_ident": ident,
                           "c_tri": np.triu(np.ones((64, 64), np.float32))})
            lamv = np.tile(np.concatenate([z["df_lam_q1"][l], z["df_lam_k1"][l], z["df_lam_q2"][l], z["df_lam_k2"][l]])[None, :], (128, 1)).astype(np.float32)
            ins_df.append({"xT": xT, "w_df": np.ascontiguousarray(w_in[:, np.concatenate([4096 + ch, 5120 + ch, 6144 + ch])]),
                           "df_lamv": lamv, "df_const": np.tile(np.array([[li, 1 - li]], np.float32), (128, 1)),
                           "df_g": np.tile(z["df_subln_g"][l][None, :], (128, 1)).astype(np.float32), "df_mask": dmask, "c_ident": ident})
            cols = np.concatenate([RW0 + ch, RW0 + 1024 + ch, RW0 + 2048 + ch, RW0 + 3072 + np.arange(288)])
            mu_all = z["rw_shift_mu"][l]
            mu = np.zeros((128, 6), np.float32)
            mu[:, 0] = mu_all[ch]
            mu[:, 1] = mu_all[1024 + ch]
            mu[:, 2] = mu_all[2048 + ch]
            mu[:, 3] = mu_all[3072:3200]
            mu[:, 4] = mu_all[3200:3328]
            mu[:32, 5] = mu_all[3328:3360]
            lw = np.zeros((128, 512), np.float32)
            lw[0:64, 0:128] = z["rw_w2"][l][:, ch]
            lw[64:128, 0:128] = z["rw_a2"][l][:, ch]
            lw[:, 128:256] = z["rw_g2"][l][0:128][:, ch]
            lw[0:32, 256:384] = z["rw_g2"][l][128:160][:, ch]
            vec = np.zeros((128, 8), np.float32)
            for i_, n_ in enumerate(["rw_w0", "rw_a0", "rw_k_k", "rw_k_a"]):
                vec[:, i_] = z[n_][l][ch]
            vec[:, 4] = z["rw_r_k"][l].reshape(-1)[ch]
            vec[:, 5] = z["rw_lnx_g"][l][ch]
            vec[:, 6] = z["rw_lnx_b"][l][ch]
            ins_rw.append({"xT": xT, "w_rw": np.ascontiguousarray(w_in[:, cols]), "rw_mu": mu, "rw_lora": lw, "rw_vec": vec, "c_rw": crw})
        r_hg = _run(_prog("hg", lambda: build_hgrn2c(T)), ins_hg)
        r_df = _run(_prog("df", lambda: build_diff(T)), ins_df)
        r_rw = _run(_prog("rw", lambda: build_rwkv(T)), ins_rw)
        oT_all = np.concatenate([r_hg[c]["o_hg"] for c in range(NCORE)] + [r_df[c]["o_df"] for c in range(NCORE)]
                                + [r_rw[c]["o_rw"] for c in range(NCORE)], axis=0)
        del r_hg, r_df, r_rw, ins_hg, ins_df, ins_rw
        sh = {"memT": memT, "mem_ln": mem_ln, "w_xg": np.ascontiguousarray(w_in[:, 10528:11808]), "w_kv": z["w_mem_kv"][l],
              "w_br": z["w_br"][l].reshape(4096, 4096), "w_gu": z["w_gate_up"][l],
              "b_gate": np.ascontiguousarray(z["b_gate"][l].reshape(4, 32, 128).transpose(2, 0, 1).reshape(128, 128)),
              "w_o": z["w_o"][l], "ln1": np.concatenate([_chunk(z["ln1_g"][l]), _chunk(z["ln1_b"][l])], axis=1)}
        nt = T // NCORE
        ins = []
        for c in range(NCORE):
            d = dict(sh)
            d["xT"] = np.ascontiguousarray(xT[:, c * nt:(c + 1) * nt])
            d["oT"] = np.ascontiguousarray(oT_all[:, c * nt:(c + 1) * nt])
            ins.append(d)
        r1 = _run(_prog("b1", lambda: build_b1(nt)), ins)
        b1 = z["exp_b1"][l]
        b1l = np.stack([b1[:, 0:256:2], b1[:, 256:512:2], b1[:, 1:256:2], b1[:, 257:512:2]], axis=1)
        sh = {"router_w": z["router_w"][l], "router_b": np.ascontiguousarray(z["router_b"][l][:, None]),
              "w1": z["exp_w1"][l].reshape(32 * 4096, 512), "b1": np.ascontiguousarray(b1l.transpose(2, 0, 1).reshape(128, 128)),
              "w2": z["exp_w2"][l].reshape(32 * 256, 4096), "b2": z["exp_b2"][l],
              "ln2": np.concatenate([_chunk(z["ln2_g"][l]), _chunk(z["ln2_b"][l])], axis=1), "c_ident": ident}
        ins = []
        for c in range(NCORE):
            d = dict(sh)
            d["x1T"] = r1[c]["x1T"]
            ins.append(d)
        r2 = _run(_prog("b2", lambda: build_b2(nt)), ins)
        x = np.ascontiguousarray(np.concatenate([r2[c]["x2T"].T for c in range(NCORE)], axis=0), dtype=np.float32)
        del r1, r2, ins
    return x[None].astype(np.float32)
```

```python
import math
import numpy as np
import concourse.bass as bass
import concourse.mybir as mybir
from concourse.bass_utils import run_bass_kernel_spmd

F32 = mybir.dt.float32
BF16 = mybir.dt.bfloat16
AF = mybir.ActivationFunctionType
ALU = mybir.AluOpType
AX = mybir.AxisListType

D = 4096
T_FULL = 8192
NCORE = 8
KC = D // 128
ALPHA = (2 * 4) ** 0.25


class Prog:
    def __init__(self, self_sync=True):
        self.nc = bass.Bass("TRN2", target_bir_lowering=False)
        nc = self.nc
        self.eng = dict(pe=nc.tensor, act=nc.scalar, dve=nc.vector, pool=nc.gpsimd, sp=nc.sync)
        self.sems = {}
        self.cnt = {}
        self.seen = {k: {} for k in self.eng}
        self.last_w = {}
        self.rd = {}
        self.self_sync = self_sync

    def sem(self, name):
        if name not in self.sems:
            self.sems[name] = self.nc.alloc_semaphore(name=name)
            self.cnt[name] = 0
        return self.sems[name]

    def _deps(self, reads, writes):
        deps = {}

        def add(s, v):
            if deps.get(s, 0) < v:
                deps[s] = v

        for k in reads:
            d = self.last_w.get(k)
            if d is not None:
                add(*d)
        for k in writes:
            d = self.last_w.get(k)
            if d is not None:
                add(*d)
            for s, v in self.rd.get(k, {}).items():
                add(s, v)
        return deps

    def _wait(self, e, deps, nosync=False):
        own = "E_" + e
        for s, v in deps.items():
            if s == own and (nosync or not self.self_sync):
                continue
            if self.seen[e].get(s, 0) < v:
                self.eng[e].wait_ge(self.sems[s], v)
                self.seen[e][s] = v

    def _record(self, tok, reads, writes):
        s, v = tok
        for k in reads:
            d = self.rd.setdefault(k, {})
            if d.get(s, 0) < v:
                d[s] = v
        for k in writes:
            self.last_w[k] = tok
            self.rd[k] = {}

    def op(self, e, fn, reads=(), writes=(), nosync=False):
        self._wait(e, self._deps(reads, writes), nosync)
        ins = fn(self.eng[e])
        s = "E_" + e
        self.sem(s)
        self.cnt[s] += 1
        ins.then_inc(self.sems[s], 1)
        self._record((s, self.cnt[s]), reads, writes)
        return ins

    def dma(self, q, out, in_, reads=(), writes=(), sem="x", **kw):
        self._wait(q, self._deps(reads, writes))
        ins = self.eng[q].dma_start(out=out, in_=in_, **kw)
        s = "D_" + sem
        self.sem(s)
        self.cnt[s] += 16
        ins.then_inc(self.sems[s], 16)
        self._record((s, self.cnt[s]), reads, writes)
        return ins

    def finish(self, keys, e="sp"):
        self._wait(e, self._deps(keys, ()))

    def sb(self, name, shape, dt):
        return self.nc.alloc_sbuf_tensor(name, shape, dt)

    def ps(self, name, shape=(128, 512), dt=F32):
        return self.nc.alloc_psum_tensor(name, list(shape), dt)

    def din(self, name, shape, dt=F32):
        return self.nc.dram_tensor(name, list(shape), dt, kind="ExternalInput").ap()

    def dout(self, name, shape, dt=F32):
        return self.nc.dram_tensor(name, list(shape), dt, kind="ExternalOutput").ap()


def _consts(p):
    cid = p.din("c_ident", [128, 128])
    ident = p.sb("ident", [128, 128], F32)
    ones = p.sb("ones", [128, 128], F32)
    p.dma("sp", ident[:, :], cid, writes=["ident"], sem="cst")
    p.op("pool", lambda e: e.memset(ones[:, :], 1.0), writes=["ones"])
    return ident, ones


def load_xt_tile(p, xT, xt_sb, key, tt, TT=512):
    src = xT.rearrange("(kc p) t -> p kc t", p=128)
    for h in range(2):
        p.dma("pool", xt_sb[:, h * 16:(h + 1) * 16, :], src[:, h * 16:(h + 1) * 16, tt * TT:(tt + 1) * TT],
              writes=[key], sem=key)


def load_w_cols(p, w_dram, w_sb, key, ncols):
    src = w_dram.rearrange("(kc p) n -> p kc n", p=128)
    for h in range(4):
        p.dma("pool", w_sb[:, h * 8:(h + 1) * 8, :], src[:, h * 8:(h + 1) * 8, :], writes=[key], sem=key)


def inproj_fm(p, ps, pskey, w_sb, wkey, c0, ncol, xt_sb, xkey, n=512):
    for kc in range(KC):
        p.op("pe", lambda e: e.matmul(ps[0:ncol, 0:n], w_sb[:, kc, c0:c0 + ncol], xt_sb[:, kc, 0:n],
                                      start=(kc == 0), stop=(kc == KC - 1)),
             reads=[wkey, xkey], writes=[pskey])


def build_hgrn2(T=T_FULL, self_sync=True):
    p = Prog(self_sync=self_sync)
    xT = p.din("xT", [D, T])
    w = p.din("w_hg", [D, 512])
    lbraw = p.din("lb_raw", [128, 4])
    lmask = p.din("lmask", [128, 4])
    ng = p.din("norm_g", [128, 1])
    out = p.dout("o_hg", [128, T])
    ident, ones = _consts(p)
    NT = T // 512

    w_sb = p.sb("w_sb", [128, KC, 512], BF16)
    load_w_cols(p, w, w_sb, "w_sb", 512)
    xt = [p.sb(f"xt{i}", [128, KC, 512], BF16) for i in range(2)]
    sm = p.sb("sm", [128, 8], F32)
    lb = p.sb("lb", [128, 4], F32)
    p.dma("sp", sm[:, 0:4], lbraw, writes=["sm"], sem="sm")
    p.dma("sp", sm[:, 4:8], lmask, writes=["sm"], sem="sm")
    p.dma("sp", lb[:, 2:3], ng, writes=["lb"], sem="lb")
    p.op("act", lambda e: e.activation(out=sm[:, 0:4], in_=sm[:, 0:4], func=AF.Exp), reads=["sm"], writes=["sm"])
    p.op("dve", lambda e: e.reduce_sum(out=lb[:, 3:4], in_=sm[:, 0:4], axis=AX.X), reads=["sm"], writes=["lb"])
    p.op("dve", lambda e: e.reciprocal(out=lb[:, 3:4], in_=lb[:, 3:4]), reads=["lb"], writes=["lb"])
    p.op("dve", lambda e: e.tensor_mul(out=sm[:, 0:4], in0=sm[:, 0:4], in1=sm[:, 4:8]), reads=["sm"], writes=["sm"])
    p.op("dve", lambda e: e.reduce_sum(out=lb[:, 0:1], in_=sm[:, 0:4], axis=AX.X), reads=["sm"], writes=["lb"])
    p.op("dve", lambda e: e.tensor_mul(out=lb[:, 0:1], in0=lb[:, 0:1], in1=lb[:, 3:4]), reads=["lb"], writes=["lb"])
    p.op("dve", lambda e: e.tensor_scalar(out=lb[:, 1:2], in0=lb[:, 0:1], scalar1=-1.0, scalar2=1.0,
                                          op0=ALU.mult, op1=ALU.add), reads=["lb"], writes=["lb"])

    pq, pf, pi_, pog = (p.ps(n) for n in ("pq", "pf", "pi", "pog"))
    ib = [p.ps("ib0"), p.ps("ib1")]
    o_ps = p.ps("o_ps")
    ssq = p.ps("ssq")
    qT = p.sb("qT", [128, 512], F32)
    fT = p.sb("fT", [128, 512], F32)
    kT = p.sb("kT", [128, 512], F32)
    iT = p.sb("iT", [128, 512], F32)
    gT = p.sb("gT", [128, 512], F32)
    sg = p.sb("sg", [128, 512], F32)
    Dg = [p.sb(f"Dg{i}", [128, 4, 128], F32) for i in range(2)]
    S = [p.sb(f"S{i}", [128, 128], F32) for i in range(2)]
    t1 = [p.sb(f"t1{i}", [128, 128], F32) for i in range(2)]
    osb = p.sb("osb", [128, 512], F32)
    osq = p.sb("osq", [128, 512], F32)
    ob = [p.sb(f"ob{i}", [128, 512], F32) for i in range(2)]
    p.op("pool", lambda e: e.memset(S[0][:, :], 0.0), writes=["S0"])
    load_xt_tile(p, xT, xt[0], "xt0", 0)
    tok = 0
    for tt in range(NT):
        xk = f"xt{tt % 2}"
        xs = xt[tt % 2]
        if tt + 1 < NT:
            load_xt_tile(p, xT, xt[(tt + 1) % 2], f"xt{(tt + 1) % 2}", tt + 1)
        for j, (ps_, k_) in enumerate(((pq, "pq"), (pf, "pf"), (pi_, "pi"), (pog, "pog"))):
            inproj_fm(p, ps_, k_, w_sb, "w_sb", j * 128, 128, xs, xk)
        p.op("act", lambda e: e.activation(out=sg[:, :], in_=pq[:, :], func=AF.Sigmoid), reads=["pq"], writes=["sg"])
        p.op("dve", lambda e: e.tensor_mul(out=qT[:, :], in0=pq[:, :], in1=sg[:, :]), reads=["pq", "sg"], writes=["qT"])
        p.op("act", lambda e: e.activation(out=sg[:, :], in_=pf[:, :], func=AF.Sigmoid), reads=["pf"], writes=["sg"])
        p.op("dve", lambda e: e.tensor_scalar(out=fT[:, :], in0=sg[:, :], scalar1=lb[:, 1:2], scalar2=lb[:, 0:1],
                                              op0=ALU.mult, op1=ALU.add), reads=["sg", "lb"], writes=["fT"])
        p.op("dve", lambda e: e.tensor_scalar(out=kT[:, :], in0=fT[:, :], scalar1=-1.0, scalar2=1.0,
                                              op0=ALU.mult, op1=ALU.add), reads=["fT"], writes=["kT"])
        p.op("act", lambda e: e.activation(out=iT[:, :], in_=pi_[:, :], func=AF.Copy), reads=["pi"], writes=["iT"])
        p.op("act", lambda e: e.activation(out=sg[:, :], in_=pog[:, :], func=AF.Sigmoid), reads=["pog"], writes=["sg"])
        p.op("dve", lambda e: e.tensor_mul(out=gT[:, :], in0=pog[:, :], in1=sg[:, :]), reads=["pog", "sg"], writes=["gT"])
        for g in range(128):
            b = g % 2
            t0 = g * 4
            p.op("pool", lambda e: e.tensor_tensor(
                out=Dg[b][:, :, :], in0=iT[:, t0:t0 + 4].unsqueeze(2).to_broadcast([128, 4, 128]),
                in1=ident[:, :].unsqueeze(1).to_broadcast([128, 4, 128]), op=ALU.mult),
                reads=["iT", "ident"], writes=[f"Dg{b}"], nosync=True)
            p.op("pe", lambda e: e.matmul(ib[b][:, :], ones[:, :], Dg[b][:, :, :].rearrange("p a b -> p (a b)"),
                                          start=True, stop=True), reads=[f"Dg{b}", "ones"], writes=[f"ib{b}"], nosync=True)
            for j in range(4):
                t = t0 + j
                so, sn = tok % 2, (tok + 1) % 2
                tb = tok % 2
                p.op("dve", lambda e: e.tensor_scalar(out=t1[tb][:, :], in0=ib[b][:, j * 128:(j + 1) * 128],
                                                      scalar1=kT[:, t:t + 1], scalar2=None, op0=ALU.mult),
                     reads=[f"ib{b}", "kT"], writes=[f"t1{tb}"], nosync=True)
                p.op("dve", lambda e: e.scalar_tensor_tensor(out=S[sn][:, :], in0=S[so][:, :], scalar=fT[:, t:t + 1],
                                                             in1=t1[tb][:, :], op0=ALU.mult, op1=ALU.add),
                     reads=[f"S{so}", "fT", f"t1{tb}"], writes=[f"S{sn}"], nosync=True)
                p.op("pe", lambda e: e.matmul(o_ps[:, t:t + 1], S[sn][:, :], qT[:, t:t + 1], start=True, stop=True),
                     reads=[f"S{sn}", "qT"], writes=["o_ps"], nosync=True)
                tok += 1
        p.op("act", lambda e: e.activation(out=osb[:, :], in_=o_ps[:, :], func=AF.Copy), reads=["o_ps"], writes=["osb"])
        p.op("act", lambda e: e.activation(out=osq[:, :], in_=o_ps[:, :], func=AF.Square), reads=["o_ps"], writes=["osq"])
        p.op("pe", lambda e: e.matmul(ssq[:, :], ones[:, :], osq[:, :], start=True, stop=True),
             reads=["osq", "ones"], writes=["ssq"])
        p.op("dve", lambda e: e.tensor_scalar(out=osq[:, :], in0=ssq[:, :], scalar1=1.0 / 128, scalar2=1e-5,
                                              op0=ALU.mult, op1=ALU.add), reads=["ssq"], writes=["osq"])
        p.op("act", lambda e: e.activation(out=osq[:, :], in_=osq[:, :], func=AF.Sqrt), reads=["osq"], writes=["osq"])
        p.op("dve", lambda e: e.reciprocal(out=osq[:, :], in_=osq[:, :]), reads=["osq"], writes=["osq"])
        p.op("dve", lambda e: e.tensor_mul(out=osb[:, :], in0=osb[:, :], in1=osq[:, :]), reads=["osb", "osq"], writes=["osb"])
        obk = f"ob{tt % 2}"
        p.op("dve", lambda e: e.scalar_tensor_tensor(out=ob[tt % 2][:, :], in0=osb[:, :], scalar=lb[:, 2:3], in1=gT[:, :],
                                                     op0=ALU.mult, op1=ALU.mult), reads=["osb", "gT", "lb"], writes=[obk])
        p.dma("sp", out[:, tt * 512:(tt + 1) * 512], ob[tt % 2][:, :], reads=[obk], writes=["out"], sem="out")
    p.finish(["out"])
    return p


def build_hgrn2c(T=T_FULL):
    p = Prog()
    xT = p.din("xT", [D, T])
    w = p.din("w_hg", [D, 512])
    lbraw = p.din("lb_raw", [128, 4])
    lmask = p.din("lmask", [128, 4])
    ng = p.din("norm_g", [128, 1])
    tri_d = p.din("c_tri", [64, 64])
    out = p.dout("o_hg", [128, T])
    ident, ones = _consts(p)
    NT = T // 512
    w_sb = p.sb("w_sb", [128, KC, 512], BF16)
    load_w_cols(p, w, w_sb, "w_sb", 512)
    xt = [p.sb(f"xt{i}", [128, KC, 512], BF16) for i in range(2)]
    sm = p.sb("sm", [128, 8], F32)
    lb = p.sb("lb", [128, 4], F32)
    tri = p.sb("tri", [64, 64], F32)
    identb = p.sb("identb", [128, 128], BF16)
    p.dma("sp", sm[:, 0:4], lbraw, writes=["sm"], sem="sm")
    p.dma("sp", sm[:, 4:8], lmask, writes=["sm"], sem="sm")
    p.dma("sp", lb[:, 2:3], ng, writes=["lb"], sem="lb")
    p.dma("sp", tri[:, :], tri_d, writes=["tri"], sem="tri")
    p.op("act", lambda e: e.activation(out=identb[:, :], in_=ident[:, :], func=AF.Copy), reads=["ident"], writes=["identb"])
    p.op("act", lambda e: e.activation(out=sm[:, 0:4], in_=sm[:, 0:4], func=AF.Exp), reads=["sm"], writes=["sm"])
    p.op("dve", lambda e: e.reduce_sum(out=lb[:, 3:4], in_=sm[:, 0:4], axis=AX.X), reads=["sm"], writes=["lb"])
    p.op("dve", lambda e: e.reciprocal(out=lb[:, 3:4], in_=lb[:, 3:4]), reads=["lb"], writes=["lb"])
    p.op("dve", lambda e: e.tensor_mul(out=sm[:, 0:4], in0=sm[:, 0:4], in1=sm[:, 4:8]), reads=["sm"], writes=["sm"])
    p.op("dve", lambda e: e.reduce_sum(out=lb[:, 0:1], in_=sm[:, 0:4], axis=AX.X), reads=["sm"], writes=["lb"])
    p.op("dve", lambda e: e.tensor_mul(out=lb[:, 0:1], in0=lb[:, 0:1], in1=lb[:, 3:4]), reads=["lb"], writes=["lb"])
    p.op("dve", lambda e: e.tensor_scalar(out=lb[:, 1:2], in0=lb[:, 0:1], scalar1=-1.0, scalar2=1.0,
                                          op0=ALU.mult, op1=ALU.add), reads=["lb"], writes=["lb"])
    wk = [p.ps("wk0"), p.ps("wk1")]
    trp = [p.ps("trp0", (128, 512), BF16), p.ps("trp1", (128, 512), BF16)]
    attp = p.ps("attp")
    o_ps = p.ps("o_ps")
    snp = [p.ps("snp0"), p.ps("snp1")]
    F = {n: p.sb("h_" + n, [128, 512], F32) for n in ["qT", "fT", "kT", "gT", "sg", "bA", "bB", "e", "d2", "osb", "osq"]}
    B = {n: p.sb("hb_" + n, [128, 512], BF16) for n in ["ib", "qd", "kd", "qb", "kl"]}
    el = p.sb("el", [128, 8], F32)
    S = p.sb("S", [128, 128], F32)
    Sb = p.sb("Sb", [128, 128], BF16)
    tm = [p.sb(f"tm{i}", [64, 256], BF16) for i in range(2)]
    attb = [p.sb(f"attb{i}", [64, 64], BF16) for i in range(2)]
    ob = [p.sb(f"ob{i}", [128, 512], F32) for i in range(2)]
    p.op("pool", lambda e: e.memset(S[:, :], 0.0), writes=["S"])
    p.op("pool", lambda e: e.memset(Sb[:, :], 0.0), writes=["Sb"])
    load_xt_tile(p, xT, xt[0], "xt0", 0)

    def v3(t):
        return t[:, :].rearrange("p (c s) -> p c s", s=64)

    for tt in range(NT):
        xk = f"xt{tt % 2}"
        xs = xt[tt % 2]
        if tt + 1 < NT:
            load_xt_tile(p, xT, xt[(tt + 1) % 2], f"xt{(tt + 1) % 2}", tt + 1)
        inproj_fm(p, wk[0], "wk0", w_sb, "w_sb", 0, 128, xs, xk)
        p.op("act", lambda e: e.activation(out=F["sg"][:, :], in_=wk[0][:, :], func=AF.Sigmoid), reads=["wk0"], writes=["sg"])
        p.op("dve", lambda e: e.tensor_mul(out=F["qT"][:, :], in0=wk[0][:, :], in1=F["sg"][:, :]), reads=["wk0", "sg"], writes=["qT"])
        inproj_fm(p, wk[1], "wk1", w_sb, "w_sb", 128, 128, xs, xk)
        p.op("act", lambda e: e.activation(out=F["sg"][:, :], in_=wk[1][:, :], func=AF.Sigmoid), reads=["wk1"], writes=["sg"])
        p.op("dve", lambda e: e.tensor_scalar(out=F["fT"][:, :], in0=F["sg"][:, :], scalar1=lb[:, 1:2], scalar2=lb[:, 0:1],
                                              op0=ALU.mult, op1=ALU.add), reads=["sg", "lb"], writes=["fT"])
        p.op("dve", lambda e: e.tensor_scalar(out=F["kT"][:, :], in0=F["fT"][:, :], scalar1=-1.0, scalar2=1.0,
                                              op0=ALU.mult, op1=ALU.add), reads=["fT"], writes=["kT"])
        inproj_fm(p, wk[0], "wk0", w_sb, "w_sb", 256, 128, xs, xk)
        p.op("act", lambda e: e.activation(out=B["ib"][:, :], in_=wk[0][:, :], func=AF.Copy), reads=["wk0"], writes=["ib"])
        inproj_fm(p, wk[1], "wk1", w_sb, "w_sb", 384, 128, xs, xk)
        p.op("act", lambda e: e.activation(out=F["sg"][:, :], in_=wk[1][:, :], func=AF.Sigmoid), reads=["wk1"], writes=["sg"])
        p.op("dve", lambda e: e.tensor_mul(out=F["gT"][:, :], in0=wk[1][:, :], in1=F["sg"][:, :]), reads=["wk1", "sg"], writes=["gT"])
        p.op("act", lambda e: e.activation(out=F["bA"][:, :], in_=F["fT"][:, :], func=AF.Ln), reads=["fT"], writes=["bA"])
        src, dst = "bA", "bB"
        for d in (1, 2, 4, 8, 16, 32):
            p.op("pool", lambda e: e.tensor_copy(out=v3(F[dst])[:, :, 0:d], in_=v3(F[src])[:, :, 0:d]), reads=[src], writes=[dst])
            p.op("dve", lambda e: e.tensor_tensor(out=v3(F[dst])[:, :, d:64], in0=v3(F[src])[:, :, d:64], in1=v3(F[src])[:, :, 0:64 - d],
                                                  op=ALU.add), reads=[src], writes=[dst])
            src, dst = dst, src
        bk = src
        ok = dst
        b3 = v3(F[bk])
        p.op("act", lambda e: e.activation(out=F["e"][:, :], in_=F[bk][:, :], func=AF.Exp), reads=[bk], writes=["e"])
        p.op("dve", lambda e: e.tensor_mul(out=B["qb"][:, :], in0=F["qT"][:, :], in1=F["e"][:, :]), reads=["qT", "e"], writes=["qb"])
        p.op("dve", lambda e: e.tensor_tensor(out=v3(F["d2"]), in0=b3, in1=b3[:, :, 63:64].to_broadcast([128, 8, 64]), op=ALU.subtract), reads=[bk], writes=["d2"])
        p.op("act", lambda e: e.activation(out=F["e"][:, :], in_=F["d2"][:, :], func=AF.Exp, scale=-1.0), reads=["d2"], writes=["e"])
        p.op("dve", lambda e: e.tensor_mul(out=B["kl"][:, :], in0=F["kT"][:, :], in1=F["e"][:, :]), reads=["kT", "e"], writes=["kl"])
        p.op("act", lambda e: e.activation(out=el[:, :].unsqueeze(2), in_=b3[:, :, 63:64], func=AF.Exp), reads=[bk], writes=["el"])
        p.op("dve", lambda e: e.tensor_tensor(out=v3(F["d2"]), in0=b3, in1=b3[:, :, 31:32].to_broadcast([128, 8, 64]), op=ALU.subtract), reads=[bk], writes=["d2"])
        p.op("act", lambda e: e.activation(out=F["e"][:, :], in_=F["d2"][:, :], func=AF.Exp), reads=["d2"], writes=["e"])
        p.op("dve", lambda e: e.tensor_mul(out=B["qd"][:, :], in0=F["qT"][:, :], in1=F["e"][:, :]), reads=["qT", "e"], writes=["qd"])
        p.op("act", lambda e: e.activation(out=F[ok][:, :], in_=F["d2"][:, :], func=AF.Exp, scale=-1.0), reads=["d2"], writes=[ok])
        p.op("dve", lambda e: e.tensor_mul(out=B["kd"][:, :], in0=F["kT"][:, :], in1=F[ok][:, :]), reads=["kT", ok], writes=["kd"])
        for c in range(8):
            cs_ = slice(c * 64, (c + 1) * 64)
            b = c % 2
            p.op("pe", lambda e: e.transpose(trp[b][0:64, 0:128], B["ib"][:, cs_], identb[:, :]), reads=["ib", "identb"], writes=[f"trp{b}"])
            p.op("pe", lambda e: e.transpose(trp[b][0:64, 128:256], B["kl"][:, cs_], identb[:, :]), reads=["kl", "identb"], writes=[f"trp{b}"])
            p.op("act", lambda e: e.activation(out=tm[b][:, :], in_=trp[b][0:64, 0:256], func=AF.Copy), reads=[f"trp{b}"], writes=[f"tm{b}"])
            p.op("pe", lambda e: e.matmul(attp[0:64, 0:64], B["kd"][:, cs_], B["qd"][:, cs_], start=True, stop=True), reads=["kd", "qd"], writes=["attp"])
            p.op("dve", lambda e: e.tensor_mul(out=attb[b][:, :], in0=attp[0:64, 0:64], in1=tri[:, :]), reads=["attp", "tri"], writes=[f"attb{b}"])
            p.op("pe", lambda e: e.matmul(o_ps[:, cs_], Sb[:, :], B["qb"][:, cs_], start=True, stop=False), reads=["Sb", "qb"], writes=["o_ps"])
            p.op("pe", lambda e: e.matmul(o_ps[:, cs_], tm[b][:, 0:128], attb[b][:, :], start=False, stop=True), reads=[f"tm{b}", f"attb{b}"], writes=["o_ps"])
            p.op("pe", lambda e: e.matmul(snp[b][:, 0:128], tm[b][:, 128:256], tm[b][:, 0:128], start=True, stop=True), reads=[f"tm{b}"], writes=[f"snp{b}"])
            p.op("dve", lambda e: e.scalar_tensor_tensor(out=S[:, :], in0=S[:, :], scalar=el[:, c:c + 1], in1=snp[b][:, 0:128],
                                                         op0=ALU.mult, op1=ALU.add), reads=["S", "el", f"snp{b}"], writes=["S"])
            p.op("act", lambda e: e.activation(out=Sb[:, :], in_=S[:, :], func=AF.Copy), reads=["S"], writes=["Sb"])
        p.op("act", lambda e: e.activation(out=F["osb"][:, :], in_=o_ps[:, :], func=AF.Copy), reads=["o_ps"], writes=["osb"])
        p.op("act", lambda e: e.activation(out=F["osq"][:, :], in_=o_ps[:, :], func=AF.Square), reads=["o_ps"], writes=["osq"])
        p.op("pe", lambda e: e.matmul(wk[0][:, :], ones[:, :], F["osq"][:, :], start=True, stop=True), reads=["osq", "ones"], writes=["wk0"])
        p.op("dve", lambda e: e.tensor_scalar(out=F["osq"][:, :], in0=wk[0][:, :], scalar1=1.0 / 128, scalar2=1e-5,
                                              op0=ALU.mult, op1=ALU.add), reads=["wk0"], writes=["osq"])
        p.op("act", lambda e: e.activation(out=F["osq"][:, :], in_=F["osq"][:, :], func=AF.Sqrt), reads=["osq"], writes=["osq"])
        p.op("dve", lambda e: e.reciprocal(out=F["osq"][:, :], in_=F["osq"][:, :]), reads=["osq"], writes=["osq"])
        p.op("dve", lambda e: e.tensor_mul(out=F["osb"][:, :], in0=F["osb"][:, :], in1=F["osq"][:, :]), reads=["osb", "osq"], writes=["osb"])
        obk = f"ob{tt % 2}"
        p.op("dve", lambda e: e.scalar_tensor_tensor(out=ob[tt % 2][:, :], in0=F["osb"][:, :], scalar=lb[:, 2:3], in1=F["gT"][:, :],
                                                     op0=ALU.mult, op1=ALU.mult), reads=["osb", "gT", "lb"], writes=[obk])
        p.dma("sp", out[:, tt * 512:(tt + 1) * 512], ob[tt % 2][:, :], reads=[obk], writes=["out"], sem="out")
    p.finish(["out"])
    return p


def build_rwkv(T=T_FULL, stage=99, self_sync=True):
    p = Prog(self_sync=self_sync)
    xT = p.din("xT", [D, T])
    w = p.din("w_rw", [D, 672])
    mu = p.din("rw_mu", [128, 6])
    lw = p.din("rw_lora", [128, 512])
    vec = p.din("rw_vec", [128, 8])
    cst = p.din("c_rw", [128, 256])
    out = p.dout("o_rw", [128, T])
    NT = T // 512
    w_sb = p.sb("w_sb", [128, KC, 672], BF16)
    load_w_cols(p, w, w_sb, "w_sb", 672)
    xt = [p.sb(f"xt{i}", [128, KC, 512], BF16) for i in range(2)]
    mus = p.sb("mus", [128, 12], F32)
    lws = p.sb("lws", [128, 512], BF16)
    vs_ = p.sb("vecs", [128, 8], F32)
    cs = p.sb("cs", [128, 256], F32)
    p.dma("sp", mus[:, 0:6], mu, writes=["mus"], sem="c0")
    p.dma("pool", lws[:, :], lw, writes=["lws"], sem="c1")
    p.dma("sp", vs_[:, :], vec, writes=["vecs"], sem="c2")
    p.dma("sp", cs[:, :], cst, writes=["cs"], sem="c3")
    p.op("dve", lambda e: e.tensor_scalar(out=mus[:, 6:12], in0=mus[:, 0:6], scalar1=-1.0, scalar2=1.0,
                                          op0=ALU.mult, op1=ALU.add), reads=["mus"], writes=["mus"])
    ident2 = cs[:, 0:64]
    bones = cs[:, 64:192]
    wk = [p.ps("wk0"), p.ps("wk1")]
    br = [[p.ps(f"br{b}{i}") for i in range(3)] for b in range(2)]
    raw = [p.sb(f"raw{j}", [128, 520], F32) for j in range(6)]
    for j in range(6):
        p.op("pool", lambda e: e.memset(raw[j][:, 0:8], 0.0), writes=[f"raw{j}"])
    names = ["rs", "ks", "vs", "was", "g1s", "g2s", "tmp", "wT", "aT", "kkT", "kka", "nkk", "kmod", "bon", "gT", "oT",
             "cen", "sq"]
    A = {n: p.sb("a_" + n, [128, 512], F32) for n in names}
    wab = p.sb("wab", [128, 512], BF16)
    sg1 = p.sb("sg1", [128, 512], BF16)
    sg2 = p.sb("sg2", [128, 512], BF16)
    Dq = [[p.sb(f"Dq{b}{i}", [128, 4, 64], F32) for i in range(5)] for b in range(2)]
    S = p.sb("S", [128, 64], F32)
    S1 = p.sb("S1", [128, 64], F32)
    junk = p.sb("junk", [128, 64], F32)
    sa = p.sb("sa", [128, 2], F32)
    ob = [p.sb(f"ob{i}", [128, 512], F32) for i in range(2)]
    p.op("pool", lambda e: e.memset(S[:, :], 0.0), writes=["S"])
    load_xt_tile(p, xT, xt[0], "xt0", 0)

    def ew(eng, fn, r, w_, ns=False):
        p.op(eng, fn, reads=r, writes=w_, nosync=ns)

    for tt in range(NT):
        xk = f"xt{tt % 2}"
        xs = xt[tt % 2]
        if tt + 1 < NT:
            load_xt_tile(p, xT, xt[(tt + 1) % 2], f"xt{(tt + 1) % 2}", tt + 1)
        blocks = [(0, 128), (128, 128), (256, 128), (384, 128), (512, 128), (640, 32)]
        shifted = ["rs", "ks", "vs", "was", "g1s", "g2s"]
        for j, (c0, nc_) in enumerate(blocks):
            ps_ = wk[j % 2]
            pk = f"wk{j % 2}"
            inproj_fm(p, ps_, pk, w_sb, "w_sb", c0, nc_, xs, xk)
            rj = raw[j]
            ew("act", lambda e: e.activation(out=rj[0:nc_, 8:520], in_=ps_[0:nc_, :], func=AF.Copy), [pk], [f"raw{j}"])
            ew("dve", lambda e: e.tensor_scalar(out=A["tmp"][0:nc_, :], in0=rj[0:nc_, 7:519], scalar1=mus[0:nc_, j:j + 1],
                                                scalar2=None, op0=ALU.mult), [f"raw{j}", "mus"], ["tmp"])
            ew("dve", lambda e: e.scalar_tensor_tensor(out=A[shifted[j]][0:nc_, :], in0=rj[0:nc_, 8:520],
                                                       scalar=mus[0:nc_, 6 + j:7 + j], in1=A["tmp"][0:nc_, :],
                                                       op0=ALU.mult, op1=ALU.add), [f"raw{j}", "mus", "tmp"], [shifted[j]])
            ew("act", lambda e: e.activation(out=rj[0:nc_, 7:8], in_=rj[0:nc_, 519:520], func=AF.Copy), [f"raw{j}"], [f"raw{j}"])
        if stage < 2:
            p.dma('sp', out[:, tt * 512:(tt + 1) * 512], A['rs'][:, :], reads=['rs'], writes=['out'], sem='out')
            continue
        ew("act", lambda e: e.activation(out=wab[0:64, :], in_=A["was"][0:64, :], func=AF.Tanh), ["was"], ["wab"])
        ew("act", lambda e: e.activation(out=wab[64:128, :], in_=A["was"][64:128, :], func=AF.Copy), ["was"], ["wab"])
        ew("act", lambda e: e.activation(out=sg1[:, :], in_=A["g1s"][:, :], func=AF.Sigmoid), ["g1s"], ["sg1"])
        ew("act", lambda e: e.activation(out=sg2[0:32, :], in_=A["g2s"][0:32, :], func=AF.Sigmoid), ["g2s"], ["sg2"])
        ew("pe", lambda e: e.matmul(wk[0][:, :], lws[0:64, 0:128], wab[0:64, :], start=True, stop=True), ["lws", "wab"], ["wk0"])
        ew("pe", lambda e: e.matmul(wk[1][:, :], lws[64:128, 0:128], wab[64:128, :], start=True, stop=True), ["lws", "wab"], ["wk1"])
        ew("act", lambda e: e.activation(out=A["tmp"][:, :], in_=wk[0][:, :], func=AF.Sigmoid, bias=vs_[:, 0:1]), ["wk0", "vecs"], ["tmp"])
        ew("act", lambda e: e.activation(out=A["wT"][:, :], in_=A["tmp"][:, :], func=AF.Exp, scale=-math.exp(-0.5)), ["tmp"], ["wT"])
        ew("act", lambda e: e.activation(out=A["aT"][:, :], in_=wk[1][:, :], func=AF.Sigmoid, bias=vs_[:, 1:2]), ["wk1", "vecs"], ["aT"])
        ew("pe", lambda e: e.matmul(wk[0][:, :], lws[:, 128:256], sg1[:, :], start=True, stop=False), ["lws", "sg1"], ["wk0"])
        ew("pe", lambda e: e.matmul(wk[0][:, :], lws[0:32, 256:384], sg2[0:32, :], start=False, stop=True), ["lws", "sg2"], ["wk0"])
        ew("act", lambda e: e.activation(out=A["gT"][:, :], in_=wk[0][:, :], func=AF.Copy), ["wk0"], ["gT"])
        if stage < 3:
            p.dma('sp', out[:, tt * 512:(tt + 1) * 512], A['gT'][:, :], reads=['gT', 'wT', 'aT'], writes=['out'], sem='out')
            continue
        ew("dve", lambda e: e.tensor_scalar(out=A["kkT"][:, :], in0=A["ks"][:, :], scalar1=vs_[:, 2:3], scalar2=None, op0=ALU.mult), ["ks", "vecs"], ["kkT"])
        ew("act", lambda e: e.activation(out=A["sq"][:, :], in_=A["kkT"][:, :], func=AF.Square), ["kkT"], ["sq"])
        ew("pe", lambda e: e.matmul(wk[1][:, :], bones, A["sq"][:, :], start=True, stop=True), ["cs", "sq"], ["wk1"])
        ew("act", lambda e: e.activation(out=A["sq"][:, :], in_=wk[1][:, :], func=AF.Sqrt), ["wk1"], ["sq"])
        ew("dve", lambda e: e.tensor_scalar(out=A["sq"][:, :], in0=A["sq"][:, :], scalar1=1e-12, scalar2=None, op0=ALU.max), ["sq"], ["sq"])
        ew("dve", lambda e: e.reciprocal(out=A["sq"][:, :], in_=A["sq"][:, :]), ["sq"], ["sq"])
        ew("dve", lambda e: e.tensor_mul(out=A["kkT"][:, :], in0=A["kkT"][:, :], in1=A["sq"][:, :]), ["kkT", "sq"], ["kkT"])
        ew("dve", lambda e: e.tensor_mul(out=A["kka"][:, :], in0=A["kkT"][:, :], in1=A["aT"][:, :]), ["kkT", "aT"], ["kka"])
        ew("dve", lambda e: e.tensor_scalar(out=A["nkk"][:, :], in0=A["kkT"][:, :], scalar1=-1.0, scalar2=None, op0=ALU.mult), ["kkT"], ["nkk"])
        ew("dve", lambda e: e.tensor_scalar(out=A["tmp"][:, :], in0=A["aT"][:, :], scalar1=-1.0, scalar2=vs_[:, 3:4], op0=ALU.add, op1=ALU.mult), ["aT", "vecs"], ["tmp"])
        ew("dve", lambda e: e.scalar_tensor_tensor(out=A["kmod"][:, :], in0=A["tmp"][:, :], scalar=1.0, in1=A["ks"][:, :], op0=ALU.add, op1=ALU.mult), ["tmp", "ks"], ["kmod"])
        ew("dve", lambda e: e.scalar_tensor_tensor(out=A["sq"][:, :], in0=A["rs"][:, :], scalar=vs_[:, 4:5], in1=A["kmod"][:, :], op0=ALU.mult, op1=ALU.mult), ["rs", "kmod", "vecs"], ["sq"])
        ew("pe", lambda e: e.matmul(wk[1][:, :], bones, A["sq"][:, :], start=True, stop=True), ["cs", "sq"], ["wk1"])
        ew("dve", lambda e: e.tensor_mul(out=A["bon"][:, :], in0=wk[1][:, :], in1=A["vs"][:, :]), ["wk1", "vs"], ["bon"])
        if stage < 4:
            p.dma('sp', out[:, tt * 512:(tt + 1) * 512], A['bon'][:, :], reads=['bon', 'nkk', 'kka'], writes=['out'], sem='out')
            continue
        quants = ["nkk", "wT", "kka", "kmod", "rs"]
        for g in range(128):
            b = g % 2
            t0 = g * 4
            for qi, qn in enumerate(quants):
                ew("pool", lambda e: e.tensor_tensor(
                    out=Dq[b][qi][:, :, :], in0=A[qn][:, t0:t0 + 4].unsqueeze(2).to_broadcast([128, 4, 64]),
                    in1=ident2.unsqueeze(1).to_broadcast([128, 4, 64]), op=ALU.mult), [qn, "cs"], [f"Dq{b}{qi}"], ns=True)
                bank = br[b][qi // 2]
                off = (qi % 2) * 256
                ew("pe", lambda e: e.matmul(bank[:, off:off + 256], bones, Dq[b][qi][:, :, :].rearrange("p a b -> p (a b)"),
                                            start=True, stop=True), [f"Dq{b}{qi}", "cs"], [f"br{b}"], ns=True)

            def bq(qi, j):
                bank = br[b][qi // 2]
                off = (qi % 2) * 256 + j * 64
                return bank[:, off:off + 64]
            if stage == 5:
                ew('dve', lambda e: e.tensor_copy(out=A['oT'][:, t0 * 1:t0 + 4], in_=br[b][2][:, 0:4]), [f'br{b}'], ['oT'], ns=True)
                continue
            for j in range(4):
                t = t0 + j
                if stage == 6 and j > 0:
                    continue
                ew("dve", lambda e: e.scalar_tensor_tensor(out=junk[:, :], in0=bq(0, j), scalar=1.0, in1=S[:, :], op0=ALU.mult,
                                                           op1=ALU.mult, accum_out=sa[:, 0:1]), ["S", f"br{b}"], ["junk", "sa"], ns=True)
                ew("dve", lambda e: e.tensor_tensor(out=S1[:, :], in0=bq(1, j), in1=S[:, :], op=ALU.mult), ["S", f"br{b}"], ["S1"], ns=True)
                ew("dve", lambda e: e.scalar_tensor_tensor(out=S1[:, :], in0=bq(2, j), scalar=sa[:, 0:1], in1=S1[:, :], op0=ALU.mult,
                                                           op1=ALU.add), ["S1", "sa", f"br{b}"], ["S1"], ns=True)
                ew("dve", lambda e: e.scalar_tensor_tensor(out=S[:, :], in0=bq(3, j), scalar=A["vs"][:, t:t + 1], in1=S1[:, :],
                                                           op0=ALU.mult, op1=ALU.add), ["S1", "vs", f"br{b}"], ["S"], ns=True)
                ew("dve", lambda e: e.scalar_tensor_tensor(out=junk[:, :], in0=bq(4, j), scalar=1.0, in1=S[:, :], op0=ALU.mult,
                                                           op1=ALU.mult, accum_out=A["oT"][:, t:t + 1]), ["S", f"br{b}"], ["junk", "oT"], ns=True)
        ew("pe", lambda e: e.matmul(wk[0][:, :], bones, A["oT"][:, :], start=True, stop=True), ["cs", "oT"], ["wk0"])
        ew("dve", lambda e: e.scalar_tensor_tensor(out=A["cen"][:, :], in0=wk[0][:, :], scalar=-1.0 / 64, in1=A["oT"][:, :], op0=ALU.mult, op1=ALU.add), ["wk0", "oT"], ["cen"])
        ew("act", lambda e: e.activation(out=A["sq"][:, :], in_=A["cen"][:, :], func=AF.Square), ["cen"], ["sq"])
        ew("pe", lambda e: e.matmul(wk[1][:, :], bones, A["sq"][:, :], start=True, stop=True), ["cs", "sq"], ["wk1"])
        ew("dve", lambda e: e.tensor_scalar(out=A["sq"][:, :], in0=wk[1][:, :], scalar1=1.0 / 64, scalar2=64e-5, op0=ALU.mult, op1=ALU.add), ["wk1"], ["sq"])
        ew("act", lambda e: e.activation(out=A["sq"][:, :], in_=A["sq"][:, :], func=AF.Sqrt), ["sq"], ["sq"])
        ew("dve", lambda e: e.reciprocal(out=A["sq"][:, :], in_=A["sq"][:, :]), ["sq"], ["sq"])
        ew("dve", lambda e: e.tensor_mul(out=A["cen"][:, :], in0=A["cen"][:, :], in1=A["sq"][:, :]), ["cen", "sq"], ["cen"])
        ew("dve", lambda e: e.tensor_scalar(out=A["cen"][:, :], in0=A["cen"][:, :], scalar1=vs_[:, 5:6], scalar2=vs_[:, 6:7], op0=ALU.mult, op1=ALU.add), ["cen", "vecs"], ["cen"])
        ew("dve", lambda e: e.tensor_add(out=A["cen"][:, :], in0=A["cen"][:, :], in1=A["bon"][:, :]), ["cen", "bon"], ["cen"])
        obk = f"ob{tt % 2}"
        ew("dve", lambda e: e.tensor_mul(out=ob[tt % 2][:, :], in0=A["cen"][:, :], in1=A["gT"][:, :]), ["cen", "gT"], [obk])
        p.dma("sp", out[:, tt * 512:(tt + 1) * 512], ob[tt % 2][:, :], reads=[obk], writes=["out"], sem="out")
    p.finish(["out"])
    return p


def build_diff(T=T_FULL):
    p = Prog()
    xT = p.din("xT", [D, T])
    w = p.din("w_df", [D, 384])
    lamv = p.din("df_lamv", [128, 256])
    lc = p.din("df_const", [128, 2])
    gv = p.din("df_g", [128, 128])
    mk = p.din("df_mask", [128, 2048])
    out = p.dout("o_df", [128, T])
    ident, ones = _consts(p)
    NT = T // 512
    NB = T // 128
    w_sb = p.sb("w_sb", [128, KC, 384], BF16)
    load_w_cols(p, w, w_sb, "w_sb", 384)
    xt = [p.sb(f"xt{i}", [128, KC, 512], BF16) for i in range(2)]
    lam = p.sb("lam", [128, 264], F32)
    cst = p.sb("cst", [128, 4], F32)
    g_sb = p.sb("g_sb", [128, 128], F32)
    mask = p.sb("mask", [128, 2048], BF16)
    p.dma("sp", lam[:, 0:256], lamv, writes=["lam"], sem="c0")
    p.dma("sp", cst[:, 0:2], lc, writes=["cst"], sem="c1")
    p.dma("sp", g_sb[:, :], gv, writes=["g_sb"], sem="c2")
    p.dma("pool", mask[:, :], mk, writes=["mask"], sem="c3")
    p.op("dve", lambda e: e.tensor_mul(out=lam[:, 0:64], in0=lam[:, 0:64], in1=lam[:, 64:128]), reads=["lam"], writes=["lam"])
    p.op("dve", lambda e: e.tensor_mul(out=lam[:, 128:192], in0=lam[:, 128:192], in1=lam[:, 192:256]), reads=["lam"], writes=["lam"])
    p.op("dve", lambda e: e.reduce_sum(out=lam[:, 256:257], in_=lam[:, 0:64], axis=AX.X), reads=["lam"], writes=["lam"])
    p.op("dve", lambda e: e.reduce_sum(out=lam[:, 257:258], in_=lam[:, 128:192], axis=AX.X), reads=["lam"], writes=["lam"])
    p.op("act", lambda e: e.activation(out=lam[:, 256:258], in_=lam[:, 256:258], func=AF.Exp), reads=["lam"], writes=["lam"])
    p.op("dve", lambda e: e.tensor_sub(out=cst[:, 2:3], in0=lam[:, 257:258], in1=lam[:, 256:257]), reads=["lam", "cst"], writes=["cst"])
    p.op("dve", lambda e: e.tensor_sub(out=cst[:, 2:3], in0=cst[:, 2:3], in1=cst[:, 0:1]), reads=["cst"], writes=["cst"])
    p.op("dve", lambda e: e.tensor_scalar(out=g_sb[:, :], in0=g_sb[:, :], scalar1=cst[:, 1:2], scalar2=None, op0=ALU.mult),
         reads=["g_sb", "cst"], writes=["g_sb"])
    KT = [p.sb("KT0", [128, T], BF16), p.sb("KT1", [128, T], BF16)]
    p.op("pool", lambda e: e.memset(KT[0][64:128, :], 0.0), writes=["KT"])
    p.op("pool", lambda e: e.memset(KT[1][0:64, :], 0.0), writes=["KT"])
    QT = p.sb("QT", [128, 512], BF16)
    V = p.sb("V", [128, NB, 132], BF16)
    p.op("pool", lambda e: e.memset(V[:, :, 128:132], 1.0), writes=["V"])
    wk = [p.ps("wk0"), p.ps("wk1")]
    sc = [p.ps("sc0"), p.ps("sc1")]
    acc = [p.ps(f"acc{i}") for i in range(4)]
    PT = [p.sb(f"PT{i}", [128, 512], BF16) for i in range(4)]
    scb = [(sc[0], "sc0"), (sc[1], "sc1"), (wk[0], "wk0"), (wk[1], "wk1")]
    om = [p.sb(f"om{i}", [128, 4, 128], F32) for i in range(2)]
    rc = p.sb("rc", [128, 8], F32)
    junk = p.sb("junk", [128, 128], F32)
    otm = p.sb("otm", [128, 128], F32)
    ob = [p.sb(f"ob{i}", [128, 512], F32) for i in range(2)]
    load_xt_tile(p, xT, xt[0], "xt0", 0)
    it = 0
    for tt in range(NT):
        xk = f"xt{tt % 2}"
        xs = xt[tt % 2]
        if tt + 1 < NT:
            load_xt_tile(p, xT, xt[(tt + 1) % 2], f"xt{(tt + 1) % 2}", tt + 1)
        inproj_fm(p, wk[0], "wk0", w_sb, "w_sb", 0, 128, xs, xk)
        p.op("act", lambda e: e.activation(out=QT[:, :], in_=wk[0][:, :], func=AF.Copy), reads=["wk0"], writes=["QT"])
        inproj_fm(p, wk[1], "wk1", w_sb, "w_sb", 128, 128, xs, xk)
        p.op("act", lambda e: e.activation(out=KT[0][0:64, tt * 512:(tt + 1) * 512], in_=wk[1][0:64, :], func=AF.Copy), reads=["wk1"], writes=["KT"])
        p.op("act", lambda e: e.activation(out=KT[1][64:128, tt * 512:(tt + 1) * 512], in_=wk[1][64:128, :], func=AF.Copy), reads=["wk1"], writes=["KT"])
        for blk in range(4):
            for kc in range(KC):
                p.op("pe", lambda e: e.matmul(wk[0][:, blk * 128:(blk + 1) * 128], xs[:, kc, blk * 128:(blk + 1) * 128],
                                              w_sb[:, kc, 256:384], start=(kc == 0), stop=(kc == KC - 1)),
                     reads=["w_sb", xk], writes=["wk0"])
        p.op("act", lambda e: e.activation(out=V[:, tt * 4:tt * 4 + 4, 0:128],
                                           in_=wk[0][:, :].rearrange("p (a b) -> p a b", a=4), func=AF.Copy),
             reads=["wk0"], writes=["V"])
        for m in range(2):
            pr = slice(m * 64, (m + 1) * 64)
            nkb = 4 * tt + 4
            for kb in range(nkb):
                b = it % 4
                it += 1
                scp, sck = scb[b]
                p.op("pe", lambda e: e.matmul(scp[:, :], KT[m][:, kb * 128:(kb + 1) * 128], QT[:, :], start=True, stop=True),
                     reads=["KT", "QT"], writes=[sck])
                p.op("act", lambda e: e.activation(out=PT[b][:, :], in_=scp[:, :], func=AF.Exp, scale=0.125),
                     reads=[sck], writes=[f"PT{b}"])
                j = kb - 4 * tt
                if j >= 0:
                    p.op("dve", lambda e: e.tensor_mul(out=PT[b][:, :], in0=PT[b][:, :], in1=mask[:, j * 512:(j + 1) * 512]),
                         reads=[f"PT{b}", "mask"], writes=[f"PT{b}"])
                for qb in range(4):
                    if j > qb:
                        continue
                    p.op("pe", lambda e: e.matmul(acc[qb][:, 0:129], PT[b][:, qb * 128:(qb + 1) * 128], V[:, kb, 0:129],
                                                  start=(kb == 0), stop=(kb == 4 * tt + qb)),
                         reads=[f"PT{b}", "V"], writes=[f"acc{qb}"])
            for qb in range(4):
                p.op("dve", lambda e: e.reciprocal(out=rc[:, qb:qb + 1], in_=acc[qb][:, 128:129]), reads=[f"acc{qb}"], writes=["rc"])
                p.op("dve", lambda e: e.tensor_scalar(out=om[m][:, qb, :], in0=acc[qb][:, 0:128], scalar1=rc[:, qb:qb + 1],
                                                      scalar2=None, op0=ALU.mult), reads=[f"acc{qb}", "rc"], writes=[f"om{m}"])
        obk = f"ob{tt % 2}"
        for qb in range(4):
            p.op("dve", lambda e: e.scalar_tensor_tensor(out=otm[:, :], in0=om[1][:, qb, :], scalar=cst[:, 2:3], in1=om[0][:, qb, :],
                                                         op0=ALU.mult, op1=ALU.add), reads=["om0", "om1", "cst"], writes=["otm"])
            p.op("dve", lambda e: e.tensor_mul(out=junk[:, :], in0=otm[:, :], in1=otm[:, :]), reads=["otm"], writes=["junk"])
            p.op("dve", lambda e: e.reduce_sum(out=rc[:, 4:5], in_=junk[:, :], axis=AX.X), reads=["junk"], writes=["rc"])
            p.op("dve", lambda e: e.tensor_scalar(out=rc[:, 4:5], in0=rc[:, 4:5], scalar1=1.0 / 128, scalar2=1e-5, op0=ALU.mult,
                                                  op1=ALU.add), reads=["rc"], writes=["rc"])
            p.op("act", lambda e: e.activation(out=rc[:, 4:5], in_=rc[:, 4:5], func=AF.Sqrt), reads=["rc"], writes=["rc"])
            p.op("dve", lambda e: e.reciprocal(out=rc[:, 4:5], in_=rc[:, 4:5]), reads=["rc"], writes=["rc"])
            p.op("dve", lambda e: e.scalar_tensor_tensor(out=otm[:, :], in0=otm[:, :], scalar=rc[:, 4:5], in1=g_sb[:, :],
                                                         op0=ALU.mult, op1=ALU.mult), reads=["otm", "rc", "g_sb"], writes=["otm"])
            p.op("pe", lambda e: e.transpose(wk[1][:, qb * 128:(qb + 1) * 128], otm[:, :], ident[:, :]), reads=["otm", "ident"], writes=["wk1"])
        p.op("act", lambda e: e.activation(out=ob[tt % 2][:, :], in_=wk[1][:, :], func=AF.Copy), reads=["wk1"], writes=[obk])
        p.dma("sp", out[:, tt * 512:(tt + 1) * 512], ob[tt % 2][:, :], reads=[obk], writes=["out"], sem="out")
    p.finish(["out"])
    return p


def stream_w(p, st, src_ap, nchunk=32):
    i = st["i"] % len(st["bufs"])
    st["i"] += 1
    buf, key = st["bufs"][i], st["keys"][i]
    p.dma("pool", buf[:, 0:nchunk, :], src_ap, writes=[key], sem=key)
    return buf, key


def ln_finish(p, s1, s2, mean, rstd, sq, eps, n=512):
    p.op("dve", lambda e: e.tensor_scalar(out=mean[:, 0:n], in0=s1[:, 0:n], scalar1=1.0 / D, scalar2=None, op0=ALU.mult), reads=["s1"], writes=["mean"])
    p.op("dve", lambda e: e.tensor_mul(out=sq[:, 0:n], in0=mean[:, 0:n], in1=mean[:, 0:n]), reads=["mean"], writes=["sq"])
    p.op("dve", lambda e: e.scalar_tensor_tensor(out=rstd[:, 0:n], in0=s2[:, 0:n], scalar=1.0 / D, in1=sq[:, 0:n], op0=ALU.mult, op1=ALU.subtract), reads=["s2", "sq"], writes=["rstd"])
    p.op("dve", lambda e: e.tensor_scalar(out=rstd[:, 0:n], in0=rstd[:, 0:n], scalar1=eps, scalar2=None, op0=ALU.add), reads=["rstd"], writes=["rstd"])
    p.op("act", lambda e: e.activation(out=rstd[:, 0:n], in_=rstd[:, 0:n], func=AF.Sqrt), reads=["rstd"], writes=["rstd"])
    p.op("dve", lambda e: e.reciprocal(out=rstd[:, 0:n], in_=rstd[:, 0:n]), reads=["rstd"], writes=["rstd"])


def build_b1(NTOK=1024):
    p = Prog()
    xT = p.din("xT", [D, NTOK])
    oT = p.din("oT", [3072, NTOK])
    memT = p.din("memT", [D, 256])
    mln = p.din("mem_ln", [128, 64])
    w_xg = p.din("w_xg", [D, 1280])
    w_kv = p.din("w_kv", [D, 2048])
    w_br = p.din("w_br", [4096, D])
    w_gu = p.din("w_gu", [256, 16384])
    bg = p.din("b_gate", [128, 128])
    w_o = p.din("w_o", [D, D])
    ln1 = p.din("ln1", [128, 64])
    x1T = p.dout("x1T", [D, NTOK])
    NT = NTOK // 512
    ones = p.sb("ones", [128, 128], F32)
    onesb = p.sb("onesb", [128, 128], BF16)
    p.op("pool", lambda e: e.memset(ones[:, :], 1.0), writes=["ones"])
    p.op("pool", lambda e: e.memset(onesb[:, :], 1.0), writes=["onesb"])
    mlns = p.sb("mlns", [128, 64], F32)
    ln1s = p.sb("ln1s", [128, 64], F32)
    bgs = p.sb("bgs", [128, 128], F32)
    p.dma("sp", mlns[:, :], mln, writes=["mlns"], sem="c0")
    p.dma("sp", ln1s[:, :], ln1, writes=["ln1s"], sem="c1")
    p.dma("sp", bgs[:, :], bg, writes=["bgs"], sem="c2")
    st = dict(i=0, bufs=[p.sb(f"wst{i}", [128, 32, 128], BF16) for i in range(6)], keys=[f"wst{i}" for i in range(6)])
    R1 = p.sb("R1", [128, 32, 512], BF16)
    obT = p.sb("obT", [128, 24, 512], BF16)
    memn = p.sb("memn", [128, 32, 256], BF16)
    mkT = p.sb("mkT", [128, 8, 256], BF16)
    mv = p.sb("mv", [128, 2, 1024], BF16)
    xqT = p.sb("xqT", [128, 8, 512], BF16)
    gdT = p.sb("gdT", [128, 2, 512], BF16)
    oxa = p.sb("oxa", [128, 8, 512], BF16)
    PT = [p.sb(f"PT{i}", [128, 512], BF16) for i in range(2)]
    fa = {n: p.sb("f_" + n, [128, 512], F32) for n in ["sq", "mean", "rstd", "gs", "macc", "tmp", "rden", "h0", "h1", "x0", "x1", "m0", "m1"]}
    wk = [p.ps(f"wk{i}") for i in range(6)]
    s1, s2 = p.ps("s1"), p.ps("s2")
    memv = memT.rearrange("(kc p) m -> p kc m", p=128)
    for ps_ in range(2):
        for kc in range(KC):
            mk_ = f"m{kc % 2}"
            p.dma("sp", fa[mk_][:, 0:256], memv[:, kc, :], writes=[mk_], sem=mk_)
            if ps_ == 0:
                p.op("act", lambda e: e.activation(out=fa["sq"][:, 0:256], in_=fa[mk_][:, 0:256], func=AF.Square), reads=[mk_], writes=["sq"])
                p.op("pe", lambda e: e.matmul(s1[:, 0:256], ones[:, :], fa[mk_][:, 0:256], start=(kc == 0), stop=(kc == KC - 1)), reads=[mk_, "ones"], writes=["s1"])
                p.op("pe", lambda e: e.matmul(s2[:, 0:256], ones[:, :], fa["sq"][:, 0:256], start=(kc == 0), stop=(kc == KC - 1)), reads=["sq", "ones"], writes=["s2"])
            else:
                p.op("dve", lambda e: e.tensor_sub(out=fa["tmp"][:, 0:256], in0=fa[mk_][:, 0:256], in1=fa["mean"][:, 0:256]), reads=[mk_, "mean"], writes=["tmp"])
                p.op("dve", lambda e: e.tensor_mul(out=fa["tmp"][:, 0:256], in0=fa["tmp"][:, 0:256], in1=fa["rstd"][:, 0:256]), reads=["tmp", "rstd"], writes=["tmp"])
                p.op("dve", lambda e: e.tensor_scalar(out=memn[:, kc, :], in0=fa["tmp"][:, 0:256], scalar1=mlns[:, kc:kc + 1], scalar2=mlns[:, 32 + kc:33 + kc], op0=ALU.mult, op1=ALU.add), reads=["tmp", "mlns"], writes=["memn"])
        if ps_ == 0:
            ln_finish(p, s1, s2, fa["mean"], fa["rstd"], fa["sq"], 1e-5, n=256)
    kvv = w_kv.rearrange("(kc p) n -> p kc n", p=128)
    for blk in range(8):
        wb, wkey = stream_w(p, st, kvv[:, :, blk * 128:(blk + 1) * 128])
        ps_ = wk[blk % 2]
        for kc in range(KC):
            p.op("pe", lambda e: e.matmul(ps_[:, 0:256], wb[:, kc, :], memn[:, kc, :], start=(kc == 0), stop=(kc == KC - 1)), reads=[wkey, "memn"], writes=[f"wk{blk % 2}"])
        p.op("act", lambda e: e.activation(out=mkT[:, blk, :], in_=ps_[:, 0:256], func=AF.Copy), reads=[f"wk{blk % 2}"], writes=["mkT"])
    for ct in range(8):
        wb, wkey = stream_w(p, st, kvv[:, :, 1024 + ct * 128:1024 + (ct + 1) * 128])
        for mb in range(2):
            ps_ = wk[mb]
            for kc in range(KC):
                p.op("pe", lambda e: e.matmul(ps_[:, 0:128], memn[:, kc, mb * 128:(mb + 1) * 128], wb[:, kc, :], start=(kc == 0), stop=(kc == KC - 1)), reads=[wkey, "memn"], writes=[f"wk{mb}"])
            p.op("act", lambda e: e.activation(out=mv[:, mb, ct * 128:(ct + 1) * 128], in_=ps_[:, 0:128], func=AF.Copy), reads=[f"wk{mb}"], writes=["mv"])
    xgv = w_xg.rearrange("(kc p) n -> p kc n", p=128)
    brv = w_br.rearrange("(c p) f -> p c f", p=128)
    guv = w_gu.rearrange("(rc p) f -> p rc f", p=128)
    wov = w_o.rearrange("(kc p) f -> p kc f", p=128)
    xTv = xT.rearrange("(kc p) t -> p kc t", p=128)
    oTv = oT.rearrange("(c p) t -> p c t", p=128)
    wg = [p.sb(f"wg{i}", [128, 4, 2, 128], BF16) for i in range(2)]
    for tt in range(NT):
        ts_ = slice(tt * 512, (tt + 1) * 512)
        for h in range(2):
            p.dma("pool", R1[:, h * 16:(h + 1) * 16, :], xTv[:, h * 16:(h + 1) * 16, ts_], writes=["R1"], sem="R1")
        for h in range(3):
            p.dma("pool", obT[:, h * 8:(h + 1) * 8, :], oTv[:, h * 8:(h + 1) * 8, ts_], writes=["obT"], sem="obT")
        for blk in range(10):
            wb, wkey = stream_w(p, st, xgv[:, :, blk * 128:(blk + 1) * 128])
            ps_, pk = wk[blk % 2], f"wk{blk % 2}"
            for kc in range(KC):
                p.op("pe", lambda e: e.matmul(ps_[:, :], wb[:, kc, :], R1[:, kc, :], start=(kc == 0), stop=(kc == KC - 1)), reads=[wkey, "R1"], writes=[pk])
            dst = xqT[:, blk, :] if blk < 8 else gdT[:, blk - 8, :]
            p.op("act", lambda e: e.activation(out=dst, in_=ps_[:, :], func=AF.Copy), reads=[pk], writes=["xqT" if blk < 8 else "gdT"])
        for h in range(4):
            for mb in range(2):
                for dc in range(2):
                    p.op("pe", lambda e: e.matmul(wk[mb][:, :], mkT[:, 2 * h + dc, mb * 128:(mb + 1) * 128], xqT[:, 2 * h + dc, :], start=(dc == 0), stop=(dc == 1)), reads=["mkT", "xqT"], writes=[f"wk{mb}"])
                p.op("act", lambda e: e.activation(out=PT[mb][:, :], in_=wk[mb][:, :], func=AF.Exp, scale=1.0 / 16), reads=[f"wk{mb}"], writes=[f"PT{mb}"])
            for mb in range(2):
                p.op("pe", lambda e: e.matmul(wk[2][:, :], onesb[:, :], PT[mb][:, :], start=(mb == 0), stop=(mb == 1)), reads=["onesb", f"PT{mb}"], writes=["wk2"])
            p.op("dve", lambda e: e.reciprocal(out=fa["rden"][:, :], in_=wk[2][:, :]), reads=["wk2"], writes=["rden"])
            for dvb in range(2):
                for mb in range(2):
                    p.op("pe", lambda e: e.matmul(wk[3 + dvb][:, :], mv[:, mb, h * 256 + dvb * 128:h * 256 + (dvb + 1) * 128], PT[mb][:, :], start=(mb == 0), stop=(mb == 1)), reads=["mv", f"PT{mb}"], writes=[f"wk{3 + dvb}"])
                p.op("dve", lambda e: e.tensor_mul(out=oxa[:, 2 * h + dvb, :], in0=wk[3 + dvb][:, :], in1=fa["rden"][:, :]), reads=[f"wk{3 + dvb}", "rden"], writes=["oxa"])
        for fb in range(32):
            wb, wkey = stream_w(p, st, brv[:, :, fb * 128:(fb + 1) * 128])
            wgb, wgk = wg[fb % 2], f"wg{fb % 2}"
            for n in range(4):
                p.dma("pool", wgb[:, n, :, :], guv[:, :, n * 4096 + fb * 128:n * 4096 + (fb + 1) * 128], writes=[wgk], sem=wgk)
            for n in range(4):
                pb, pbk = wk[n % 2], f"wk{n % 2}"
                pg, pgk = wk[2 + n % 2], f"wk{2 + n % 2}"
                for kc in range(8):
                    rhs = obT[:, n * 8 + kc, :] if n < 3 else oxa[:, kc, :]
                    p.op("pe", lambda e: e.matmul(pb[:, :], wb[:, n * 8 + kc, :], rhs, start=(kc == 0), stop=(kc == 7)), reads=[wkey, "obT" if n < 3 else "oxa"], writes=[pbk])
                for rc in range(2):
                    p.op("pe", lambda e: e.matmul(pg[:, :], wgb[:, n, rc, :], gdT[:, rc, :], start=(rc == 0), stop=(rc == 1)), reads=[wgk, "gdT"], writes=[pgk])
                p.op("act", lambda e: e.activation(out=fa["gs"][:, :], in_=pg[:, :], func=AF.Sigmoid, bias=bgs[:, n * 32 + fb:n * 32 + fb + 1]), reads=[pgk, "bgs"], writes=["gs"])
                if n == 0:
                    p.op("dve", lambda e: e.tensor_mul(out=fa["macc"][:, :], in0=pb[:, :], in1=fa["gs"][:, :]), reads=[pbk, "gs"], writes=["macc"])
                else:
                    p.op("dve", lambda e: e.tensor_mul(out=fa["tmp"][:, :], in0=pb[:, :], in1=fa["gs"][:, :]), reads=[pbk, "gs"], writes=["tmp"])
                    dst = R1[:, fb, :] if n == 3 else fa["macc"][:, :]
                    p.op("dve", lambda e: e.tensor_add(out=dst, in0=fa["macc"][:, :], in1=fa["tmp"][:, :]), reads=["macc", "tmp"], writes=["R1" if n == 3 else "macc"])
        for fb in range(32):
            wb, wkey = stream_w(p, st, wov[:, :, fb * 128:(fb + 1) * 128])
            xk_, hk_ = f"x{fb % 2}", f"h{fb % 2}"
            p.dma("sp", fa[xk_][:, :], xT[fb * 128:(fb + 1) * 128, ts_], writes=[xk_], sem=xk_)
            py, pyk = wk[fb % 2], f"wk{fb % 2}"
            for kc in range(KC):
                p.op("pe", lambda e: e.matmul(py[:, :], wb[:, kc, :], R1[:, kc, :], start=(kc == 0), stop=(kc == KC - 1)), reads=[wkey, "R1"], writes=[pyk])
            p.op("dve", lambda e: e.scalar_tensor_tensor(out=fa[hk_][:, :], in0=fa[xk_][:, :], scalar=ALPHA, in1=py[:, :], op0=ALU.mult, op1=ALU.add), reads=[xk_, pyk], writes=[hk_])
            p.op("act", lambda e: e.activation(out=fa["sq"][:, :], in_=fa[hk_][:, :], func=AF.Square), reads=[hk_], writes=["sq"])
            p.op("pe", lambda e: e.matmul(s1[:, :], ones[:, :], fa[hk_][:, :], start=(fb == 0), stop=(fb == 31)), reads=[hk_, "ones"], writes=["s1"])
            p.op("pe", lambda e: e.matmul(s2[:, :], ones[:, :], fa["sq"][:, :], start=(fb == 0), stop=(fb == 31)), reads=["sq", "ones"], writes=["s2"])
            p.dma("sp", x1T[fb * 128:(fb + 1) * 128, ts_], fa[hk_][:, :], reads=[hk_], writes=["x1T"], sem="hout")
        ln_finish(p, s1, s2, fa["mean"], fa["rstd"], fa["sq"], 1e-5)
        for fb in range(32):
            hk_, xk_ = f"h{fb % 2}", f"x{fb % 2}"
            p.dma("sp", fa[hk_][:, :], x1T[fb * 128:(fb + 1) * 128, ts_], reads=["x1T"], writes=[hk_], sem=hk_)
            p.op("dve", lambda e: e.tensor_sub(out=fa["tmp"][:, :], in0=fa[hk_][:, :], in1=fa["mean"][:, :]), reads=[hk_, "mean"], writes=["tmp"])
            p.op("dve", lambda e: e.tensor_mul(out=fa["tmp"][:, :], in0=fa["tmp"][:, :], in1=fa["rstd"][:, :]), reads=["tmp", "rstd"], writes=["tmp"])
            p.op("dve", lambda e: e.tensor_scalar(out=fa[xk_][:, :], in0=fa["tmp"][:, :], scalar1=ln1s[:, fb:fb + 1], scalar2=ln1s[:, 32 + fb:33 + fb], op0=ALU.mult, op1=ALU.add), reads=["tmp", "ln1s"], writes=[xk_])
            p.dma("sp", x1T[fb * 128:(fb + 1) * 128, ts_], fa[xk_][:, :], reads=[xk_, "x1T"], writes=["x1T_f"], sem="xout")
    p.finish(["x1T_f"])
    return p


def build_b2(NTOK=1024):
    p = Prog()
    x1T = p.din("x1T", [D, NTOK])
    rw = p.din("router_w", [D, 32])
    rb = p.din("router_b", [32, 1])
    w1 = p.din("w1", [32 * D, 512])
    b1 = p.din("b1", [128, 128])
    w2 = p.din("w2", [32 * 256, D])
    b2 = p.din("b2", [32, D])
    ln2 = p.din("ln2", [128, 64])
    cid = p.din("c_ident", [128, 128])
    x2T = p.dout("x2T", [D, NTOK])
    NT = NTOK // 512
    ident = p.sb("ident", [128, 128], F32)
    ones = p.sb("ones", [128, 128], F32)
    p.dma("sp", ident[:, :], cid, writes=["ident"], sem="c0")
    p.op("pool", lambda e: e.memset(ones[:, :], 1.0), writes=["ones"])
    rws = p.sb("rws", [128, 32, 32], F32)
    rbs = p.sb("rbs", [32, 1], F32)
    b1s = p.sb("b1s", [128, 128], F32)
    ln2s = p.sb("ln2s", [128, 64], F32)
    p.dma("sp", rws[:, :, :], rw.rearrange("(kc p) e -> p kc e", p=128), writes=["rws"], sem="c1")
    p.dma("sp", rbs[:, :], rb, writes=["rbs"], sem="c2")
    p.dma("sp", b1s[:, :], b1, writes=["b1s"], sem="c3")
    p.dma("sp", ln2s[:, :], ln2, writes=["ln2s"], sem="c4")
    xb = p.sb("xb", [128, 32, 512], BF16)
    AT = p.sb("AT", [128, 64, 512], BF16)
    w1b = [p.sb(f"w1b{i}", [128, 8, 512], BF16) for i in range(3)]
    w2b = [p.sb(f"w2b{i}", [128, 64, 128], BF16) for i in range(2)]
    b2s = [p.sb(f"b2s{i}", [32, 128], F32) for i in range(2)]
    fa = {n: p.sb("f_" + n, [128, 512], F32) for n in ["sq", "mean", "rstd", "g", "sg", "l", "tmp", "h0", "h1", "x0", "x1", "lgT", "gT", "gm"]}
    lgt = p.sb("lgt", [128, 4, 32], F32)
    gt = p.sb("gt", [128, 4, 32], F32)
    m8 = p.sb("m8", [128, 8], F32)
    sm = p.sb("sm", [128, 4], F32)
    hp = [p.ps(f"hp{i}") for i in range(4)]
    wk = [p.ps("wk0"), p.ps("wk1")]
    s1, s2 = p.ps("s1"), p.ps("s2")
    w1v = w1.rearrange("(e kc p) f -> p e kc f", p=128, kc=32)
    w2v = w2.rearrange("(c p) d -> p c d", p=128)
    wi = 0
    for tt in range(NT):
        ts_ = slice(tt * 512, (tt + 1) * 512)
        for kc in range(KC):
            xk_ = f"x{kc % 2}"
            p.dma("sp", fa[xk_][:, :], x1T[kc * 128:(kc + 1) * 128, ts_], writes=[xk_], sem=xk_)
            p.op("pe", lambda e: e.matmul(wk[0][0:32, :], rws[:, kc, :], fa[xk_][:, :], start=(kc == 0), stop=(kc == KC - 1)), reads=["rws", xk_], writes=["wk0"])
            p.op("act", lambda e: e.activation(out=xb[:, kc, :], in_=fa[xk_][:, :], func=AF.Copy), reads=[xk_], writes=["xb"])
        p.op("dve", lambda e: e.tensor_scalar(out=fa["lgT"][0:32, :], in0=wk[0][0:32, :], scalar1=rbs[:, 0:1], scalar2=None, op0=ALU.add), reads=["wk0", "rbs"], writes=["lgT"])
        for q in range(4):
            p.op("pe", lambda e: e.transpose(wk[1][:, q * 32:(q + 1) * 32], fa["lgT"][0:32, q * 128:(q + 1) * 128], ident[0:32, 0:32]), reads=["lgT", "ident"], writes=["wk1"])
        p.op("dve", lambda e: e.tensor_copy(out=lgt[:, :, :], in_=wk[1][:, 0:128].rearrange("p (a b) -> p a b", a=4)), reads=["wk1"], writes=["lgt"])
        for q in range(4):
            p.op("dve", lambda e: e.max(out=m8[:, :], in_=lgt[:, q, :]), reads=["lgt"], writes=["m8"])
            p.op("dve", lambda e: e.tensor_scalar(out=sm[:, 0:1], in0=m8[:, 0:1], scalar1=-1.0, scalar2=None, op0=ALU.mult), reads=["m8"], writes=["sm"])
            p.op("act", lambda e: e.activation(out=gt[:, q, :], in_=lgt[:, q, :], func=AF.Exp, bias=sm[:, 0:1]), reads=["lgt", "sm"], writes=["gt"])
            p.op("dve", lambda e: e.scalar_tensor_tensor(out=gt[:, q, :], in0=lgt[:, q, :], scalar=m8[:, 3:4], in1=gt[:, q, :], op0=ALU.is_ge, op1=ALU.mult), reads=["lgt", "m8", "gt"], writes=["gt"])
            p.op("dve", lambda e: e.reduce_sum(out=sm[:, 1:2], in_=gt[:, q, :], axis=AX.X), reads=["gt"], writes=["sm"])
            p.op("dve", lambda e: e.reciprocal(out=sm[:, 1:2], in_=sm[:, 1:2]), reads=["sm"], writes=["sm"])
            p.op("dve", lambda e: e.tensor_scalar(out=gt[:, q, :], in0=gt[:, q, :], scalar1=sm[:, 1:2], scalar2=None, op0=ALU.mult), reads=["gt", "sm"], writes=["gt"])
            p.op("pe", lambda e: e.transpose(wk[0][0:32, q * 128:(q + 1) * 128], gt[:, q, :], ident[:, :]), reads=["gt", "ident"], writes=["wk0"])
        p.op("act", lambda e: e.activation(out=fa["gT"][0:32, :], in_=wk[0][0:32, :], func=AF.Copy), reads=["wk0"], writes=["gT"])
        for ex in range(32):
            for qd in range(4):
                wb, wkey = w1b[wi % 3], f"w1b{wi % 3}"
                wi += 1
                p.dma("pool", wb[:, :, :], w1v[:, ex, qd * 8:(qd + 1) * 8, :], writes=[wkey], sem=wkey)
                for k8 in range(8):
                    kc = qd * 8 + k8
                    for blk in range(4):
                        c0 = (blk % 2) * 256 + (blk // 2)
                        p.op("pe", lambda e: e.matmul(hp[blk][:, :], wb[:, k8, c0:min(c0 + 256, 512):2], xb[:, kc, :], start=(kc == 0), stop=(kc == KC - 1)), reads=[wkey, "xb"], writes=[f"hp{blk}"])
            p.op("dve", lambda e: e.tensor_scalar(out=fa["gm"][0:32, :], in0=fa["gT"][0:32, :], scalar1=ident[0:32, ex:ex + 1], scalar2=None, op0=ALU.mult), reads=["gT", "ident"], writes=["gm"])
            p.op("pe", lambda e: e.matmul(wk[1][:, :], ones[0:32, :], fa["gm"][0:32, :], start=True, stop=True), reads=["gm", "ones"], writes=["wk1"])
            for j in range(2):
                bc = ex * 4 + j
                p.op("dve", lambda e: e.tensor_scalar(out=fa["g"][:, :], in0=hp[j][:, :], scalar1=b1s[:, bc:bc + 1], scalar2=7.0, op0=ALU.add, op1=ALU.min), reads=[f"hp{j}", "b1s"], writes=["g"])
                p.op("act", lambda e: e.activation(out=fa["sg"][:, :], in_=fa["g"][:, :], func=AF.Sigmoid, scale=1.702), reads=["g"], writes=["sg"])
                p.op("dve", lambda e: e.tensor_scalar(out=fa["l"][:, :], in0=hp[2 + j][:, :], scalar1=b1s[:, bc + 2:bc + 3], scalar2=7.0, op0=ALU.add, op1=ALU.min), reads=[f"hp{2 + j}", "b1s"], writes=["l"])
                p.op("dve", lambda e: e.tensor_scalar(out=fa["l"][:, :], in0=fa["l"][:, :], scalar1=-7.0, scalar2=1.0, op0=ALU.max, op1=ALU.add), reads=["l"], writes=["l"])
                p.op("pool", lambda e: e.tensor_tensor(out=fa["g"][:, :], in0=fa["g"][:, :], in1=fa["sg"][:, :], op=ALU.mult), reads=["g", "sg"], writes=["g"])
                p.op("pool", lambda e: e.tensor_tensor(out=fa["g"][:, :], in0=fa["g"][:, :], in1=fa["l"][:, :], op=ALU.mult), reads=["g", "l"], writes=["g"])
                p.op("dve", lambda e: e.tensor_mul(out=AT[:, ex * 2 + j, :], in0=wk[1][:, :], in1=fa["g"][:, :]), reads=["wk1", "g"], writes=["AT"])
        for fb in range(32):
            wb, wkey = w2b[fb % 2], f"w2b{fb % 2}"
            for h in range(2):
                p.dma("pool", wb[:, h * 32:(h + 1) * 32, :], w2v[:, h * 32:(h + 1) * 32, fb * 128:(fb + 1) * 128], writes=[wkey], sem=wkey)
            bb, bk = b2s[fb % 2], f"b2s{fb % 2}"
            p.dma("sp", bb[:, :], b2[:, fb * 128:(fb + 1) * 128], writes=[bk], sem=bk)
            xk_, hk_ = f"x{fb % 2}", f"h{fb % 2}"
            p.dma("sp", fa[xk_][:, :], x1T[fb * 128:(fb + 1) * 128, ts_], writes=[xk_], sem=xk_)
            py, pyk = hp[fb % 2], f"hp{fb % 2}"
            for c in range(64):
                p.op("pe", lambda e: e.matmul(py[:, :], wb[:, c, :], AT[:, c, :], start=(c == 0), stop=(c == 63)), reads=[wkey, "AT"], writes=[pyk])
            pb, pbk = hp[2 + fb % 2], f"hp{2 + fb % 2}"
            p.op("pe", lambda e: e.matmul(pb[:, :], bb[:, :], fa["gT"][0:32, :], start=True, stop=True), reads=[bk, "gT"], writes=[pbk])
            p.op("dve", lambda e: e.scalar_tensor_tensor(out=fa["tmp"][:, :], in0=fa[xk_][:, :], scalar=ALPHA, in1=py[:, :], op0=ALU.mult, op1=ALU.add), reads=[xk_, pyk], writes=["tmp"])
            p.op("dve", lambda e: e.tensor_add(out=fa[hk_][:, :], in0=fa["tmp"][:, :], in1=pb[:, :]), reads=["tmp", pbk], writes=[hk_])
            p.op("act", lambda e: e.activation(out=fa["sq"][:, :], in_=fa[hk_][:, :], func=AF.Square), reads=[hk_], writes=["sq"])
            p.op("pe", lambda e: e.matmul(s1[:, :], ones[:, :], fa[hk_][:, :], start=(fb == 0), stop=(fb == 31)), reads=[hk_, "ones"], writes=["s1"])
            p.op("pe", lambda e: e.matmul(s2[:, :], ones[:, :], fa["sq"][:, :], start=(fb == 0), stop=(fb == 31)), reads=["sq", "ones"], writes=["s2"])
            p.dma("sp", x2T[fb * 128:(fb + 1) * 128, ts_], fa[hk_][:, :], reads=[hk_], writes=["x2T"], sem="hout")
        ln_finish(p, s1, s2, fa["mean"], fa["rstd"], fa["sq"], 1e-5)
        for fb in range(32):
            hk_, xk_ = f"h{fb % 2}", f"x{fb % 2}"
            p.dma("sp", fa[hk_][:, :], x2T[fb * 128:(fb + 1) * 128, ts_], reads=["x2T"], writes=[hk_], sem=hk_)
            p.op("dve", lambda e: e.tensor_sub(out=fa["tmp"][:, :], in0=fa[hk_][:, :], in1=fa["mean"][:, :]), reads=[hk_, "mean"], writes=["tmp"])
            p.op("dve", lambda e: e.tensor_mul(out=fa["tmp"][:, :], in0=fa["tmp"][:, :], in1=fa["rstd"][:, :]), reads=["tmp", "rstd"], writes=["tmp"])
            p.op("dve", lambda e: e.tensor_scalar(out=fa[xk_][:, :], in0=fa["tmp"][:, :], scalar1=ln2s[:, fb:fb + 1], scalar2=ln2s[:, 32 + fb:33 + fb], op0=ALU.mult, op1=ALU.add), reads=["tmp", "ln2s"], writes=[xk_])
            p.dma("sp", x2T[fb * 128:(fb + 1) * 128, ts_], fa[xk_][:, :], reads=[xk_, "x2T"], writes=["x2T_f"], sem="xout")
    p.finish(["x2T_f"])
    return p


def _chunk(v):
    return np.ascontiguousarray(np.asarray(v, np.float32).reshape(32, 128).T)


_PROGS = {}


def _prog(name, fn):
    return fn()


def _run(p, ins):
    return run_bass_kernel_spmd(p.nc, ins, core_ids=list(range(NCORE))).results


def kernel(**inputs):
    z = {k: np.asarray(v) for k, v in inputs.items()}
    x = np.ascontiguousarray(z["x"][0], dtype=np.float32)
    T = x.shape[0]
    ident = np.eye(128, dtype=np.float32)
    memT = np.ascontiguousarray(z["mem"][0].T)
    mem_ln = np.concatenate([_chunk(z["mem_ln_g"]), _chunk(z["mem_ln_b"])], axis=1)
    kk = np.arange(128)[:, None]
    qq = np.arange(512)[None, :]
    dmask = np.concatenate([(kk + 128 * j <= qq).astype(np.float32) for j in range(4)], axis=1)
    crw = np.zeros((128, 256), np.float32)
    crw[:, 0:64] = np.tile(np.eye(64, dtype=np.float32), (2, 1))
    crw[0:64, 64:128] = 1
    crw[64:128, 128:192] = 1
    RW0 = 7 * 1024
    for l in range(4):
        w_in = z["w_in"][l]
        xT = np.ascontiguousarray(x.T)
        ins_hg, ins_df, ins_rw = [], [], []
        li = 0.8 - 0.6 * math.exp(-0.3 * l)
        for c in range(NCORE):
            ch = np.arange(c * 128, (c + 1) * 128)
            lm = np.zeros((128, 4), np.float32)
            lm[:, 1:l + 1] = 1.0
            ins_hg.append({"xT": xT, "w_hg": np.ascontiguousarray(w_in[:, np.concatenate([j * 1024 + ch for j in range(4)])]),
                           "lb_raw": np.ascontiguousarray(z["hg_lb_raw"][:, ch].T), "lmask": lm,
                           "norm_g": np.ascontiguousarray(z["hg_norm_g"][l][:, None]), "c_ident": ident,
                           "c_tri": np.triu(np.ones((64, 64), np.float32))})
            lamv = np.tile(np.concatenate([z["df_lam_q1"][l], z["df_lam_k1"][l], z["df_lam_q2"][l], z["df_lam_k2"][l]])[None, :], (128, 1)).astype(np.float32)
            ins_df.append({"xT": xT, "w_df": np.ascontiguousarray(w_in[:, np.concatenate([4096 + ch, 5120 + ch, 6144 + ch])]),
                           "df_lamv": lamv, "df_const": np.tile(np.array([[li, 1 - li]], np.float32), (128, 1)),
                           "df_g": np.tile(z["df_subln_g"][l][None, :], (128, 1)).astype(np.float32), "df_mask": dmask, "c_ident": ident})
            cols = np.concatenate([RW0 + ch, RW0 + 1024 + ch, RW0 + 2048 + ch, RW0 + 3072 + np.arange(288)])
            mu_all = z["rw_shift_mu"][l]
            mu = np.zeros((128, 6), np.float32)
            mu[:, 0] = mu_all[ch]
            mu[:, 1] = mu_all[1024 + ch]
            mu[:, 2] = mu_all[2048 + ch]
            mu[:, 3] = mu_all[3072:3200]
            mu[:, 4] = mu_all[3200:3328]
            mu[:32, 5] = mu_all[3328:3360]
            lw = np.zeros((128, 512), np.float32)
            lw[0:64, 0:128] = z["rw_w2"][l][:, ch]
            lw[64:128, 0:128] = z["rw_a2"][l][:, ch]
            lw[:, 128:256] = z["rw_g2"][l][0:128][:, ch]
            lw[0:32, 256:384] = z["rw_g2"][l][128:160][:, ch]
            vec = np.zeros((128, 8), np.float32)
            for i_, n_ in enumerate(["rw_w0", "rw_a0", "rw_k_k", "rw_k_a"]):
                vec[:, i_] = z[n_][l][ch]
            vec[:, 4] = z["rw_r_k"][l].reshape(-1)[ch]
            vec[:, 5] = z["rw_lnx_g"][l][ch]
            vec[:, 6] = z["rw_lnx_b"][l][ch]
            ins_rw.append({"xT": xT, "w_rw": np.ascontiguousarray(w_in[:, cols]), "rw_mu": mu, "rw_lora": lw, "rw_vec": vec, "c_rw": crw})
        r_hg = _run(_prog("hg", lambda: build_hgrn2c(T)), ins_hg)
        r_df = _run(_prog("df", lambda: build_diff(T)), ins_df)
        r_rw = _run(_prog("rw", lambda: build_rwkv(T, self_sync=False)), ins_rw)
        oT_all = np.concatenate([r_hg[c]["o_hg"] for c in range(NCORE)] + [r_df[c]["o_df"] for c in range(NCORE)]
                                + [r_rw[c]["o_rw"] for c in range(NCORE)], axis=0)
        del r_hg, r_df, r_rw, ins_hg, ins_df, ins_rw
        sh = {"memT": memT, "mem_ln": mem_ln, "w_xg": np.ascontiguousarray(w_in[:, 10528:11808]), "w_kv": z["w_mem_kv"][l],
              "w_br": z["w_br"][l].reshape(4096, 4096), "w_gu": z["w_gate_up"][l],
              "b_gate": np.ascontiguousarray(z["b_gate"][l].reshape(4, 32, 128).transpose(2, 0, 1).reshape(128, 128)),
              "w_o": z["w_o"][l], "ln1": np.concatenate([_chunk(z["ln1_g"][l]), _chunk(z["ln1_b"][l])], axis=1)}
        nt = T // NCORE
        ins = []
        for c in range(NCORE):
            d = dict(sh)
            d["xT"] = np.ascontiguousarray(xT[:, c * nt:(c + 1) * nt])
            d["oT"] = np.ascontiguousarray(oT_all[:, c * nt:(c + 1) * nt])
            ins.append(d)
        r1 = _run(_prog("b1", lambda: build_b1(nt)), ins)
        b1 = z["exp_b1"][l]
        b1l = np.stack([b1[:, 0:256:2], b1[:, 256:512:2], b1[:, 1:256:2], b1[:, 257:512:2]], axis=1)
        sh = {"router_w": z["router_w"][l], "router_b": np.ascontiguousarray(z["router_b"][l][:, None]),
              "w1": z["exp_w1"][l].reshape(32 * 4096, 512), "b1": np.ascontiguousarray(b1l.transpose(2, 0, 1).reshape(128, 128)),
              "w2": z["exp_w2"][l].reshape(32 * 256, 4096), "b2": z["exp_b2"][l],
              "ln2": np.concatenate([_chunk(z["ln2_g"][l]), _chunk(z["ln2_b"][l])], axis=1), "c_ident": ident}
        ins = []
        for c in range(NCORE):
            d = dict(sh)
            d["x1T"] = r1[c]["x1T"]
            ins.append(d)
        r2 = _run(_prog("b2", lambda: build_b2(nt)), ins)
        x = np.ascontiguousarray(np.concatenate([r2[c]["x2T"].T for c in range(NCORE)], axis=0), dtype=np.float32)
        del r1, r2, ins
    return x[None].astype(np.float32)
```

```python
import math
import numpy as np
import concourse.bass as bass
import concourse.mybir as mybir
from concourse.bass_utils import run_bass_kernel_spmd

F32 = mybir.dt.float32
BF16 = mybir.dt.bfloat16
AF = mybir.ActivationFunctionType
ALU = mybir.AluOpType
AX = mybir.AxisListType

D = 4096
T_FULL = 8192
NCORE = 8
KC = D // 128
ALPHA = (2 * 4) ** 0.25


class Prog:
    def __init__(self, self_sync=True):
        self.nc = bass.Bass("TRN2", target_bir_lowering=False)
        nc = self.nc
        self.eng = dict(pe=nc.tensor, act=nc.scalar, dve=nc.vector, pool=nc.gpsimd, sp=nc.sync)
        self.sems = {}
        self.cnt = {}
        self.seen = {k: {} for k in self.eng}
        self.last_w = {}
        self.rd = {}
        self.self_sync = self_sync

    def sem(self, name):
        if name not in self.sems:
            self.sems[name] = self.nc.alloc_semaphore(name=name)
            self.cnt[name] = 0
        return self.sems[name]

    def _deps(self, reads, writes):
        deps = {}

        def add(s, v):
            if deps.get(s, 0) < v:
                deps[s] = v

        for k in reads:
            d = self.last_w.get(k)
            if d is not None:
                add(*d)
        for k in writes:
            d = self.last_w.get(k)
            if d is not None:
                add(*d)
            for s, v in self.rd.get(k, {}).items():
                add(s, v)
        return deps

    def _wait(self, e, deps, nosync=False):
        own = "E_" + e
        for s, v in deps.items():
            if s == own and (nosync or not self.self_sync or e == "pe"):
                continue
            if self.seen[e].get(s, 0) < v:
                self.eng[e].wait_ge(self.sems[s], v)
                self.seen[e][s] = v

    def _record(self, tok, reads, writes):
        s, v = tok
        for k in reads:
            d = self.rd.setdefault(k, {})
            if d.get(s, 0) < v:
                d[s] = v
        for k in writes:
            self.last_w[k] = tok
            self.rd[k] = {}

    def op(self, e, fn, reads=(), writes=(), nosync=False):
        self._wait(e, self._deps(reads, writes), nosync)
        ins = fn(self.eng[e])
        s = "E_" + e
        self.sem(s)
        self.cnt[s] += 1
        ins.then_inc(self.sems[s], 1)
        self._record((s, self.cnt[s]), reads, writes)
        return ins

    def dma(self, q, out, in_, reads=(), writes=(), sem="x", **kw):
        self._wait(q, self._deps(reads, writes))
        ins = self.eng[q].dma_start(out=out, in_=in_, **kw)
        s = "D_" + sem
        self.sem(s)
        self.cnt[s] += 16
        ins.then_inc(self.sems[s], 16)
        self._record((s, self.cnt[s]), reads, writes)
        return ins

    def finish(self, keys, e="sp"):
        self._wait(e, self._deps(keys, ()))

    def sb(self, name, shape, dt):
        return self.nc.alloc_sbuf_tensor(name, shape, dt)

    def ps(self, name, shape=(128, 512), dt=F32):
        return self.nc.alloc_psum_tensor(name, list(shape), dt)

    def din(self, name, shape, dt=F32):
        return self.nc.dram_tensor(name, list(shape), dt, kind="ExternalInput").ap()

    def dout(self, name, shape, dt=F32):
        return self.nc.dram_tensor(name, list(shape), dt, kind="ExternalOutput").ap()


def _consts(p):
    cid = p.din("c_ident", [128, 128])
    ident = p.sb("ident", [128, 128], F32)
    ones = p.sb("ones", [128, 128], F32)
    p.dma("sp", ident[:, :], cid, writes=["ident"], sem="cst")
    p.op("pool", lambda e: e.memset(ones[:, :], 1.0), writes=["ones"])
    return ident, ones


def load_xt_tile(p, xT, xt_sb, key, tt, TT=512):
    src = xT.rearrange("(kc p) t -> p kc t", p=128)
    for h in range(2):
        p.dma("pool", xt_sb[:, h * 16:(h + 1) * 16, :], src[:, h * 16:(h + 1) * 16, tt * TT:(tt + 1) * TT],
              writes=[key], sem=key)


def load_w_cols(p, w_dram, w_sb, key, ncols):
    src = w_dram.rearrange("(kc p) n -> p kc n", p=128)
    for h in range(4):
        p.dma("pool", w_sb[:, h * 8:(h + 1) * 8, :], src[:, h * 8:(h + 1) * 8, :], writes=[key], sem=key)


def inproj_fm(p, ps, pskey, w_sb, wkey, c0, ncol, xt_sb, xkey, n=512):
    for kc in range(KC):
        p.op("pe", lambda e: e.matmul(ps[0:ncol, 0:n], w_sb[:, kc, c0:c0 + ncol], xt_sb[:, kc, 0:n],
                                      start=(kc == 0), stop=(kc == KC - 1)),
             reads=[wkey, xkey], writes=[pskey])


def build_hgrn2(T=T_FULL, self_sync=True):
    p = Prog(self_sync=self_sync)
    xT = p.din("xT", [D, T])
    w = p.din("w_hg", [D, 512])
    lbraw = p.din("lb_raw", [128, 4])
    lmask = p.din("lmask", [128, 4])
    ng = p.din("norm_g", [128, 1])
    out = p.dout("o_hg", [128, T])
    ident, ones = _consts(p)
    NT = T // 512

    w_sb = p.sb("w_sb", [128, KC, 512], BF16)
    load_w_cols(p, w, w_sb, "w_sb", 512)
    xt = [p.sb(f"xt{i}", [128, KC, 512], BF16) for i in range(2)]
    sm = p.sb("sm", [128, 8], F32)
    lb = p.sb("lb", [128, 4], F32)
    p.dma("sp", sm[:, 0:4], lbraw, writes=["sm"], sem="sm")
    p.dma("sp", sm[:, 4:8], lmask, writes=["sm"], sem="sm")
    p.dma("sp", lb[:, 2:3], ng, writes=["lb"], sem="lb")
    p.op("act", lambda e: e.activation(out=sm[:, 0:4], in_=sm[:, 0:4], func=AF.Exp), reads=["sm"], writes=["sm"])
    p.op("dve", lambda e: e.reduce_sum(out=lb[:, 3:4], in_=sm[:, 0:4], axis=AX.X), reads=["sm"], writes=["lb"])
    p.op("dve", lambda e: e.reciprocal(out=lb[:, 3:4], in_=lb[:, 3:4]), reads=["lb"], writes=["lb"])
    p.op("dve", lambda e: e.tensor_mul(out=sm[:, 0:4], in0=sm[:, 0:4], in1=sm[:, 4:8]), reads=["sm"], writes=["sm"])
    p.op("dve", lambda e: e.reduce_sum(out=lb[:, 0:1], in_=sm[:, 0:4], axis=AX.X), reads=["sm"], writes=["lb"])
    p.op("dve", lambda e: e.tensor_mul(out=lb[:, 0:1], in0=lb[:, 0:1], in1=lb[:, 3:4]), reads=["lb"], writes=["lb"])
    p.op("dve", lambda e: e.tensor_scalar(out=lb[:, 1:2], in0=lb[:, 0:1], scalar1=-1.0, scalar2=1.0,
                                          op0=ALU.mult, op1=ALU.add), reads=["lb"], writes=["lb"])

    pq, pf, pi_, pog = (p.ps(n) for n in ("pq", "pf", "pi", "pog"))
    ib = [p.ps("ib0"), p.ps("ib1")]
    o_ps = p.ps("o_ps")
    ssq = p.ps("ssq")
    qT = p.sb("qT", [128, 512], F32)
    fT = p.sb("fT", [128, 512], F32)
    kT = p.sb("kT", [128, 512], F32)
    iT = p.sb("iT", [128, 512], F32)
    gT = p.sb("gT", [128, 512], F32)
    sg = p.sb("sg", [128, 512], F32)
    Dg = [p.sb(f"Dg{i}", [128, 4, 128], F32) for i in range(2)]
    S = [p.sb(f"S{i}", [128, 128], F32) for i in range(2)]
    t1 = [p.sb(f"t1{i}", [128, 128], F32) for i in range(2)]
    osb = p.sb("osb", [128, 512], F32)
    osq = p.sb("osq", [128, 512], F32)
    ob = [p.sb(f"ob{i}", [128, 512], F32) for i in range(2)]
    p.op("pool", lambda e: e.memset(S[0][:, :], 0.0), writes=["S0"])
    load_xt_tile(p, xT, xt[0], "xt0", 0)
    tok = 0
    for tt in range(NT):
        xk = f"xt{tt % 2}"
        xs = xt[tt % 2]
        if tt + 1 < NT:
            load_xt_tile(p, xT, xt[(tt + 1) % 2], f"xt{(tt + 1) % 2}", tt + 1)
        for j, (ps_, k_) in enumerate(((pq, "pq"), (pf, "pf"), (pi_, "pi"), (pog, "pog"))):
            inproj_fm(p, ps_, k_, w_sb, "w_sb", j * 128, 128, xs, xk)
        p.op("act", lambda e: e.activation(out=sg[:, :], in_=pq[:, :], func=AF.Sigmoid), reads=["pq"], writes=["sg"])
        p.op("dve", lambda e: e.tensor_mul(out=qT[:, :], in0=pq[:, :], in1=sg[:, :]), reads=["pq", "sg"], writes=["qT"])
        p.op("act", lambda e: e.activation(out=sg[:, :], in_=pf[:, :], func=AF.Sigmoid), reads=["pf"], writes=["sg"])
        p.op("dve", lambda e: e.tensor_scalar(out=fT[:, :], in0=sg[:, :], scalar1=lb[:, 1:2], scalar2=lb[:, 0:1],
                                              op0=ALU.mult, op1=ALU.add), reads=["sg", "lb"], writes=["fT"])
        p.op("dve", lambda e: e.tensor_scalar(out=kT[:, :], in0=fT[:, :], scalar1=-1.0, scalar2=1.0,
                                              op0=ALU.mult, op1=ALU.add), reads=["fT"], writes=["kT"])
        p.op("act", lambda e: e.activation(out=iT[:, :], in_=pi_[:, :], func=AF.Copy), reads=["pi"], writes=["iT"])
        p.op("act", lambda e: e.activation(out=sg[:, :], in_=pog[:, :], func=AF.Sigmoid), reads=["pog"], writes=["sg"])
        p.op("dve", lambda e: e.tensor_mul(out=gT[:, :], in0=pog[:, :], in1=sg[:, :]), reads=["pog", "sg"], writes=["gT"])
        for g in range(128):
            b = g % 2
            t0 = g * 4
            p.op("pool", lambda e: e.tensor_tensor(
                out=Dg[b][:, :, :], in0=iT[:, t0:t0 + 4].unsqueeze(2).to_broadcast([128, 4, 128]),
                in1=ident[:, :].unsqueeze(1).to_broadcast([128, 4, 128]), op=ALU.mult),
                reads=["iT", "ident"], writes=[f"Dg{b}"], nosync=True)
            p.op("pe", lambda e: e.matmul(ib[b][:, :], ones[:, :], Dg[b][:, :, :].rearrange("p a b -> p (a b)"),
                                          start=True, stop=True), reads=[f"Dg{b}", "ones"], writes=[f"ib{b}"], nosync=True)
            for j in range(4):
                t = t0 + j
                so, sn = tok % 2, (tok + 1) % 2
                tb = tok % 2
                p.op("dve", lambda e: e.tensor_scalar(out=t1[tb][:, :], in0=ib[b][:, j * 128:(j + 1) * 128],
                                                      scalar1=kT[:, t:t + 1], scalar2=None, op0=ALU.mult),
                     reads=[f"ib{b}", "kT"], writes=[f"t1{tb}"], nosync=True)
                p.op("dve", lambda e: e.scalar_tensor_tensor(out=S[sn][:, :], in0=S[so][:, :], scalar=fT[:, t:t + 1],
                                                             in1=t1[tb][:, :], op0=ALU.mult, op1=ALU.add),
                     reads=[f"S{so}", "fT", f"t1{tb}"], writes=[f"S{sn}"], nosync=True)
                p.op("pe", lambda e: e.matmul(o_ps[:, t:t + 1], S[sn][:, :], qT[:, t:t + 1], start=True, stop=True),
                     reads=[f"S{sn}", "qT"], writes=["o_ps"], nosync=True)
                tok += 1
        p.op("act", lambda e: e.activation(out=osb[:, :], in_=o_ps[:, :], func=AF.Copy), reads=["o_ps"], writes=["osb"])
        p.op("act", lambda e: e.activation(out=osq[:, :], in_=o_ps[:, :], func=AF.Square), reads=["o_ps"], writes=["osq"])
        p.op("pe", lambda e: e.matmul(ssq[:, :], ones[:, :], osq[:, :], start=True, stop=True),
             reads=["osq", "ones"], writes=["ssq"])
        p.op("dve", lambda e: e.tensor_scalar(out=osq[:, :], in0=ssq[:, :], scalar1=1.0 / 128, scalar2=1e-5,
                                              op0=ALU.mult, op1=ALU.add), reads=["ssq"], writes=["osq"])
        p.op("act", lambda e: e.activation(out=osq[:, :], in_=osq[:, :], func=AF.Sqrt), reads=["osq"], writes=["osq"])
        p.op("dve", lambda e: e.reciprocal(out=osq[:, :], in_=osq[:, :]), reads=["osq"], writes=["osq"])
        p.op("dve", lambda e: e.tensor_mul(out=osb[:, :], in0=osb[:, :], in1=osq[:, :]), reads=["osb", "osq"], writes=["osb"])
        obk = f"ob{tt % 2}"
        p.op("dve", lambda e: e.scalar_tensor_tensor(out=ob[tt % 2][:, :], in0=osb[:, :], scalar=lb[:, 2:3], in1=gT[:, :],
                                                     op0=ALU.mult, op1=ALU.mult), reads=["osb", "gT", "lb"], writes=[obk])
        p.dma("sp", out[:, tt * 512:(tt + 1) * 512], ob[tt % 2][:, :], reads=[obk], writes=["out"], sem="out")
    p.finish(["out"])
    return p


def build_hgrn2c(T=T_FULL):
    p = Prog()
    xT = p.din("xT", [D, T])
    w = p.din("w_hg", [D, 512])
    lbraw = p.din("lb_raw", [128, 4])
    lmask = p.din("lmask", [128, 4])
    ng = p.din("norm_g", [128, 1])
    tri_d = p.din("c_tri", [64, 64])
    out = p.dout("o_hg", [128, T])
    ident, ones = _consts(p)
    NT = T // 512
    w_sb = p.sb("w_sb", [128, KC, 512], BF16)
    load_w_cols(p, w, w_sb, "w_sb", 512)
    xt = [p.sb(f"xt{i}", [128, KC, 512], BF16) for i in range(2)]
    sm = p.sb("sm", [128, 8], F32)
    lb = p.sb("lb", [128, 4], F32)
    tri = p.sb("tri", [64, 64], F32)
    identb = p.sb("identb", [128, 128], BF16)
    p.dma("sp", sm[:, 0:4], lbraw, writes=["sm"], sem="sm")
    p.dma("sp", sm[:, 4:8], lmask, writes=["sm"], sem="sm")
    p.dma("sp", lb[:, 2:3], ng, writes=["lb"], sem="lb")
    p.dma("sp", tri[:, :], tri_d, writes=["tri"], sem="tri")
    p.op("act", lambda e: e.activation(out=identb[:, :], in_=ident[:, :], func=AF.Copy), reads=["ident"], writes=["identb"])
    p.op("act", lambda e: e.activation(out=sm[:, 0:4], in_=sm[:, 0:4], func=AF.Exp), reads=["sm"], writes=["sm"])
    p.op("dve", lambda e: e.reduce_sum(out=lb[:, 3:4], in_=sm[:, 0:4], axis=AX.X), reads=["sm"], writes=["lb"])
    p.op("dve", lambda e: e.reciprocal(out=lb[:, 3:4], in_=lb[:, 3:4]), reads=["lb"], writes=["lb"])
    p.op("dve", lambda e: e.tensor_mul(out=sm[:, 0:4], in0=sm[:, 0:4], in1=sm[:, 4:8]), reads=["sm"], writes=["sm"])
    p.op("dve", lambda e: e.reduce_sum(out=lb[:, 0:1], in_=sm[:, 0:4], axis=AX.X), reads=["sm"], writes=["lb"])
    p.op("dve", lambda e: e.tensor_mul(out=lb[:, 0:1], in0=lb[:, 0:1], in1=lb[:, 3:4]), reads=["lb"], writes=["lb"])
    p.op("dve", lambda e: e.tensor_scalar(out=lb[:, 1:2], in0=lb[:, 0:1], scalar1=-1.0, scalar2=1.0,
                                          op0=ALU.mult, op1=ALU.add), reads=["lb"], writes=["lb"])
    wk = [p.ps("wk0"), p.ps("wk1")]
    trp = [p.ps("trp0", (128, 512), BF16), p.ps("trp1", (128, 512), BF16)]
    attp = p.ps("attp")
    o_ps = p.ps("o_ps")
    snp = [p.ps("snp0"), p.ps("snp1")]
    F = {n: p.sb("h_" + n, [128, 512], F32) for n in ["qT", "fT", "kT", "gT", "sg", "bA", "bB", "e", "d2", "osb", "osq"]}
    B = {n: p.sb("hb_" + n, [128, 512], BF16) for n in ["ib", "qd", "kd", "qb", "kl"]}
    el = p.sb("el", [128, 8], F32)
    S = p.sb("S", [128, 128], F32)
    Sb = p.sb("Sb", [128, 128], BF16)
    tm = [p.sb(f"tm{i}", [64, 256], BF16) for i in range(2)]
    attb = [p.sb(f"attb{i}", [64, 64], BF16) for i in range(2)]
    ob = [p.sb(f"ob{i}", [128, 512], F32) for i in range(2)]
    p.op("pool", lambda e: e.memset(S[:, :], 0.0), writes=["S"])
    p.op("pool", lambda e: e.memset(Sb[:, :], 0.0), writes=["Sb"])
    load_xt_tile(p, xT, xt[0], "xt0", 0)

    def v3(t):
        return t[:, :].rearrange("p (c s) -> p c s", s=64)

    for tt in range(NT):
        xk = f"xt{tt % 2}"
        xs = xt[tt % 2]
        if tt + 1 < NT:
            load_xt_tile(p, xT, xt[(tt + 1) % 2], f"xt{(tt + 1) % 2}", tt + 1)
        inproj_fm(p, wk[0], "wk0", w_sb, "w_sb", 0, 128, xs, xk)
        p.op("act", lambda e: e.activation(out=F["sg"][:, :], in_=wk[0][:, :], func=AF.Sigmoid), reads=["wk0"], writes=["sg"])
        p.op("dve", lambda e: e.tensor_mul(out=F["qT"][:, :], in0=wk[0][:, :], in1=F["sg"][:, :]), reads=["wk0", "sg"], writes=["qT"])
        inproj_fm(p, wk[1], "wk1", w_sb, "w_sb", 128, 128, xs, xk)
        p.op("act", lambda e: e.activation(out=F["sg"][:, :], in_=wk[1][:, :], func=AF.Sigmoid), reads=["wk1"], writes=["sg"])
        p.op("dve", lambda e: e.tensor_scalar(out=F["fT"][:, :], in0=F["sg"][:, :], scalar1=lb[:, 1:2], scalar2=lb[:, 0:1],
                                              op0=ALU.mult, op1=ALU.add), reads=["sg", "lb"], writes=["fT"])
        p.op("dve", lambda e: e.tensor_scalar(out=F["kT"][:, :], in0=F["fT"][:, :], scalar1=-1.0, scalar2=1.0,
                                              op0=ALU.mult, op1=ALU.add), reads=["fT"], writes=["kT"])
        inproj_fm(p, wk[0], "wk0", w_sb, "w_sb", 256, 128, xs, xk)
        p.op("act", lambda e: e.activation(out=B["ib"][:, :], in_=wk[0][:, :], func=AF.Copy), reads=["wk0"], writes=["ib"])
        inproj_fm(p, wk[1], "wk1", w_sb, "w_sb", 384, 128, xs, xk)
        p.op("act", lambda e: e.activation(out=F["sg"][:, :], in_=wk[1][:, :], func=AF.Sigmoid), reads=["wk1"], writes=["sg"])
        p.op("dve", lambda e: e.tensor_mul(out=F["gT"][:, :], in0=wk[1][:, :], in1=F["sg"][:, :]), reads=["wk1", "sg"], writes=["gT"])
        p.op("act", lambda e: e.activation(out=F["bA"][:, :], in_=F["fT"][:, :], func=AF.Ln), reads=["fT"], writes=["bA"])
        src, dst = "bA", "bB"
        for d in (1, 2, 4, 8, 16, 32):
            p.op("pool", lambda e: e.tensor_copy(out=v3(F[dst])[:, :, 0:d], in_=v3(F[src])[:, :, 0:d]), reads=[src], writes=[dst])
            p.op("dve", lambda e: e.tensor_tensor(out=v3(F[dst])[:, :, d:64], in0=v3(F[src])[:, :, d:64], in1=v3(F[src])[:, :, 0:64 - d],
                                                  op=ALU.add), reads=[src], writes=[dst])
            src, dst = dst, src
        bk = src
        ok = dst
        b3 = v3(F[bk])
        p.op("act", lambda e: e.activation(out=F["e"][:, :], in_=F[bk][:, :], func=AF.Exp), reads=[bk], writes=["e"])
        p.op("dve", lambda e: e.tensor_mul(out=B["qb"][:, :], in0=F["qT"][:, :], in1=F["e"][:, :]), reads=["qT", "e"], writes=["qb"])
        p.op("dve", lambda e: e.tensor_tensor(out=v3(F["d2"]), in0=b3, in1=b3[:, :, 63:64].to_broadcast([128, 8, 64]), op=ALU.subtract), reads=[bk], writes=["d2"])
        p.op("act", lambda e: e.activation(out=F["e"][:, :], in_=F["d2"][:, :], func=AF.Exp, scale=-1.0), reads=["d2"], writes=["e"])
        p.op("dve", lambda e: e.tensor_mul(out=B["kl"][:, :], in0=F["kT"][:, :], in1=F["e"][:, :]), reads=["kT", "e"], writes=["kl"])
        p.op("act", lambda e: e.activation(out=el[:, :].unsqueeze(2), in_=b3[:, :, 63:64], func=AF.Exp), reads=[bk], writes=["el"])
        p.op("dve", lambda e: e.tensor_tensor(out=v3(F["d2"]), in0=b3, in1=b3[:, :, 31:32].to_broadcast([128, 8, 64]), op=ALU.subtract), reads=[bk], writes=["d2"])
        p.op("act", lambda e: e.activation(out=F["e"][:, :], in_=F["d2"][:, :], func=AF.Exp), reads=["d2"], writes=["e"])
        p.op("dve", lambda e: e.tensor_mul(out=B["qd"][:, :], in0=F["qT"][:, :], in1=F["e"][:, :]), reads=["qT", "e"], writes=["qd"])
        p.op("act", lambda e: e.activation(out=F[ok][:, :], in_=F["d2"][:, :], func=AF.Exp, scale=-1.0), reads=["d2"], writes=[ok])
        p.op("dve", lambda e: e.tensor_mul(out=B["kd"][:, :], in0=F["kT"][:, :], in1=F[ok][:, :]), reads=["kT", ok], writes=["kd"])
        for c in range(8):
            cs_ = slice(c * 64, (c + 1) * 64)
            b = c % 2
            p.op("pe", lambda e: e.transpose(trp[b][0:64, 0:128], B["ib"][:, cs_], identb[:, :]), reads=["ib", "identb"], writes=[f"trp{b}"])
            p.op("pe", lambda e: e.transpose(trp[b][0:64, 128:256], B["kl"][:, cs_], identb[:, :]), reads=["kl", "identb"], writes=[f"trp{b}"])
            p.op("act", lambda e: e.activation(out=tm[b][:, :], in_=trp[b][0:64, 0:256], func=AF.Copy), reads=[f"trp{b}"], writes=[f"tm{b}"])
            p.op("pe", lambda e: e.matmul(attp[0:64, 0:64], B["kd"][:, cs_], B["qd"][:, cs_], start=True, stop=True), reads=["kd", "qd"], writes=["attp"])
            p.op("dve", lambda e: e.tensor_mul(out=attb[b][:, :], in0=attp[0:64, 0:64], in1=tri[:, :]), reads=["attp", "tri"], writes=[f"attb{b}"])
            p.op("pe", lambda e: e.matmul(o_ps[:, cs_], Sb[:, :], B["qb"][:, cs_], start=True, stop=False), reads=["Sb", "qb"], writes=["o_ps"])
            p.op("pe", lambda e: e.matmul(o_ps[:, cs_], tm[b][:, 0:128], attb[b][:, :], start=False, stop=True), reads=[f"tm{b}", f"attb{b}"], writes=["o_ps"])
            p.op("pe", lambda e: e.matmul(snp[b][:, 0:128], tm[b][:, 128:256], tm[b][:, 0:128], start=True, stop=True), reads=[f"tm{b}"], writes=[f"snp{b}"])
            p.op("dve", lambda e: e.scalar_tensor_tensor(out=S[:, :], in0=S[:, :], scalar=el[:, c:c + 1], in1=snp[b][:, 0:128],
                                                         op0=ALU.mult, op1=ALU.add), reads=["S", "el", f"snp{b}"], writes=["S"])
            p.op("act", lambda e: e.activation(out=Sb[:, :], in_=S[:, :], func=AF.Copy), reads=["S"], writes=["Sb"])
        p.op("act", lambda e: e.activation(out=F["osb"][:, :], in_=o_ps[:, :], func=AF.Copy), reads=["o_ps"], writes=["osb"])
        p.op("act", lambda e: e.activation(out=F["osq"][:, :], in_=o_ps[:, :], func=AF.Square), reads=["o_ps"], writes=["osq"])
        p.op("pe", lambda e: e.matmul(wk[0][:, :], ones[:, :], F["osq"][:, :], start=True, stop=True), reads=["osq", "ones"], writes=["wk0"])
        p.op("dve", lambda e: e.tensor_scalar(out=F["osq"][:, :], in0=wk[0][:, :], scalar1=1.0 / 128, scalar2=1e-5,
                                              op0=ALU.mult, op1=ALU.add), reads=["wk0"], writes=["osq"])
        p.op("act", lambda e: e.activation(out=F["osq"][:, :], in_=F["osq"][:, :], func=AF.Sqrt), reads=["osq"], writes=["osq"])
        p.op("dve", lambda e: e.reciprocal(out=F["osq"][:, :], in_=F["osq"][:, :]), reads=["osq"], writes=["osq"])
        p.op("dve", lambda e: e.tensor_mul(out=F["osb"][:, :], in0=F["osb"][:, :], in1=F["osq"][:, :]), reads=["osb", "osq"], writes=["osb"])
        obk = f"ob{tt % 2}"
        p.op("dve", lambda e: e.scalar_tensor_tensor(out=ob[tt % 2][:, :], in0=F["osb"][:, :], scalar=lb[:, 2:3], in1=F["gT"][:, :],
                                                     op0=ALU.mult, op1=ALU.mult), reads=["osb", "gT", "lb"], writes=[obk])
        p.dma("sp", out[:, tt * 512:(tt + 1) * 512], ob[tt % 2][:, :], reads=[obk], writes=["out"], sem="out")
    p.finish(["out"])
    return p


def build_rwkv(T=T_FULL, stage=99, self_sync=True):
    p = Prog(self_sync=self_sync)
    xT = p.din("xT", [D, T])
    w = p.din("w_rw", [D, 672])
    mu = p.din("rw_mu", [128, 6])
    lw = p.din("rw_lora", [128, 512])
    vec = p.din("rw_vec", [128, 8])
    cst = p.din("c_rw", [128, 256])
    out = p.dout("o_rw", [128, T])
    NT = T // 512
    w_sb = p.sb("w_sb", [128, KC, 672], BF16)
    load_w_cols(p, w, w_sb, "w_sb", 672)
    xt = [p.sb(f"xt{i}", [128, KC, 512], BF16) for i in range(2)]
    mus = p.sb("mus", [128, 12], F32)
    lws = p.sb("lws", [128, 512], BF16)
    vs_ = p.sb("vecs", [128, 8], F32)
    cs = p.sb("cs", [128, 256], F32)
    p.dma("sp", mus[:, 0:6], mu, writes=["mus"], sem="c0")
    p.dma("pool", lws[:, :], lw, writes=["lws"], sem="c1")
    p.dma("sp", vs_[:, :], vec, writes=["vecs"], sem="c2")
    p.dma("sp", cs[:, :], cst, writes=["cs"], sem="c3")
    p.op("dve", lambda e: e.tensor_scalar(out=mus[:, 6:12], in0=mus[:, 0:6], scalar1=-1.0, scalar2=1.0,
                                          op0=ALU.mult, op1=ALU.add), reads=["mus"], writes=["mus"])
    ident2 = cs[:, 0:64]
    bones = cs[:, 64:192]
    wk = [p.ps("wk0"), p.ps("wk1")]
    br = [[p.ps(f"br{b}{i}") for i in range(3)] for b in range(2)]
    raw = [p.sb(f"raw{j}", [128, 520], F32) for j in range(6)]
    for j in range(6):
        p.op("pool", lambda e: e.memset(raw[j][:, 0:8], 0.0), writes=[f"raw{j}"])
    names = ["rs", "ks", "vs", "was", "g1s", "g2s", "tmp", "wT", "aT", "kkT", "kka", "nkk", "kmod", "bon", "gT", "oT",
             "cen", "sq"]
    A = {n: p.sb("a_" + n, [128, 512], F32) for n in names}
    wab = p.sb("wab", [128, 512], BF16)
    sg1 = p.sb("sg1", [128, 512], BF16)
    sg2 = p.sb("sg2", [128, 512], BF16)
    Dq = [[p.sb(f"Dq{b}{i}", [128, 4, 64], F32) for i in range(5)] for b in range(2)]
    S = p.sb("S", [128, 64], F32)
    S1 = p.sb("S1", [128, 64], F32)
    junk = p.sb("junk", [128, 64], F32)
    sa = p.sb("sa", [128, 2], F32)
    ob = [p.sb(f"ob{i}", [128, 512], F32) for i in range(2)]
    p.op("pool", lambda e: e.memset(S[:, :], 0.0), writes=["S"])
    load_xt_tile(p, xT, xt[0], "xt0", 0)

    def ew(eng, fn, r, w_, ns=False):
        p.op(eng, fn, reads=r, writes=w_, nosync=ns)

    for tt in range(NT):
        xk = f"xt{tt % 2}"
        xs = xt[tt % 2]
        if tt + 1 < NT:
            load_xt_tile(p, xT, xt[(tt + 1) % 2], f"xt{(tt + 1) % 2}", tt + 1)
        blocks = [(0, 128), (128, 128), (256, 128), (384, 128), (512, 128), (640, 32)]
        shifted = ["rs", "ks", "vs", "was", "g1s", "g2s"]
        for j, (c0, nc_) in enumerate(blocks):
            ps_ = wk[j % 2]
            pk = f"wk{j % 2}"
            inproj_fm(p, ps_, pk, w_sb, "w_sb", c0, nc_, xs, xk)
            rj = raw[j]
            ew("act", lambda e: e.activation(out=rj[0:nc_, 8:520], in_=ps_[0:nc_, :], func=AF.Copy), [pk], [f"raw{j}"])
            ew("dve", lambda e: e.tensor_scalar(out=A["tmp"][0:nc_, :], in0=rj[0:nc_, 7:519], scalar1=mus[0:nc_, j:j + 1],
                                                scalar2=None, op0=ALU.mult), [f"raw{j}", "mus"], ["tmp"])
            ew("dve", lambda e: e.scalar_tensor_tensor(out=A[shifted[j]][0:nc_, :], in0=rj[0:nc_, 8:520],
                                                       scalar=mus[0:nc_, 6 + j:7 + j], in1=A["tmp"][0:nc_, :],
                                                       op0=ALU.mult, op1=ALU.add), [f"raw{j}", "mus", "tmp"], [shifted[j]])
            ew("act", lambda e: e.activation(out=rj[0:nc_, 7:8], in_=rj[0:nc_, 519:520], func=AF.Copy), [f"raw{j}"], [f"raw{j}"])
        if stage < 2:
            p.dma('sp', out[:, tt * 512:(tt + 1) * 512], A['rs'][:, :], reads=['rs'], writes=['out'], sem='out')
            continue
        ew("act", lambda e: e.activation(out=wab[0:64, :], in_=A["was"][0:64, :], func=AF.Tanh), ["was"], ["wab"])
        ew("act", lambda e: e.activation(out=wab[64:128, :], in_=A["was"][64:128, :], func=AF.Copy), ["was"], ["wab"])
        ew("act", lambda e: e.activation(out=sg1[:, :], in_=A["g1s"][:, :], func=AF.Sigmoid), ["g1s"], ["sg1"])
        ew("act", lambda e: e.activation(out=sg2[0:32, :], in_=A["g2s"][0:32, :], func=AF.Sigmoid), ["g2s"], ["sg2"])
        ew("pe", lambda e: e.matmul(wk[0][:, :], lws[0:64, 0:128], wab[0:64, :], start=True, stop=True), ["lws", "wab"], ["wk0"])
        ew("pe", lambda e: e.matmul(wk[1][:, :], lws[64:128, 0:128], wab[64:128, :], start=True, stop=True), ["lws", "wab"], ["wk1"])
        ew("act", lambda e: e.activation(out=A["tmp"][:, :], in_=wk[0][:, :], func=AF.Sigmoid, bias=vs_[:, 0:1]), ["wk0", "vecs"], ["tmp"])
        ew("act", lambda e: e.activation(out=A["wT"][:, :], in_=A["tmp"][:, :], func=AF.Exp, scale=-math.exp(-0.5)), ["tmp"], ["wT"])
        ew("act", lambda e: e.activation(out=A["aT"][:, :], in_=wk[1][:, :], func=AF.Sigmoid, bias=vs_[:, 1:2]), ["wk1", "vecs"], ["aT"])
        ew("pe", lambda e: e.matmul(wk[0][:, :], lws[:, 128:256], sg1[:, :], start=True, stop=False), ["lws", "sg1"], ["wk0"])
        ew("pe", lambda e: e.matmul(wk[0][:, :], lws[0:32, 256:384], sg2[0:32, :], start=False, stop=True), ["lws", "sg2"], ["wk0"])
        ew("act", lambda e: e.activation(out=A["gT"][:, :], in_=wk[0][:, :], func=AF.Copy), ["wk0"], ["gT"])
        if stage < 3:
            p.dma('sp', out[:, tt * 512:(tt + 1) * 512], A['gT'][:, :], reads=['gT', 'wT', 'aT'], writes=['out'], sem='out')
            continue
        ew("dve", lambda e: e.tensor_scalar(out=A["kkT"][:, :], in0=A["ks"][:, :], scalar1=vs_[:, 2:3], scalar2=None, op0=ALU.mult), ["ks", "vecs"], ["kkT"])
        ew("act", lambda e: e.activation(out=A["sq"][:, :], in_=A["kkT"][:, :], func=AF.Square), ["kkT"], ["sq"])
        ew("pe", lambda e: e.matmul(wk[1][:, :], bones, A["sq"][:, :], start=True, stop=True), ["cs", "sq"], ["wk1"])
        ew("act", lambda e: e.activation(out=A["sq"][:, :], in_=wk[1][:, :], func=AF.Sqrt), ["wk1"], ["sq"])
        ew("dve", lambda e: e.tensor_scalar(out=A["sq"][:, :], in0=A["sq"][:, :], scalar1=1e-12, scalar2=None, op0=ALU.max), ["sq"], ["sq"])
        ew("dve", lambda e: e.reciprocal(out=A["sq"][:, :], in_=A["sq"][:, :]), ["sq"], ["sq"])
        ew("dve", lambda e: e.tensor_mul(out=A["kkT"][:, :], in0=A["kkT"][:, :], in1=A["sq"][:, :]), ["kkT", "sq"], ["kkT"])
        ew("dve", lambda e: e.tensor_mul(out=A["kka"][:, :], in0=A["kkT"][:, :], in1=A["aT"][:, :]), ["kkT", "aT"], ["kka"])
        ew("dve", lambda e: e.tensor_scalar(out=A["nkk"][:, :], in0=A["kkT"][:, :], scalar1=-1.0, scalar2=None, op0=ALU.mult), ["kkT"], ["nkk"])
        ew("dve", lambda e: e.tensor_scalar(out=A["tmp"][:, :], in0=A["aT"][:, :], scalar1=-1.0, scalar2=vs_[:, 3:4], op0=ALU.add, op1=ALU.mult), ["aT", "vecs"], ["tmp"])
        ew("dve", lambda e: e.scalar_tensor_tensor(out=A["kmod"][:, :], in0=A["tmp"][:, :], scalar=1.0, in1=A["ks"][:, :], op0=ALU.add, op1=ALU.mult), ["tmp", "ks"], ["kmod"])
        ew("dve", lambda e: e.scalar_tensor_tensor(out=A["sq"][:, :], in0=A["rs"][:, :], scalar=vs_[:, 4:5], in1=A["kmod"][:, :], op0=ALU.mult, op1=ALU.mult), ["rs", "kmod", "vecs"], ["sq"])
        ew("pe", lambda e: e.matmul(wk[1][:, :], bones, A["sq"][:, :], start=True, stop=True), ["cs", "sq"], ["wk1"])
        ew("dve", lambda e: e.tensor_mul(out=A["bon"][:, :], in0=wk[1][:, :], in1=A["vs"][:, :]), ["wk1", "vs"], ["bon"])
        if stage < 4:
            p.dma('sp', out[:, tt * 512:(tt + 1) * 512], A['bon'][:, :], reads=['bon', 'nkk', 'kka'], writes=['out'], sem='out')
            continue
        quants = ["nkk", "wT", "kka", "kmod", "rs"]
        for g in range(128):
            b = g % 2
            t0 = g * 4
            for qi, qn in enumerate(quants):
                ew("pool", lambda e: e.tensor_tensor(
                    out=Dq[b][qi][:, :, :], in0=A[qn][:, t0:t0 + 4].unsqueeze(2).to_broadcast([128, 4, 64]),
                    in1=ident2.unsqueeze(1).to_broadcast([128, 4, 64]), op=ALU.mult), [qn, "cs"], [f"Dq{b}{qi}"], ns=True)
                bank = br[b][qi // 2]
                off = (qi % 2) * 256
                ew("pe", lambda e: e.matmul(bank[:, off:off + 256], bones, Dq[b][qi][:, :, :].rearrange("p a b -> p (a b)"),
                                            start=True, stop=True), [f"Dq{b}{qi}", "cs"], [f"br{b}"], ns=True)

            def bq(qi, j):
                bank = br[b][qi // 2]
                off = (qi % 2) * 256 + j * 64
                return bank[:, off:off + 64]
            if stage == 5:
                ew('dve', lambda e: e.tensor_copy(out=A['oT'][:, t0 * 1:t0 + 4], in_=br[b][2][:, 0:4]), [f'br{b}'], ['oT'], ns=True)
                continue
            for j in range(4):
                t = t0 + j
                if stage == 6 and j > 0:
                    continue
                ew("dve", lambda e: e.scalar_tensor_tensor(out=junk[:, :], in0=bq(0, j), scalar=1.0, in1=S[:, :], op0=ALU.mult,
                                                           op1=ALU.mult, accum_out=sa[:, 0:1]), ["S", f"br{b}"], ["junk", "sa"], ns=True)
                ew("dve", lambda e: e.tensor_tensor(out=S1[:, :], in0=bq(1, j), in1=S[:, :], op=ALU.mult), ["S", f"br{b}"], ["S1"], ns=True)
                ew("dve", lambda e: e.scalar_tensor_tensor(out=S1[:, :], in0=bq(2, j), scalar=sa[:, 0:1], in1=S1[:, :], op0=ALU.mult,
                                                           op1=ALU.add), ["S1", "sa", f"br{b}"], ["S1"], ns=True)
                ew("dve", lambda e: e.scalar_tensor_tensor(out=S[:, :], in0=bq(3, j), scalar=A["vs"][:, t:t + 1], in1=S1[:, :],
                                                           op0=ALU.mult, op1=ALU.add), ["S1", "vs", f"br{b}"], ["S"], ns=True)
                ew("dve", lambda e: e.scalar_tensor_tensor(out=junk[:, :], in0=bq(4, j), scalar=1.0, in1=S[:, :], op0=ALU.mult,
                                                           op1=ALU.mult, accum_out=A["oT"][:, t:t + 1]), ["S", f"br{b}"], ["junk", "oT"], ns=True)
        ew("pe", lambda e: e.matmul(wk[0][:, :], bones, A["oT"][:, :], start=True, stop=True), ["cs", "oT"], ["wk0"])
        ew("dve", lambda e: e.scalar_tensor_tensor(out=A["cen"][:, :], in0=wk[0][:, :], scalar=-1.0 / 64, in1=A["oT"][:, :], op0=ALU.mult, op1=ALU.add), ["wk0", "oT"], ["cen"])
        ew("act", lambda e: e.activation(out=A["sq"][:, :], in_=A["cen"][:, :], func=AF.Square), ["cen"], ["sq"])
        ew("pe", lambda e: e.matmul(wk[1][:, :], bones, A["sq"][:, :], start=True, stop=True), ["cs", "sq"], ["wk1"])
        ew("dve", lambda e: e.tensor_scalar(out=A["sq"][:, :], in0=wk[1][:, :], scalar1=1.0 / 64, scalar2=64e-5, op0=ALU.mult, op1=ALU.add), ["wk1"], ["sq"])
        ew("act", lambda e: e.activation(out=A["sq"][:, :], in_=A["sq"][:, :], func=AF.Sqrt), ["sq"], ["sq"])
        ew("dve", lambda e: e.reciprocal(out=A["sq"][:, :], in_=A["sq"][:, :]), ["sq"], ["sq"])
        ew("dve", lambda e: e.tensor_mul(out=A["cen"][:, :], in0=A["cen"][:, :], in1=A["sq"][:, :]), ["cen", "sq"], ["cen"])
        ew("dve", lambda e: e.tensor_scalar(out=A["cen"][:, :], in0=A["cen"][:, :], scalar1=vs_[:, 5:6], scalar2=vs_[:, 6:7], op0=ALU.mult, op1=ALU.add), ["cen", "vecs"], ["cen"])
        ew("dve", lambda e: e.tensor_add(out=A["cen"][:, :], in0=A["cen"][:, :], in1=A["bon"][:, :]), ["cen", "bon"], ["cen"])
        obk = f"ob{tt % 2}"
        ew("dve", lambda e: e.tensor_mul(out=ob[tt % 2][:, :], in0=A["cen"][:, :], in1=A["gT"][:, :]), ["cen", "gT"], [obk])
        p.dma("sp", out[:, tt * 512:(tt + 1) * 512], ob[tt % 2][:, :], reads=[obk], writes=["out"], sem="out")
    p.finish(["out"])
    return p


def build_diff(T=T_FULL, self_sync=True):
    p = Prog(self_sync=self_sync)
    xT = p.din("xT", [D, T])
    w = p.din("w_df", [D, 384])
    lamv = p.din("df_lamv", [128, 256])
    lc = p.din("df_const", [128, 2])
    gv = p.din("df_g", [128, 128])
    mk = p.din("df_mask", [128, 2048])
    out = p.dout("o_df", [128, T])
    ident, ones = _consts(p)
    NT = T // 512
    NB = T // 128
    w_sb = p.sb("w_sb", [128, KC, 384], BF16)
    load_w_cols(p, w, w_sb, "w_sb", 384)
    xt = [p.sb(f"xt{i}", [128, KC, 512], BF16) for i in range(2)]
    lam = p.sb("lam", [128, 264], F32)
    cst = p.sb("cst", [128, 4], F32)
    g_sb = p.sb("g_sb", [128, 128], F32)
    mask = p.sb("mask", [128, 2048], BF16)
    p.dma("sp", lam[:, 0:256], lamv, writes=["lam"], sem="c0")
    p.dma("sp", cst[:, 0:2], lc, writes=["cst"], sem="c1")
    p.dma("sp", g_sb[:, :], gv, writes=["g_sb"], sem="c2")
    p.dma("pool", mask[:, :], mk, writes=["mask"], sem="c3")
    p.op("dve", lambda e: e.tensor_mul(out=lam[:, 0:64], in0=lam[:, 0:64], in1=lam[:, 64:128]), reads=["lam"], writes=["lam"])
    p.op("dve", lambda e: e.tensor_mul(out=lam[:, 128:192], in0=lam[:, 128:192], in1=lam[:, 192:256]), reads=["lam"], writes=["lam"])
    p.op("dve", lambda e: e.reduce_sum(out=lam[:, 256:257], in_=lam[:, 0:64], axis=AX.X), reads=["lam"], writes=["lam"])
    p.op("dve", lambda e: e.reduce_sum(out=lam[:, 257:258], in_=lam[:, 128:192], axis=AX.X), reads=["lam"], writes=["lam"])
    p.op("act", lambda e: e.activation(out=lam[:, 256:258], in_=lam[:, 256:258], func=AF.Exp), reads=["lam"], writes=["lam"])
    p.op("dve", lambda e: e.tensor_sub(out=cst[:, 2:3], in0=lam[:, 257:258], in1=lam[:, 256:257]), reads=["lam", "cst"], writes=["cst"])
    p.op("dve", lambda e: e.tensor_sub(out=cst[:, 2:3], in0=cst[:, 2:3], in1=cst[:, 0:1]), reads=["cst"], writes=["cst"])
    p.op("dve", lambda e: e.tensor_scalar(out=g_sb[:, :], in0=g_sb[:, :], scalar1=cst[:, 1:2], scalar2=None, op0=ALU.mult),
         reads=["g_sb", "cst"], writes=["g_sb"])
    KT = [p.sb("KT0", [128, T], BF16), p.sb("KT1", [128, T], BF16)]
    p.op("pool", lambda e: e.memset(KT[0][64:128, :], 0.0), writes=["KT"])
    p.op("pool", lambda e: e.memset(KT[1][0:64, :], 0.0), writes=["KT"])
    QT = p.sb("QT", [128, 512], BF16)
    V = p.sb("V", [128, NB, 132], BF16)
    p.op("pool", lambda e: e.memset(V[:, :, 128:132], 1.0), writes=["V"])
    wk = [p.ps("wk0"), p.ps("wk1")]
    sc = [p.ps("sc0"), p.ps("sc1")]
    acc = [p.ps(f"acc{i}") for i in range(4)]
    PT = [p.sb(f"PT{i}", [128, 512], BF16) for i in range(4)]
    scb = [(sc[0], "sc0"), (sc[1], "sc1"), (wk[0], "wk0"), (wk[1], "wk1")]
    om = [p.sb(f"om{i}", [128, 4, 128], F32) for i in range(2)]
    rc = p.sb("rc", [128, 8], F32)
    junk = p.sb("junk", [128, 128], F32)
    otm = p.sb("otm", [128, 128], F32)
    ob = [p.sb(f"ob{i}", [128, 512], F32) for i in range(2)]
    load_xt_tile(p, xT, xt[0], "xt0", 0)
    it = 0
    for tt in range(NT):
        xk = f"xt{tt % 2}"
        xs = xt[tt % 2]
        if tt + 1 < NT:
            load_xt_tile(p, xT, xt[(tt + 1) % 2], f"xt{(tt + 1) % 2}", tt + 1)
        inproj_fm(p, wk[0], "wk0", w_sb, "w_sb", 0, 128, xs, xk)
        p.op("act", lambda e: e.activation(out=QT[:, :], in_=wk[0][:, :], func=AF.Copy), reads=["wk0"], writes=["QT"])
        inproj_fm(p, wk[1], "wk1", w_sb, "w_sb", 128, 128, xs, xk)
        p.op("act", lambda e: e.activation(out=KT[0][0:64, tt * 512:(tt + 1) * 512], in_=wk[1][0:64, :], func=AF.Copy), reads=["wk1"], writes=["KT"])
        p.op("act", lambda e: e.activation(out=KT[1][64:128, tt * 512:(tt + 1) * 512], in_=wk[1][64:128, :], func=AF.Copy), reads=["wk1"], writes=["KT"])
        for blk in range(4):
            for kc in range(KC):
                p.op("pe", lambda e: e.matmul(wk[0][:, blk * 128:(blk + 1) * 128], xs[:, kc, blk * 128:(blk + 1) * 128],
                                              w_sb[:, kc, 256:384], start=(kc == 0), stop=(kc == KC - 1)),
                     reads=["w_sb", xk], writes=["wk0"])
        p.op("act", lambda e: e.activation(out=V[:, tt * 4:tt * 4 + 4, 0:128],
                                           in_=wk[0][:, :].rearrange("p (a b) -> p a b", a=4), func=AF.Copy),
             reads=["wk0"], writes=["V"])
        for m in range(2):
            pr = slice(m * 64, (m + 1) * 64)
            nkb = 4 * tt + 4
            for kb in range(nkb):
                b = it % 4
                it += 1
                scp, sck = scb[b]
                p.op("pe", lambda e: e.matmul(scp[:, :], KT[m][:, kb * 128:(kb + 1) * 128], QT[:, :], start=True, stop=True),
                     reads=["KT", "QT"], writes=[sck])
                p.op("act", lambda e: e.activation(out=PT[b][:, :], in_=scp[:, :], func=AF.Exp, scale=0.125),
                     reads=[sck], writes=[f"PT{b}"])
                j = kb - 4 * tt
                if j >= 0:
                    p.op("dve", lambda e: e.tensor_mul(out=PT[b][:, :], in0=PT[b][:, :], in1=mask[:, j * 512:(j + 1) * 512]),
                         reads=[f"PT{b}", "mask"], writes=[f"PT{b}"])
                for qb in range(4):
                    if j > qb:
                        continue
                    p.op("pe", lambda e: e.matmul(acc[qb][:, 0:129], PT[b][:, qb * 128:(qb + 1) * 128], V[:, kb, 0:129],
                                                  start=(kb == 0), stop=(kb == 4 * tt + qb)),
                         reads=[f"PT{b}", "V"], writes=[f"acc{qb}"])
            for qb in range(4):
                p.op("dve", lambda e: e.reciprocal(out=rc[:, qb:qb + 1], in_=acc[qb][:, 128:129]), reads=[f"acc{qb}"], writes=["rc"])
                p.op("dve", lambda e: e.tensor_scalar(out=om[m][:, qb, :], in0=acc[qb][:, 0:128], scalar1=rc[:, qb:qb + 1],
                                                      scalar2=None, op0=ALU.mult), reads=[f"acc{qb}", "rc"], writes=[f"om{m}"])
        obk = f"ob{tt % 2}"
        for qb in range(4):
            p.op("dve", lambda e: e.scalar_tensor_tensor(out=otm[:, :], in0=om[1][:, qb, :], scalar=cst[:, 2:3], in1=om[0][:, qb, :],
                                                         op0=ALU.mult, op1=ALU.add), reads=["om0", "om1", "cst"], writes=["otm"])
            p.op("dve", lambda e: e.tensor_mul(out=junk[:, :], in0=otm[:, :], in1=otm[:, :]), reads=["otm"], writes=["junk"])
            p.op("dve", lambda e: e.reduce_sum(out=rc[:, 4:5], in_=junk[:, :], axis=AX.X), reads=["junk"], writes=["rc"])
            p.op("dve", lambda e: e.tensor_scalar(out=rc[:, 4:5], in0=rc[:, 4:5], scalar1=1.0 / 128, scalar2=1e-5, op0=ALU.mult,
                                                  op1=ALU.add), reads=["rc"], writes=["rc"])
            p.op("act", lambda e: e.activation(out=rc[:, 4:5], in_=rc[:, 4:5], func=AF.Sqrt), reads=["rc"], writes=["rc"])
            p.op("dve", lambda e: e.reciprocal(out=rc[:, 4:5], in_=rc[:, 4:5]), reads=["rc"], writes=["rc"])
            p.op("dve", lambda e: e.scalar_tensor_tensor(out=otm[:, :], in0=otm[:, :], scalar=rc[:, 4:5], in1=g_sb[:, :],
                                                         op0=ALU.mult, op1=ALU.mult), reads=["otm", "rc", "g_sb"], writes=["otm"])
            p.op("pe", lambda e: e.transpose(wk[1][:, qb * 128:(qb + 1) * 128], otm[:, :], ident[:, :]), reads=["otm", "ident"], writes=["wk1"])
        p.op("act", lambda e: e.activation(out=ob[tt % 2][:, :], in_=wk[1][:, :], func=AF.Copy), reads=["wk1"], writes=[obk])
        p.dma("sp", out[:, tt * 512:(tt + 1) * 512], ob[tt % 2][:, :], reads=[obk], writes=["out"], sem="out")
    p.finish(["out"])
    return p


def stream_w(p, st, src_ap, nchunk=32):
    i = st["i"] % len(st["bufs"])
    st["i"] += 1
    buf, key = st["bufs"][i], st["keys"][i]
    p.dma("pool", buf[:, 0:nchunk, :], src_ap, writes=[key], sem=key)
    return buf, key


def ln_finish(p, s1, s2, mean, rstd, sq, eps, n=512):
    p.op("dve", lambda e: e.tensor_scalar(out=mean[:, 0:n], in0=s1[:, 0:n], scalar1=1.0 / D, scalar2=None, op0=ALU.mult), reads=["s1"], writes=["mean"])
    p.op("dve", lambda e: e.tensor_mul(out=sq[:, 0:n], in0=mean[:, 0:n], in1=mean[:, 0:n]), reads=["mean"], writes=["sq"])
    p.op("dve", lambda e: e.scalar_tensor_tensor(out=rstd[:, 0:n], in0=s2[:, 0:n], scalar=1.0 / D, in1=sq[:, 0:n], op0=ALU.mult, op1=ALU.subtract), reads=["s2", "sq"], writes=["rstd"])
    p.op("dve", lambda e: e.tensor_scalar(out=rstd[:, 0:n], in0=rstd[:, 0:n], scalar1=eps, scalar2=None, op0=ALU.add), reads=["rstd"], writes=["rstd"])
    p.op("act", lambda e: e.activation(out=rstd[:, 0:n], in_=rstd[:, 0:n], func=AF.Sqrt), reads=["rstd"], writes=["rstd"])
    p.op("dve", lambda e: e.reciprocal(out=rstd[:, 0:n], in_=rstd[:, 0:n]), reads=["rstd"], writes=["rstd"])


def build_b1(NTOK=1024):
    p = Prog()
    xT = p.din("xT", [D, NTOK])
    oT = p.din("oT", [3072, NTOK])
    memT = p.din("memT", [D, 256])
    mln = p.din("mem_ln", [128, 64])
    w_xg = p.din("w_xg", [D, 1280])
    w_kv = p.din("w_kv", [D, 2048])
    w_br = p.din("w_br", [4096, D])
    w_gu = p.din("w_gu", [256, 16384])
    bg = p.din("b_gate", [128, 128])
    w_o = p.din("w_o", [D, D])
    ln1 = p.din("ln1", [128, 64])
    x1T = p.dout("x1T", [D, NTOK])
    NT = NTOK // 512
    ones = p.sb("ones", [128, 128], F32)
    onesb = p.sb("onesb", [128, 128], BF16)
    p.op("pool", lambda e: e.memset(ones[:, :], 1.0), writes=["ones"])
    p.op("pool", lambda e: e.memset(onesb[:, :], 1.0), writes=["onesb"])
    mlns = p.sb("mlns", [128, 64], F32)
    ln1s = p.sb("ln1s", [128, 64], F32)
    bgs = p.sb("bgs", [128, 128], F32)
    p.dma("sp", mlns[:, :], mln, writes=["mlns"], sem="c0")
    p.dma("sp", ln1s[:, :], ln1, writes=["ln1s"], sem="c1")
    p.dma("sp", bgs[:, :], bg, writes=["bgs"], sem="c2")
    st = dict(i=0, bufs=[p.sb(f"wst{i}", [128, 32, 128], BF16) for i in range(6)], keys=[f"wst{i}" for i in range(6)])
    R1 = p.sb("R1", [128, 32, 512], BF16)
    obT = p.sb("obT", [128, 24, 512], BF16)
    memn = p.sb("memn", [128, 32, 256], BF16)
    mkT = p.sb("mkT", [128, 8, 256], BF16)
    mv = p.sb("mv", [128, 2, 1024], BF16)
    xqT = p.sb("xqT", [128, 8, 512], BF16)
    gdT = p.sb("gdT", [128, 2, 512], BF16)
    oxa = p.sb("oxa", [128, 8, 512], BF16)
    PT = [p.sb(f"PT{i}", [128, 512], BF16) for i in range(2)]
    fa = {n: p.sb("f_" + n, [128, 512], F32) for n in ["sq", "mean", "rstd", "gs", "macc", "tmp", "rden", "h0", "h1", "x0", "x1", "m0", "m1"]}
    wk = [p.ps(f"wk{i}") for i in range(6)]
    s1, s2 = p.ps("s1"), p.ps("s2")
    memv = memT.rearrange("(kc p) m -> p kc m", p=128)
    for ps_ in range(2):
        for kc in range(KC):
            mk_ = f"m{kc % 2}"
            p.dma("sp", fa[mk_][:, 0:256], memv[:, kc, :], writes=[mk_], sem=mk_)
            if ps_ == 0:
                p.op("act", lambda e: e.activation(out=fa["sq"][:, 0:256], in_=fa[mk_][:, 0:256], func=AF.Square), reads=[mk_], writes=["sq"])
                p.op("pe", lambda e: e.matmul(s1[:, 0:256], ones[:, :], fa[mk_][:, 0:256], start=(kc == 0), stop=(kc == KC - 1)), reads=[mk_, "ones"], writes=["s1"])
                p.op("pe", lambda e: e.matmul(s2[:, 0:256], ones[:, :], fa["sq"][:, 0:256], start=(kc == 0), stop=(kc == KC - 1)), reads=["sq", "ones"], writes=["s2"])
            else:
                p.op("dve", lambda e: e.tensor_sub(out=fa["tmp"][:, 0:256], in0=fa[mk_][:, 0:256], in1=fa["mean"][:, 0:256]), reads=[mk_, "mean"], writes=["tmp"])
                p.op("dve", lambda e: e.tensor_mul(out=fa["tmp"][:, 0:256], in0=fa["tmp"][:, 0:256], in1=fa["rstd"][:, 0:256]), reads=["tmp", "rstd"], writes=["tmp"])
                p.op("dve", lambda e: e.tensor_scalar(out=memn[:, kc, :], in0=fa["tmp"][:, 0:256], scalar1=mlns[:, kc:kc + 1], scalar2=mlns[:, 32 + kc:33 + kc], op0=ALU.mult, op1=ALU.add), reads=["tmp", "mlns"], writes=["memn"])
        if ps_ == 0:
            ln_finish(p, s1, s2, fa["mean"], fa["rstd"], fa["sq"], 1e-5, n=256)
    kvv = w_kv.rearrange("(kc p) n -> p kc n", p=128)
    for blk in range(8):
        wb, wkey = stream_w(p, st, kvv[:, :, blk * 128:(blk + 1) * 128])
        ps_ = wk[blk % 2]
        for kc in range(KC):
            p.op("pe", lambda e: e.matmul(ps_[:, 0:256], wb[:, kc, :], memn[:, kc, :], start=(kc == 0), stop=(kc == KC - 1)), reads=[wkey, "memn"], writes=[f"wk{blk % 2}"])
        p.op("act", lambda e: e.activation(out=mkT[:, blk, :], in_=ps_[:, 0:256], func=AF.Copy), reads=[f"wk{blk % 2}"], writes=["mkT"])
    for ct in range(8):
        wb, wkey = stream_w(p, st, kvv[:, :, 1024 + ct * 128:1024 + (ct + 1) * 128])
        for mb in range(2):
            ps_ = wk[mb]
            for kc in range(KC):
                p.op("pe", lambda e: e.matmul(ps_[:, 0:128], memn[:, kc, mb * 128:(mb + 1) * 128], wb[:, kc, :], start=(kc == 0), stop=(kc == KC - 1)), reads=[wkey, "memn"], writes=[f"wk{mb}"])
            p.op("act", lambda e: e.activation(out=mv[:, mb, ct * 128:(ct + 1) * 128], in_=ps_[:, 0:128], func=AF.Copy), reads=[f"wk{mb}"], writes=["mv"])
    xgv = w_xg.rearrange("(kc p) n -> p kc n", p=128)
    brv = w_br.rearrange("(c p) f -> p c f", p=128)
    guv = w_gu.rearrange("(rc p) f -> p rc f", p=128)
    wov = w_o.rearrange("(kc p) f -> p kc f", p=128)
    xTv = xT.rearrange("(kc p) t -> p kc t", p=128)
    oTv = oT.rearrange("(c p) t -> p c t", p=128)
    wg = [p.sb(f"wg{i}", [128, 4, 2, 128], BF16) for i in range(2)]
    for tt in range(NT):
        ts_ = slice(tt * 512, (tt + 1) * 512)
        for h in range(2):
            p.dma("pool", R1[:, h * 16:(h + 1) * 16, :], xTv[:, h * 16:(h + 1) * 16, ts_], writes=["R1"], sem="R1")
        for h in range(3):
            p.dma("pool", obT[:, h * 8:(h + 1) * 8, :], oTv[:, h * 8:(h + 1) * 8, ts_], writes=["obT"], sem="obT")
        for blk in range(10):
            wb, wkey = stream_w(p, st, xgv[:, :, blk * 128:(blk + 1) * 128])
            ps_, pk = wk[blk % 2], f"wk{blk % 2}"
            for kc in range(KC):
                p.op("pe", lambda e: e.matmul(ps_[:, :], wb[:, kc, :], R1[:, kc, :], start=(kc == 0), stop=(kc == KC - 1)), reads=[wkey, "R1"], writes=[pk])
            dst = xqT[:, blk, :] if blk < 8 else gdT[:, blk - 8, :]
            p.op("act", lambda e: e.activation(out=dst, in_=ps_[:, :], func=AF.Copy), reads=[pk], writes=["xqT" if blk < 8 else "gdT"])
        for h in range(4):
            for mb in range(2):
                for dc in range(2):
                    p.op("pe", lambda e: e.matmul(wk[mb][:, :], mkT[:, 2 * h + dc, mb * 128:(mb + 1) * 128], xqT[:, 2 * h + dc, :], start=(dc == 0), stop=(dc == 1)), reads=["mkT", "xqT"], writes=[f"wk{mb}"])
                p.op("act", lambda e: e.activation(out=PT[mb][:, :], in_=wk[mb][:, :], func=AF.Exp, scale=1.0 / 16), reads=[f"wk{mb}"], writes=[f"PT{mb}"])
            for mb in range(2):
                p.op("pe", lambda e: e.matmul(wk[2][:, :], onesb[:, :], PT[mb][:, :], start=(mb == 0), stop=(mb == 1)), reads=["onesb", f"PT{mb}"], writes=["wk2"])
            p.op("dve", lambda e: e.reciprocal(out=fa["rden"][:, :], in_=wk[2][:, :]), reads=["wk2"], writes=["rden"])
            for dvb in range(2):
                for mb in range(2):
                    p.op("pe", lambda e: e.matmul(wk[3 + dvb][:, :], mv[:, mb, h * 256 + dvb * 128:h * 256 + (dvb + 1) * 128], PT[mb][:, :], start=(mb == 0), stop=(mb == 1)), reads=["mv", f"PT{mb}"], writes=[f"wk{3 + dvb}"])
                p.op("dve", lambda e: e.tensor_mul(out=oxa[:, 2 * h + dvb, :], in0=wk[3 + dvb][:, :], in1=fa["rden"][:, :]), reads=[f"wk{3 + dvb}", "rden"], writes=["oxa"])
        for fb in range(32):
            wb, wkey = stream_w(p, st, brv[:, :, fb * 128:(fb + 1) * 128])
            wgb, wgk = wg[fb % 2], f"wg{fb % 2}"
            for n in range(4):
                p.dma("pool", wgb[:, n, :, :], guv[:, :, n * 4096 + fb * 128:n * 4096 + (fb + 1) * 128], writes=[wgk], sem=wgk)
            for n in range(4):
                pb, pbk = wk[n % 2], f"wk{n % 2}"
                pg, pgk = wk[2 + n % 2], f"wk{2 + n % 2}"
                for kc in range(8):
                    rhs = obT[:, n * 8 + kc, :] if n < 3 else oxa[:, kc, :]
                    p.op("pe", lambda e: e.matmul(pb[:, :], wb[:, n * 8 + kc, :], rhs, start=(kc == 0), stop=(kc == 7)), reads=[wkey, "obT" if n < 3 else "oxa"], writes=[pbk])
                for rc in range(2):
                    p.op("pe", lambda e: e.matmul(pg[:, :], wgb[:, n, rc, :], gdT[:, rc, :], start=(rc == 0), stop=(rc == 1)), reads=[wgk, "gdT"], writes=[pgk])
                p.op("act", lambda e: e.activation(out=fa["gs"][:, :], in_=pg[:, :], func=AF.Sigmoid, bias=bgs[:, n * 32 + fb:n * 32 + fb + 1]), reads=[pgk, "bgs"], writes=["gs"])
                if n == 0:
                    p.op("dve", lambda e: e.tensor_mul(out=fa["macc"][:, :], in0=pb[:, :], in1=fa["gs"][:, :]), reads=[pbk, "gs"], writes=["macc"])
                else:
                    p.op("dve", lambda e: e.tensor_mul(out=fa["tmp"][:, :], in0=pb[:, :], in1=fa["gs"][:, :]), reads=[pbk, "gs"], writes=["tmp"])
                    dst = R1[:, fb, :] if n == 3 else fa["macc"][:, :]
                    p.op("dve", lambda e: e.tensor_add(out=dst, in0=fa["macc"][:, :], in1=fa["tmp"][:, :]), reads=["macc", "tmp"], writes=["R1" if n == 3 else "macc"])
        for fb in range(32):
            wb, wkey = stream_w(p, st, wov[:, :, fb * 128:(fb + 1) * 128])
            xk_, hk_ = f"x{fb % 2}", f"h{fb % 2}"
            p.dma("sp", fa[xk_][:, :], xT[fb * 128:(fb + 1) * 128, ts_], writes=[xk_], sem=xk_)
            py, pyk = wk[fb % 2], f"wk{fb % 2}"
            for kc in range(KC):
                p.op("pe", lambda e: e.matmul(py[:, :], wb[:, kc, :], R1[:, kc, :], start=(kc == 0), stop=(kc == KC - 1)), reads=[wkey, "R1"], writes=[pyk])
            p.op("dve", lambda e: e.scalar_tensor_tensor(out=fa[hk_][:, :], in0=fa[xk_][:, :], scalar=ALPHA, in1=py[:, :], op0=ALU.mult, op1=ALU.add), reads=[xk_, pyk], writes=[hk_])
            p.op("act", lambda e: e.activation(out=fa["sq"][:, :], in_=fa[hk_][:, :], func=AF.Square), reads=[hk_], writes=["sq"])
            p.op("pe", lambda e: e.matmul(s1[:, :], ones[:, :], fa[hk_][:, :], start=(fb == 0), stop=(fb == 31)), reads=[hk_, "ones"], writes=["s1"])
            p.op("pe", lambda e: e.matmul(s2[:, :], ones[:, :], fa["sq"][:, :], start=(fb == 0), stop=(fb == 31)), reads=["sq", "ones"], writes=["s2"])
            p.dma("sp", x1T[fb * 128:(fb + 1) * 128, ts_], fa[hk_][:, :], reads=[hk_], writes=["x1T"], sem="hout")
        ln_finish(p, s1, s2, fa["mean"], fa["rstd"], fa["sq"], 1e-5)
        for fb in range(32):
            hk_, xk_ = f"h{fb % 2}", f"x{fb % 2}"
            p.dma("sp", fa[hk_][:, :], x1T[fb * 128:(fb + 1) * 128, ts_], reads=["x1T"], writes=[hk_], sem=hk_)
            p.op("dve", lambda e: e.tensor_sub(out=fa["tmp"][:, :], in0=fa[hk_][:, :], in1=fa["mean"][:, :]), reads=[hk_, "mean"], writes=["tmp"])
            p.op("dve", lambda e: e.tensor_mul(out=fa["tmp"][:, :], in0=fa["tmp"][:, :], in1=fa["rstd"][:, :]), reads=["tmp", "rstd"], writes=["tmp"])
            p.op("dve", lambda e: e.tensor_scalar(out=fa[xk_][:, :], in0=fa["tmp"][:, :], scalar1=ln1s[:, fb:fb + 1], scalar2=ln1s[:, 32 + fb:33 + fb], op0=ALU.mult, op1=ALU.add), reads=["tmp", "ln1s"], writes=[xk_])
            p.dma("sp", x1T[fb * 128:(fb + 1) * 128, ts_], fa[xk_][:, :], reads=[xk_, "x1T"], writes=["x1T_f"], sem="xout")
    p.finish(["x1T_f"])
    return p


def build_b2(NTOK=1024):
    p = Prog()
    x1T = p.din("x1T", [D, NTOK])
    rw = p.din("router_w", [D, 32])
    rb = p.din("router_b", [32, 1])
    w1 = p.din("w1", [32 * D, 512])
    b1 = p.din("b1", [128, 128])
    w2 = p.din("w2", [32 * 256, D])
    b2 = p.din("b2", [32, D])
    ln2 = p.din("ln2", [128, 64])
    cid = p.din("c_ident", [128, 128])
    x2T = p.dout("x2T", [D, NTOK])
    NT = NTOK // 512
    ident = p.sb("ident", [128, 128], F32)
    ones = p.sb("ones", [128, 128], F32)
    p.dma("sp", ident[:, :], cid, writes=["ident"], sem="c0")
    p.op("pool", lambda e: e.memset(ones[:, :], 1.0), writes=["ones"])
    rws = p.sb("rws", [128, 32, 32], F32)
    rbs = p.sb("rbs", [32, 1], F32)
    b1s = p.sb("b1s", [128, 128], F32)
    ln2s = p.sb("ln2s", [128, 64], F32)
    p.dma("sp", rws[:, :, :], rw.rearrange("(kc p) e -> p kc e", p=128), writes=["rws"], sem="c1")
    p.dma("sp", rbs[:, :], rb, writes=["rbs"], sem="c2")
    p.dma("sp", b1s[:, :], b1, writes=["b1s"], sem="c3")
    p.dma("sp", ln2s[:, :], ln2, writes=["ln2s"], sem="c4")
    xb = p.sb("xb", [128, 32, 512], BF16)
    AT = p.sb("AT", [128, 64, 512], BF16)
    w1b = [p.sb(f"w1b{i}", [128, 8, 512], BF16) for i in range(3)]
    w2b = [p.sb(f"w2b{i}", [128, 64, 128], BF16) for i in range(2)]
    b2s = [p.sb(f"b2s{i}", [32, 128], F32) for i in range(2)]
    fa = {n: p.sb("f_" + n, [128, 512], F32) for n in ["sq", "mean", "rstd", "g", "sg", "l", "tmp", "h0", "h1", "x0", "x1", "lgT", "gT", "gm"]}
    lgt = p.sb("lgt", [128, 4, 32], F32)
    gt = p.sb("gt", [128, 4, 32], F32)
    m8 = p.sb("m8", [128, 8], F32)
    sm = p.sb("sm", [128, 4], F32)
    hp = [p.ps(f"hp{i}") for i in range(4)]
    wk = [p.ps("wk0"), p.ps("wk1")]
    s1, s2 = p.ps("s1"), p.ps("s2")
    w1v = w1.rearrange("(e kc p) f -> p e kc f", p=128, kc=32)
    w2v = w2.rearrange("(c p) d -> p c d", p=128)
    wi = 0
    for tt in range(NT):
        ts_ = slice(tt * 512, (tt + 1) * 512)
        for kc in range(KC):
            xk_ = f"x{kc % 2}"
            p.dma("sp", fa[xk_][:, :], x1T[kc * 128:(kc + 1) * 128, ts_], writes=[xk_], sem=xk_)
            p.op("pe", lambda e: e.matmul(wk[0][0:32, :], rws[:, kc, :], fa[xk_][:, :], start=(kc == 0), stop=(kc == KC - 1)), reads=["rws", xk_], writes=["wk0"])
            p.op("act", lambda e: e.activation(out=xb[:, kc, :], in_=fa[xk_][:, :], func=AF.Copy), reads=[xk_], writes=["xb"])
        p.op("dve", lambda e: e.tensor_scalar(out=fa["lgT"][0:32, :], in0=wk[0][0:32, :], scalar1=rbs[:, 0:1], scalar2=None, op0=ALU.add), reads=["wk0", "rbs"], writes=["lgT"])
        for q in range(4):
            p.op("pe", lambda e: e.transpose(wk[1][:, q * 32:(q + 1) * 32], fa["lgT"][0:32, q * 128:(q + 1) * 128], ident[0:32, 0:32]), reads=["lgT", "ident"], writes=["wk1"])
        p.op("dve", lambda e: e.tensor_copy(out=lgt[:, :, :], in_=wk[1][:, 0:128].rearrange("p (a b) -> p a b", a=4)), reads=["wk1"], writes=["lgt"])
        for q in range(4):
            p.op("dve", lambda e: e.max(out=m8[:, :], in_=lgt[:, q, :]), reads=["lgt"], writes=["m8"])
            p.op("dve", lambda e: e.tensor_scalar(out=sm[:, 0:1], in0=m8[:, 0:1], scalar1=-1.0, scalar2=None, op0=ALU.mult), reads=["m8"], writes=["sm"])
            p.op("act", lambda e: e.activation(out=gt[:, q, :], in_=lgt[:, q, :], func=AF.Exp, bias=sm[:, 0:1]), reads=["lgt", "sm"], writes=["gt"])
            p.op("dve", lambda e: e.scalar_tensor_tensor(out=gt[:, q, :], in0=lgt[:, q, :], scalar=m8[:, 3:4], in1=gt[:, q, :], op0=ALU.is_ge, op1=ALU.mult), reads=["lgt", "m8", "gt"], writes=["gt"])
            p.op("dve", lambda e: e.reduce_sum(out=sm[:, 1:2], in_=gt[:, q, :], axis=AX.X), reads=["gt"], writes=["sm"])
            p.op("dve", lambda e: e.reciprocal(out=sm[:, 1:2], in_=sm[:, 1:2]), reads=["sm"], writes=["sm"])
            p.op("dve", lambda e: e.tensor_scalar(out=gt[:, q, :], in0=gt[:, q, :], scalar1=sm[:, 1:2], scalar2=None, op0=ALU.mult), reads=["gt", "sm"], writes=["gt"])
            p.op("pe", lambda e: e.transpose(wk[0][0:32, q * 128:(q + 1) * 128], gt[:, q, :], ident[:, :]), reads=["gt", "ident"], writes=["wk0"])
        p.op("act", lambda e: e.activation(out=fa["gT"][0:32, :], in_=wk[0][0:32, :], func=AF.Copy), reads=["wk0"], writes=["gT"])
        for ex in range(32):
            for qd in range(4):
                wb, wkey = w1b[wi % 3], f"w1b{wi % 3}"
                wi += 1
                p.dma("pool", wb[:, :, :], w1v[:, ex, qd * 8:(qd + 1) * 8, :], writes=[wkey], sem=wkey)
                for k8 in range(8):
                    kc = qd * 8 + k8
                    for blk in range(4):
                        c0 = (blk % 2) * 256 + (blk // 2)
                        p.op("pe", lambda e: e.matmul(hp[blk][:, :], wb[:, k8, c0:min(c0 + 256, 512):2], xb[:, kc, :], start=(kc == 0), stop=(kc == KC - 1)), reads=[wkey, "xb"], writes=[f"hp{blk}"])
            p.op("dve", lambda e: e.tensor_scalar(out=fa["gm"][0:32, :], in0=fa["gT"][0:32, :], scalar1=ident[0:32, ex:ex + 1], scalar2=None, op0=ALU.mult), reads=["gT", "ident"], writes=["gm"])
            p.op("pe", lambda e: e.matmul(wk[1][:, :], ones[0:32, :], fa["gm"][0:32, :], start=True, stop=True), reads=["gm", "ones"], writes=["wk1"])
            for j in range(2):
                bc = ex * 4 + j
                p.op("dve", lambda e: e.tensor_scalar(out=fa["g"][:, :], in0=hp[j][:, :], scalar1=b1s[:, bc:bc + 1], scalar2=7.0, op0=ALU.add, op1=ALU.min), reads=[f"hp{j}", "b1s"], writes=["g"])
                p.op("act", lambda e: e.activation(out=fa["sg"][:, :], in_=fa["g"][:, :], func=AF.Sigmoid, scale=1.702), reads=["g"], writes=["sg"])
                p.op("dve", lambda e: e.tensor_scalar(out=fa["l"][:, :], in0=hp[2 + j][:, :], scalar1=b1s[:, bc + 2:bc + 3], scalar2=7.0, op0=ALU.add, op1=ALU.min), reads=[f"hp{2 + j}", "b1s"], writes=["l"])
                p.op("dve", lambda e: e.tensor_scalar(out=fa["l"][:, :], in0=fa["l"][:, :], scalar1=-7.0, scalar2=1.0, op0=ALU.max, op1=ALU.add), reads=["l"], writes=["l"])
                p.op("pool", lambda e: e.tensor_tensor(out=fa["g"][:, :], in0=fa["g"][:, :], in1=fa["sg"][:, :], op=ALU.mult), reads=["g", "sg"], writes=["g"])
                p.op("pool", lambda e: e.tensor_tensor(out=fa["g"][:, :], in0=fa["g"][:, :], in1=fa["l"][:, :], op=ALU.mult), reads=["g", "l"], writes=["g"])
                p.op("dve", lambda e: e.tensor_mul(out=AT[:, ex * 2 + j, :], in0=wk[1][:, :], in1=fa["g"][:, :]), reads=["wk1", "g"], writes=["AT"])
        for fb in range(32):
            wb, wkey = w2b[fb % 2], f"w2b{fb % 2}"
            for h in range(2):
                p.dma("pool", wb[:, h * 32:(h + 1) * 32, :], w2v[:, h * 32:(h + 1) * 32, fb * 128:(fb + 1) * 128], writes=[wkey], sem=wkey)
            bb, bk = b2s[fb % 2], f"b2s{fb % 2}"
            p.dma("sp", bb[:, :], b2[:, fb * 128:(fb + 1) * 128], writes=[bk], sem=bk)
            xk_, hk_ = f"x{fb % 2}", f"h{fb % 2}"
            p.dma("sp", fa[xk_][:, :], x1T[fb * 128:(fb + 1) * 128, ts_], writes=[xk_], sem=xk_)
            py, pyk = hp[fb % 2], f"hp{fb % 2}"
            for c in range(64):
                p.op("pe", lambda e: e.matmul(py[:, :], wb[:, c, :], AT[:, c, :], start=(c == 0), stop=(c == 63)), reads=[wkey, "AT"], writes=[pyk])
            pb, pbk = hp[2 + fb % 2], f"hp{2 + fb % 2}"
            p.op("pe", lambda e: e.matmul(pb[:, :], bb[:, :], fa["gT"][0:32, :], start=True, stop=True), reads=[bk, "gT"], writes=[pbk])
            p.op("dve", lambda e: e.scalar_tensor_tensor(out=fa["tmp"][:, :], in0=fa[xk_][:, :], scalar=ALPHA, in1=py[:, :], op0=ALU.mult, op1=ALU.add), reads=[xk_, pyk], writes=["tmp"])
            p.op("dve", lambda e: e.tensor_add(out=fa[hk_][:, :], in0=fa["tmp"][:, :], in1=pb[:, :]), reads=["tmp", pbk], writes=[hk_])
            p.op("act", lambda e: e.activation(out=fa["sq"][:, :], in_=fa[hk_][:, :], func=AF.Square), reads=[hk_], writes=["sq"])
            p.op("pe", lambda e: e.matmul(s1[:, :], ones[:, :], fa[hk_][:, :], start=(fb == 0), stop=(fb == 31)), reads=[hk_, "ones"], writes=["s1"])
            p.op("pe", lambda e: e.matmul(s2[:, :], ones[:, :], fa["sq"][:, :], start=(fb == 0), stop=(fb == 31)), reads=["sq", "ones"], writes=["s2"])
            p.dma("sp", x2T[fb * 128:(fb + 1) * 128, ts_], fa[hk_][:, :], reads=[hk_], writes=["x2T"], sem="hout")
        ln_finish(p, s1, s2, fa["mean"], fa["rstd"], fa["sq"], 1e-5)
        for fb in range(32):
            hk_, xk_ = f"h{fb % 2}", f"x{fb % 2}"
            p.dma("sp", fa[hk_][:, :], x2T[fb * 128:(fb + 1) * 128, ts_], reads=["x2T"], writes=[hk_], sem=hk_)
            p.op("dve", lambda e: e.tensor_sub(out=fa["tmp"][:, :], in0=fa[hk_][:, :], in1=fa["mean"][:, :]), reads=[hk_, "mean"], writes=["tmp"])
            p.op("dve", lambda e: e.tensor_mul(out=fa["tmp"][:, :], in0=fa["tmp"][:, :], in1=fa["rstd"][:, :]), reads=["tmp", "rstd"], writes=["tmp"])
            p.op("dve", lambda e: e.tensor_scalar(out=fa[xk_][:, :], in0=fa["tmp"][:, :], scalar1=ln2s[:, fb:fb + 1], scalar2=ln2s[:, 32 + fb:33 + fb], op0=ALU.mult, op1=ALU.add), reads=["tmp", "ln2s"], writes=[xk_])
            p.dma("sp", x2T[fb * 128:(fb + 1) * 128, ts_], fa[xk_][:, :], reads=[xk_, "x2T"], writes=["x2T_f"], sem="xout")
    p.finish(["x2T_f"])
    return p


def _chunk(v):
    return np.ascontiguousarray(np.asarray(v, np.float32).reshape(32, 128).T)


_PROGS = {}


def _prog(name, fn):
    return fn()


def _run(p, ins):
    return run_bass_kernel_spmd(p.nc, ins, core_ids=list(range(NCORE))).results


def kernel(**inputs):
    z = {k: np.asarray(v) for k, v in inputs.items()}
    x = np.ascontiguousarray(z["x"][0], dtype=np.float32)
    T = x.shape[0]
    ident = np.eye(128, dtype=np.float32)
    memT = np.ascontiguousarray(z["mem"][0].T)
    mem_ln = np.concatenate([_chunk(z["mem_ln_g"]), _chunk(z["mem_ln_b"])], axis=1)
    kk = np.arange(128)[:, None]
    qq = np.arange(512)[None, :]
    dmask = np.concatenate([(kk + 128 * j <= qq).astype(np.float32) for j in range(4)], axis=1)
    crw = np.zeros((128, 256), np.float32)
    crw[:, 0:64] = np.tile(np.eye(64, dtype=np.float32), (2, 1))
    crw[0:64, 64:128] = 1
    crw[64:128, 128:192] = 1
    RW0 = 7 * 1024
    for l in range(4):
        w_in = z["w_in"][l]
        xT = np.ascontiguousarray(x.T)
        ins_hg, ins_df, ins_rw = [], [], []
        li = 0.8 - 0.6 * math.exp(-0.3 * l)
        for c in range(NCORE):
            ch = np.arange(c * 128, (c + 1) * 128)
            lm = np.zeros((128, 4), np.float32)
            lm[:, 1:l + 1] = 1.0
            ins_hg.append({"xT": xT, "w_hg": np.ascontiguousarray(w_in[:, np.concatenate([j * 1024 + ch for j in range(4)])]),
                           "lb_raw": np.ascontiguousarray(z["hg_lb_raw"][:, ch].T), "lmask": lm,
                           "norm_g": np.ascontiguousarray(z["hg_norm_g"][l][:, None]), "c_ident": ident,
                           "c_tri": np.triu(np.ones((64, 64), np.float32))})
            lamv = np.tile(np.concatenate([z["df_lam_q1"][l], z["df_lam_k1"][l], z["df_lam_q2"][l], z["df_lam_k2"][l]])[None, :], (128, 1)).astype(np.float32)
            ins_df.append({"xT": xT, "w_df": np.ascontiguousarray(w_in[:, np.concatenate([4096 + ch, 5120 + ch, 6144 + ch])]),
                           "df_lamv": lamv, "df_const": np.tile(np.array([[li, 1 - li]], np.float32), (128, 1)),
                           "df_g": np.tile(z["df_subln_g"][l][None, :], (128, 1)).astype(np.float32), "df_mask": dmask, "c_ident": ident})
            cols = np.concatenate([RW0 + ch, RW0 + 1024 + ch, RW0 + 2048 + ch, RW0 + 3072 + np.arange(288)])
            mu_all = z["rw_shift_mu"][l]
            mu = np.zeros((128, 6), np.float32)
            mu[:, 0] = mu_all[ch]
            mu[:, 1] = mu_all[1024 + ch]
            mu[:, 2] = mu_all[2048 + ch]
            mu[:, 3] = mu_all[3072:3200]
            mu[:, 4] = mu_all[3200:3328]
            mu[:32, 5] = mu_all[3328:3360]
            lw = np.zeros((128, 512), np.float32)
            lw[0:64, 0:128] = z["rw_w2"][l][:, ch]
            lw[64:128, 0:128] = z["rw_a2"][l][:, ch]
            lw[:, 128:256] = z["rw_g2"][l][0:128][:, ch]
            lw[0:32, 256:384] = z["rw_g2"][l][128:160][:, ch]
            vec = np.zeros((128, 8), np.float32)
            for i_, n_ in enumerate(["rw_w0", "rw_a0", "rw_k_k", "rw_k_a"]):
                vec[:, i_] = z[n_][l][ch]
            vec[:, 4] = z["rw_r_k"][l].reshape(-1)[ch]
            vec[:, 5] = z["rw_lnx_g"][l][ch]
            vec[:, 6] = z["rw_lnx_b"][l][ch]
            ins_rw.append({"xT": xT, "w_rw": np.ascontiguousarray(w_in[:, cols]), "rw_mu": mu, "rw_lora": lw, "rw_vec": vec, "c_rw": crw})
        r_hg = _run(_prog("hg", lambda: build_hgrn2c(T)), ins_hg)
        r_df = _run(_prog("df", lambda: build_diff(T)), ins_df)
        r_rw = _run(_prog("rw", lambda: build_rwkv(T, self_sync=False)), ins_rw)
        oT_all = np.concatenate([r_hg[c]["o_hg"] for c in range(NCORE)] + [r_df[c]["o_df"] for c in range(NCORE)]
                                + [r_rw[c]["o_rw"] for c in range(NCORE)], axis=0)
        del r_hg, r_df, r_rw, ins_hg, ins_df, ins_rw
        sh = {"memT": memT, "mem_ln": mem_ln, "w_xg": np.ascontiguousarray(w_in[:, 10528:11808]), "w_kv": z["w_mem_kv"][l],
              "w_br": z["w_br"][l].reshape(4096, 4096), "w_gu": z["w_gate_up"][l],
              "b_gate": np.ascontiguousarray(z["b_gate"][l].reshape(4, 32, 128).transpose(2, 0, 1).reshape(128, 128)),
              "w_o": z["w_o"][l], "ln1": np.concatenate([_chunk(z["ln1_g"][l]), _chunk(z["ln1_b"][l])], axis=1)}
        nt = T // NCORE
        ins = []
        for c in range(NCORE):
            d = dict(sh)
            d["xT"] = np.ascontiguousarray(xT[:, c * nt:(c + 1) * nt])
            d["oT"] = np.ascontiguousarray(oT_all[:, c * nt:(c + 1) * nt])
            ins.append(d)
        r1 = _run(_prog("b1", lambda: build_b1(nt)), ins)
        b1 = z["exp_b1"][l]
        b1l = np.stack([b1[:, 0:256:2], b1[:, 256:512:2], b1[:, 1:256:2], b1[:, 257:512:2]], axis=1)
        sh = {"router_w": z["router_w"][l], "router_b": np.ascontiguousarray(z["router_b"][l][:, None]),
              "w1": z["exp_w1"][l].reshape(32 * 4096, 512), "b1": np.ascontiguousarray(b1l.transpose(2, 0, 1).reshape(128, 128)),
              "w2": z["exp_w2"][l].reshape(32 * 256, 4096), "b2": z["exp_b2"][l],
              "ln2": np.concatenate([_chunk(z["ln2_g"][l]), _chunk(z["ln2_b"][l])], axis=1), "c_ident": ident}
        ins = []
        for c in range(NCORE):
            d = dict(sh)
            d["x1T"] = r1[c]["x1T"]
            ins.append(d)
        r2 = _run(_prog("b2", lambda: build_b2(nt)), ins)
        x = np.ascontiguousarray(np.concatenate([r2[c]["x2T"].T for c in range(NCORE)], axis=0), dtype=np.float32)
        del r1, r2, ins
    return x[None].astype(np.float32)
```
